# Optimizing a Trainium2 kernel written in Bass

```python
import jax, jax.numpy as jnp
from jax import lax
import numpy as np

D_MODEL = 2048
BATCH = 2
SEQ = 8192
DEPTH = 2

GRID_W = 64
CTX_LEN = 256
ROPE_THETA = 10000.0
NORM_EPS = 1e-6
Q_BLOCK = 128

GQA_HEADS = 8
GQA_KV_HEADS = 2
HEAD_DIM = 128
GLA_HEADS = 4
GLA_DK = 128
GLA_DV = 256
GLA_GATE_RANK = 16
GLA_TAU = 16.0
GLA_CHUNK = 64
MLA_HEADS = 8
MLA_Q_RANK = 512
MLA_KV_RANK = 512
MLA_NOPE = 128
MLA_ROPE = 64
MLA_V = 128
GQA_W = GQA_HEADS * HEAD_DIM
GLA_W = GLA_HEADS * GLA_DV
MLA_W = MLA_HEADS * MLA_V
PEER_HEADS = 8
PEER_NKEYS = 128
PEER_EXPERTS = PEER_NKEYS * PEER_NKEYS
PEER_QDIM = 256
PEER_HALF = PEER_QDIM // 2
PEER_TOPK = 16
PEER_BLOCK = 128

IN_SPLITS = (GQA_HEADS * HEAD_DIM, GQA_KV_HEADS * HEAD_DIM, GQA_KV_HEADS * HEAD_DIM,
             GLA_HEADS * GLA_DK, GLA_HEADS * GLA_DK, GLA_HEADS * GLA_DV, GLA_HEADS * GLA_DV,
             GLA_GATE_RANK, GLA_GATE_RANK,
             MLA_Q_RANK, MLA_KV_RANK, MLA_ROPE,
             D_MODEL, D_MODEL, D_MODEL)
IN_COLS = sum(IN_SPLITS)

kernel_name = 'hybrid_gqa_gla_mla_peer_dit'


def rms_norm(x, g):
    xf = x.astype(jnp.float32)
    y = xf * lax.rsqrt(jnp.mean(xf * xf, axis=-1, keepdims=True) + NORM_EPS)
    return (y * g.astype(jnp.float32)).astype(x.dtype)


def split_cols(p, sizes):
    out, off = [], 0
    for n in sizes:
        out.append(p[..., off:off + n])
        off += n
    return out


def axial_rope(row_idx, col_idx, dim):
    quarter = dim // 4
    inv_freq = ROPE_THETA ** (-jnp.arange(quarter, dtype=jnp.float32) / quarter)
    ang = jnp.concatenate([row_idx[:, None].astype(jnp.float32) * inv_freq,
                           col_idx[:, None].astype(jnp.float32) * inv_freq], axis=-1)
    return jnp.cos(ang), jnp.sin(ang)


def apply_rope(x, cos, sin):
    half = x.shape[-1] // 2
    xf = x.astype(jnp.float32)
    x1, x2 = xf[..., :half], xf[..., half:]
    cs, sn = cos[None, :, None, :], sin[None, :, None, :]
    return jnp.concatenate([x1 * cs - x2 * sn, x2 * cs + x1 * sn], axis=-1).astype(x.dtype)


def block_attention(q, k, v):
    B, Sq, Hkv, G, dq = q.shape
    dv = v.shape[-1]
    nblk = Sq // Q_BLOCK
    scale = dq ** -0.5
    qb = q.reshape(B, nblk, Q_BLOCK, Hkv, G, dq).transpose(1, 0, 2, 3, 4, 5)

    def one_block(qblk):
        s = jnp.einsum('bqhgd,bkhd->bhgqk', qblk, k, preferred_element_type=jnp.float32) * scale
        p = jax.nn.softmax(s, axis=-1)
        return jnp.einsum('bhgqk,bkhe->bqhge', p.astype(v.dtype), v)

    o = lax.map(one_block, qb)
    return o.transpose(1, 0, 2, 3, 4, 5).reshape(B, Sq, Hkv, G, dv)


def gla_scan(q, k, v, log_a, state0):
    B, S, H, dk = q.shape
    dv = v.shape[-1]
    n = S // GLA_CHUNK

    def chunks(t):
        return t.reshape(B, n, GLA_CHUNK, H, t.shape[-1]).transpose(1, 0, 3, 2, 4).astype(jnp.float32)

    tri = jnp.tril(jnp.ones((GLA_CHUNK, GLA_CHUNK), dtype=bool))[:, :, None]

    def step(state, inp):
        qc, kc, vc, ac = inp
        b = jnp.cumsum(ac, axis=2)
        b_end = b[:, :, -1:, :]
        o_inter = jnp.einsum('bhtd,bhde->bhte', qc * jnp.exp(b), state)
        decay = jnp.exp(jnp.where(tri, b[:, :, :, None, :] - b[:, :, None, :, :], -jnp.inf))
        scores = jnp.einsum('bhtd,bhsd,bhtsd->bhts', qc, kc, decay)
        o_intra = jnp.einsum('bhts,bhse->bhte', scores, vc)
        new_state = (jnp.exp(b_end[:, :, 0, :, None]) * state
                     + jnp.einsum('bhsd,bhse->bhde', kc * jnp.exp(b_end - b), vc))
        return new_state, o_inter + o_intra

    state, o = lax.scan(step, state0, (chunks(q), chunks(k), chunks(v), chunks(log_a)))
    o = o.transpose(1, 0, 3, 2, 4).reshape(B, S, H, dv).astype(v.dtype)
    return o, state


def gla_scan_reverse(q, k, v, log_a, state0):
    o, state = gla_scan(jnp.flip(q, 1), jnp.flip(k, 1), jnp.flip(v, 1), jnp.flip(log_a, 1), state0)
    return jnp.flip(o, 1), state


def stream_inputs(p, lp, rope_a, rope_m):
    B, S = p.shape[:2]
    aq, ak, av, lq, lk, lv, lg, lrf, lrb, mcq, mckv, mkr, ga, gl, gm = split_cols(p, IN_SPLITS)
    aq = rms_norm(aq.reshape(B, S, GQA_HEADS, HEAD_DIM), lp['gqa_qn_g'])
    ak = rms_norm(ak.reshape(B, S, GQA_KV_HEADS, HEAD_DIM), lp['gqa_kn_g'])
    mq = (rms_norm(mcq, lp['mla_qn_g']) @ lp['mla_wuq']).reshape(B, S, MLA_HEADS, MLA_NOPE + MLA_ROPE)
    mkv = (rms_norm(mckv, lp['mla_kvn_g']) @ lp['mla_wukv']).reshape(B, S, MLA_HEADS, MLA_NOPE + MLA_V)
    mq_nope, mq_rope = mq[..., :MLA_NOPE], mq[..., MLA_NOPE:]
    mk_nope, mv = mkv[..., :MLA_NOPE], mkv[..., MLA_NOPE:]
    mk_rope = mkr.reshape(B, S, 1, MLA_ROPE)
    if rope_a is not None:
        aq = apply_rope(aq, *rope_a)
        ak = apply_rope(ak, *rope_a)
        mq_rope = apply_rope(mq_rope, *rope_m)
        mk_rope = apply_rope(mk_rope, *rope_m)

    def log_decay(lr, w, bias):
        pre = (lr @ w + bias).astype(jnp.float32)
        return (jax.nn.log_sigmoid(pre) / GLA_TAU).reshape(B, S, GLA_HEADS, GLA_DK)

    return dict(
        a_q=aq.reshape(B, S, GQA_KV_HEADS, GQA_HEADS // GQA_KV_HEADS, HEAD_DIM),
        a_k=ak,
        a_v=av.reshape(B, S, GQA_KV_HEADS, HEAD_DIM),
        l_q=lq.reshape(B, S, GLA_HEADS, GLA_DK) * GLA_DK ** -0.5,
        l_k=lk.reshape(B, S, GLA_HEADS, GLA_DK),
        l_v=lv.reshape(B, S, GLA_HEADS, GLA_DV),
        l_g=lg,
        l_af=log_decay(lrf, lp['gla_wa2_f'], lp['gla_ba_f']),
        l_ab=log_decay(lrb, lp['gla_wa2_b'], lp['gla_ba_b']),
        m_q=jnp.concatenate([mq_nope, mq_rope], axis=-1)[:, :, :, None, :],
        m_k=jnp.concatenate([mk_nope, jnp.broadcast_to(mk_rope, (B, S, MLA_HEADS, MLA_ROPE))], axis=-1),
        m_v=mv,
        gates=(ga, gl, gm),
    )


def gla_readout(o, gate, g):
    B, S = o.shape[:2]
    y = rms_norm(o, g) * jax.nn.silu(gate.reshape(B, S, GLA_HEADS, GLA_DV))
    return y.reshape(B, S, GLA_W)


def merge_branches(o_a, o_l, o_m, gates, lp):
    ga, gl, gm = gates
    y = (jax.nn.sigmoid(ga) * (o_a @ lp['w_br_gqa'])
         + jax.nn.sigmoid(gl) * (o_l @ lp['w_br_gla'])
         + jax.nn.sigmoid(gm) * (o_m @ lp['w_br_mla']))
    return y @ lp['w_out']


def token_mixer(h_l, h_c, lp, rope_a, rope_m, need_ctx):
    B, S = h_l.shape[:2]
    Sc = h_c.shape[1]
    L = stream_inputs(h_l @ lp['w_in'], lp, rope_a, rope_m)
    C = stream_inputs(h_c @ lp['w_in'], lp, None, None)
    o_a = block_attention(L['a_q'], jnp.concatenate([C['a_k'], L['a_k']], axis=1),
                          jnp.concatenate([C['a_v'], L['a_v']], axis=1)).reshape(B, S, GQA_W)
    o_m = block_attention(L['m_q'], jnp.concatenate([C['m_k'], L['m_k']], axis=1),
                          jnp.concatenate([C['m_v'], L['m_v']], axis=1)).reshape(B, S, MLA_W)
    zero = jnp.zeros((B, GLA_HEADS, GLA_DK, GLA_DV), jnp.float32)
    oc_f, st_f = gla_scan(C['l_q'], C['l_k'], C['l_v'], C['l_af'], zero)
    oc_b, st_b = gla_scan_reverse(C['l_q'], C['l_k'], C['l_v'], C['l_ab'], zero)
    ol_f, _ = gla_scan(L['l_q'], L['l_k'], L['l_v'], L['l_af'], st_f)
    ol_b, _ = gla_scan_reverse(L['l_q'], L['l_k'], L['l_v'], L['l_ab'], st_b)
    o_l = gla_readout(ol_f + ol_b, L['l_g'], lp['gla_on_g'])
    y_l = merge_branches(o_a, o_l, o_m, L['gates'], lp)
    if not need_ctx:
        return y_l, None
    oc_a = block_attention(C['a_q'], C['a_k'], C['a_v']).reshape(B, Sc, GQA_W)
    oc_m = block_attention(C['m_q'], C['m_k'], C['m_v']).reshape(B, Sc, MLA_W)
    oc_l = gla_readout(oc_f + oc_b, C['l_g'], lp['gla_on_g'])
    y_c = merge_branches(oc_a, oc_l, oc_m, C['gates'], lp)
    return y_l, y_c


def peer_ffn(h, wq, k1, k2, u_tab, v_tab):
    T, D = h.shape
    n = T // PEER_BLOCK

    def one_block(hb):
        q = (hb @ wq).reshape(PEER_BLOCK, PEER_HEADS, 2, PEER_HALF)
        s1 = jnp.einsum('thd,hnd->thn', q[:, :, 0], k1, preferred_element_type=jnp.float32)
        s2 = jnp.einsum('thd,hnd->thn', q[:, :, 1], k2, preferred_element_type=jnp.float32)
        v1, i1 = lax.top_k(s1, PEER_TOPK)
        v2, i2 = lax.top_k(s2, PEER_TOPK)
        ncand = PEER_TOPK * PEER_TOPK
        cand = (v1[..., :, None] + v2[..., None, :]).reshape(PEER_BLOCK, PEER_HEADS, ncand)
        cand_idx = (i1[..., :, None] * PEER_NKEYS + i2[..., None, :]).reshape(PEER_BLOCK, PEER_HEADS, ncand)
        best, pos = lax.top_k(cand, PEER_TOPK)
        idx = jnp.take_along_axis(cand_idx, pos, axis=-1)
        g = jax.nn.softmax(best, axis=-1)
        act = jax.nn.gelu(jnp.einsum('td,thkd->thk', hb, u_tab[idx]), approximate=False)
        return jnp.einsum('thk,thkd->td', (g * act).astype(hb.dtype), v_tab[idx])

    return lax.map(one_block, h.reshape(n, PEER_BLOCK, D)).reshape(T, D)


def ada_params(cvec, w, b):
    return jnp.split(jax.nn.silu(cvec) @ w + b, 6, axis=-1)


def setup_inputs(seed: int = 0) -> dict:
    key = jax.random.key(seed)
    ks = jax.random.split(key, 40)
    counter = iter(range(40))
    L, D = DEPTH, D_MODEL

    def normal(shape, scale):
        return jax.random.normal(ks[next(counter)], shape, jnp.float32) * scale

    def gain(shape):
        return 1.0 + normal(shape, 0.02)

    return {
        'x': normal((BATCH, SEQ, D), 1.0),
        'c': normal((BATCH, D), 1.0),
        'ctx': normal((BATCH, CTX_LEN, D), 1.0),
        'c_ctx': normal((D,), 1.0),
        'w_mod': normal((L, D, 6 * D), 0.5 * D ** -0.5),
        'b_mod': normal((L, 6 * D), 0.01),
        'norm1_g': gain((L, D)),
        'w_in': normal((L, D, IN_COLS), D ** -0.5),
        'gqa_qn_g': gain((L, HEAD_DIM)),
        'gqa_kn_g': gain((L, HEAD_DIM)),
        'gla_wa2_f': normal((L, GLA_GATE_RANK, GLA_HEADS * GLA_DK), GLA_GATE_RANK ** -0.5),
        'gla_ba_f': normal((L, GLA_HEADS * GLA_DK), 0.1),
        'gla_wa2_b': normal((L, GLA_GATE_RANK, GLA_HEADS * GLA_DK), GLA_GATE_RANK ** -0.5),
        'gla_ba_b': normal((L, GLA_HEADS * GLA_DK), 0.1),
        'gla_on_g': gain((L, GLA_DV)),
        'mla_qn_g': gain((L, MLA_Q_RANK)),
        'mla_wuq': normal((L, MLA_Q_RANK, MLA_HEADS * (MLA_NOPE + MLA_ROPE)), MLA_Q_RANK ** -0.5),
        'mla_kvn_g': gain((L, MLA_KV_RANK)),
        'mla_wukv': normal((L, MLA_KV_RANK, MLA_HEADS * (MLA_NOPE + MLA_V)), MLA_KV_RANK ** -0.5),
        'w_br_gqa': normal((L, GQA_W, D), GQA_W ** -0.5),
        'w_br_gla': normal((L, GLA_W, D), GLA_W ** -0.5),
        'w_br_mla': normal((L, MLA_W, D), MLA_W ** -0.5),
        'w_out': normal((L, D, D), D ** -0.5),
        'norm2_g': gain((L, D)),
        'peer_wq': normal((L, D, PEER_HEADS * PEER_QDIM), D ** -0.5),
        'peer_k1': normal((L, PEER_HEADS, PEER_NKEYS, PEER_HALF), PEER_HALF ** -0.5),
        'peer_k2': normal((L, PEER_HEADS, PEER_NKEYS, PEER_HALF), PEER_HALF ** -0.5),
        'peer_u': normal((L, PEER_EXPERTS, D), D ** -0.5),
        'peer_v': normal((L, PEER_EXPERTS, D), 0.5),
        'final_g': gain((D,)),
    }


def reference(x, c, ctx, c_ctx, w_mod, b_mod, norm1_g, w_in, gqa_qn_g, gqa_kn_g,
              gla_wa2_f, gla_ba_f, gla_wa2_b, gla_ba_b, gla_on_g,
              mla_qn_g, mla_wuq, mla_kvn_g, mla_wukv,
              w_br_gqa, w_br_gla, w_br_mla, w_out, norm2_g,
              peer_wq, peer_k1, peer_k2, peer_u, peer_v, final_g):
    B, S, D = x.shape
    rows = S // GRID_W
    row_idx = jnp.repeat(jnp.arange(rows, dtype=jnp.int32), GRID_W)
    col_idx = jnp.tile(jnp.arange(GRID_W, dtype=jnp.int32), rows)
    rope_a = axial_rope(row_idx, col_idx, HEAD_DIM)
    rope_m = axial_rope(row_idx, col_idx, MLA_ROPE)
    for i in range(DEPTH):
        need_ctx = i < DEPTH - 1
        lp = dict(w_in=w_in[i], gqa_qn_g=gqa_qn_g[i], gqa_kn_g=gqa_kn_g[i],
                  gla_wa2_f=gla_wa2_f[i], gla_ba_f=gla_ba_f[i], gla_wa2_b=gla_wa2_b[i], gla_ba_b=gla_ba_b[i],
                  gla_on_g=gla_on_g[i], mla_qn_g=mla_qn_g[i], mla_wuq=mla_wuq[i],
                  mla_kvn_g=mla_kvn_g[i], mla_wukv=mla_wukv[i],
                  w_br_gqa=w_br_gqa[i], w_br_gla=w_br_gla[i], w_br_mla=w_br_mla[i], w_out=w_out[i])
        sh1, sc1, g1, sh2, sc2, g2 = [m[:, None, :] for m in ada_params(c, w_mod[i], b_mod[i])]
        mc = ada_params(c_ctx, w_mod[i], b_mod[i])
        h_l = rms_norm(x, norm1_g[i]) * (1.0 + sc1) + sh1
        h_c = rms_norm(ctx, norm1_g[i]) * (1.0 + mc[1]) + mc[0]
        y_l, y_c = token_mixer(h_l, h_c, lp, rope_a, rope_m, need_ctx)
        x = x + g1 * y_l
        h2 = rms_norm(x, norm2_g[i]) * (1.0 + sc2) + sh2
        x = x + g2 * peer_ffn(h2.reshape(B * S, D), peer_wq[i], peer_k1[i], peer_k2[i],
                              peer_u[i], peer_v[i]).reshape(B, S, D)
        if need_ctx:
            ctx = ctx + mc[2] * y_c
            h2c = rms_norm(ctx, norm2_g[i]) * (1.0 + mc[4]) + mc[3]
            ctx = ctx + mc[5] * peer_ffn(h2c.reshape(-1, D), peer_wq[i], peer_k1[i], peer_k2[i],
                                         peer_u[i], peer_v[i]).reshape(ctx.shape)
    return rms_norm(x, final_g)
```

```python
from contextlib import ExitStack
import numpy as np
import ml_dtypes
import concourse.bass as bass
import concourse.mybir as mybir
from concourse.bass_utils import run_bass_kernel_spmd

F32 = mybir.dt.float32
BF16 = mybir.dt.bfloat16
ALU = mybir.AluOpType
AF = mybir.ActivationFunctionType
AX = mybir.AxisListType

ENGS = ("pe", "act", "dve", "pool", "sp")
SEMQ = ENGS + ("cc",)
SEM_ROT = 12000
NSEM = {"pe": 8, "act": 10, "dve": 14, "pool": 8, "sp": 1, "cc": 1}
NSLOT = {"sp": 16, "pool": 12, "act": 4}

D = 2048
T = 2304
NCTX = 256
NLAT = 2048
KC = 16
EPS = 1e-6
import os
STOP = os.environ.get('K_STOP', '')
BLKS = [(0, 256), (256, 512), (768, 512), (1280, 512), (1792, 512)]
IN_COLS = 11872
O_AQ, O_AK, O_AV, O_LQ, O_LK, O_LV, O_LG, O_LRF, O_LRB, O_MCQ, O_MCKV, O_MKR, O_GA = (
    0, 1024, 1280, 1536, 2048, 2560, 3584, 4608, 4624, 4640, 5152, 5664, 5728)


class Dep:
    __slots__ = ("w", "r", "x")

    def __init__(self, x=False):
        self.w = None
        self.r = []
        self.x = x


class Rec:
    __slots__ = ("eng", "cnt", "is_dma", "dma_idx", "q")

    def __init__(self, eng, is_dma=False):
        self.eng = eng
        self.cnt = None
        self.is_dma = is_dma
        self.dma_idx = None


class Prog:
    def __init__(self):
        self.nc = bass.Bass("TRN2", target_bir_lowering=False)
        nc = self.nc
        self.E = {"pe": nc.tensor, "act": nc.scalar, "dve": nc.vector, "pool": nc.gpsimd, "sp": nc.sync}
        self.stack = ExitStack()
        self.sems = {e: [self.stack.enter_context(nc.semaphore(f"s_{e}_{i}")) for i in range(NSEM[e])] for e in SEMQ}
        self.slots = {q: [self.stack.enter_context(nc.semaphore(f"s_dma_{q}_{i}")) for i in range(n)] for q, n in NSLOT.items()}
        self.count = {e: 0 for e in SEMQ}
        self.last_cc = None
        self.last = {e: None for e in ENGS}
        self.known = {e: {} for e in ENGS}
        self.pe_unsig = []
        self.ndma = 0
        self.dma_recs = {q: [] for q in NSLOT}
        self.ntile = 0
        self.ninstr = {e: 0 for e in ENGS}
        self.nwait = 0

    def dram(self, name, shape, dtype, kind="Internal"):
        if kind == "Internal":
            self.ntile += 1
            name = f"{name}_i{self.ntile}"
        return self.nc.dram_tensor(name, list(shape), dtype, kind=kind).ap()

    def sbuf(self, stack, shape, dtype, name="sb"):
        self.ntile += 1
        return stack.enter_context(self.nc.sbuf_tensor(f"{name}_{self.ntile}", list(shape), dtype))

    def psum(self, stack, shape, dtype, name="ps"):
        self.ntile += 1
        return stack.enter_context(self.nc.psum_tensor(f"{name}_{self.ntile}", list(shape), dtype))

    def _sem_of(self, rec):
        if rec.is_dma:
            k = rec.dma_idx
            ns = NSLOT[rec.q]
            return ("d", rec.q, k % ns), k // ns + 1, self.slots[rec.q][k % ns], 16 * (k // ns + 1)
        if rec.cnt is None:
            raise RuntimeError("dependency on unsignaled PE instruction")
        c = rec.cnt - 1
        return ("c", rec.eng), rec.cnt, self.sems[rec.eng][c // SEM_ROT], (c % SEM_ROT) + 1

    def _wait(self, engname, rec):
        key, val, s, v = self._sem_of(rec)
        kn = self.known[engname]
        if kn.get(key, 0) >= val:
            return
        kn[key] = val
        self.E[engname].wait_ge(s, v)
        self.nwait += 1

    def _deps(self, rec, reads, writes):
        deps = []
        for d in reads:
            if d.w is not None:
                deps.append(d.w)
            if d.x:
                deps.extend(r for r in d.r if r.eng != rec.eng)
        for d in writes:
            if d.w is not None:
                deps.append(d.w)
            deps.extend(d.r)
        seen = set()
        for x in deps:
            if id(x) in seen:
                continue
            seen.add(id(x))
            if x.eng == "pe" and rec.eng == "pe" and not x.is_dma and not rec.is_dma:
                continue
            self._wait("pool" if rec.eng == "cc" else rec.eng, x)
        for d in reads:
            d.r.append(rec)
        for d in writes:
            d.w = rec
            d.r = []

    def op(self, eng, fn, reads=(), writes=(), sig=None):
        rec = Rec(eng)
        self._deps(rec, reads, writes)
        ins = fn(self.E[eng])
        self.ninstr[eng] += 1
        if sig is None:
            sig = eng != "pe"
        if sig:
            self.count[eng] += 1
            rec.cnt = self.count[eng]
            c = rec.cnt - 1
            ins.then_inc(self.sems[eng][c // SEM_ROT], 1)
            if eng == "pe":
                for r in self.pe_unsig:
                    r.cnt = rec.cnt
                self.pe_unsig = []
        else:
            self.pe_unsig.append(rec)
        self.last[eng] = rec
        return rec

    def dma(self, out, in_, reads=(), writes=(), eng="sp", **kw):
        rec = Rec(eng, is_dma=True)
        rec.q = eng
        ns = NSLOT[eng]
        rec.dma_idx = len(self.dma_recs[eng])
        self.ndma += 1
        if rec.dma_idx >= ns:
            self._wait(eng, self.dma_recs[eng][rec.dma_idx - ns])
        self._deps(rec, reads, writes)
        ins = self.E[eng].dma_start(out=out, in_=in_, **kw)
        ins.then_inc(self.slots[eng][rec.dma_idx % ns], 16)
        self.ninstr[eng] += 1
        self.dma_recs[eng].append(rec)
        return rec

    def collective(self, kind, groups, src, dst, reads=(), writes=()):
        rec = Rec("cc")
        self._deps(rec, reads, writes)
        ins = self.nc.gpsimd.collective_compute(kind, ALU.bypass, replica_groups=groups, ins=[src.opt()], outs=[dst.opt()])
        self.count["cc"] += 1
        rec.cnt = self.count["cc"]
        ins.then_inc(self.sems["cc"][0])
        self.ninstr["pool"] += 1
        self.last_cc = rec
        return rec

    def barrier(self):
        if self.pe_unsig:
            raise RuntimeError("barrier with unsignaled PE instrs")
        lasts = [self.last[e] for e in ENGS if self.last[e] is not None and e != "sp"]
        if self.last_cc is not None:
            lasts.append(self.last_cc)
        pend = [r for q in NSLOT for r in self.dma_recs[q][-NSLOT[q]:]]
        for e in ENGS:
            for x in lasts + pend:
                if (not x.is_dma) and x.eng == e and e == "pe":
                    continue
                self._wait(e, x)

    def finish(self):
        self.barrier()
        self.stack.close()
        return self.nc


class Ring:
    def __init__(self, P, stack, shape, dtype, n, name, psum=False):
        mk = P.psum if psum else P.sbuf
        self.tiles = [(mk(stack, shape, dtype, name), Dep(x=psum)) for _ in range(n)]
        self.i = 0

    def get(self):
        t = self.tiles[self.i % len(self.tiles)]
        self.i += 1
        return t


class Consts:
    pass


def load_consts(P, st, I):
    C = Consts()

    def ld(name, shape, dtype=F32, cast=False):
        t = P.sbuf(st, shape, BF16 if cast else dtype, name)
        d = Dep()
        P.dma(t[:], I[name], writes=[d], eng="pool" if cast else "sp")
        return t, d

    C.identf, C.d_identf = ld("ident", [128, 128])
    C.identb, C.d_identb = ld("ident", [128, 128], cast=True)
    C.onesb, C.d_onesb = ld("ones", [128, 128], cast=True)
    C.onesf, C.d_onesf = ld("ones", [128, 128])
    return C


def rms_fm(P, R, C, producers, gains, n, nfeat, outs, d_outs):
    ys = []
    ss, d_ss = R["aux"].get()
    k = len(producers)
    for i, prod in enumerate(producers):
        ps, d_ps = prod()
        rows = ps.shape[0]
        y, d_y = R["f32"].get()
        sq, d_sq = R["bf"].get()
        P.op("act", lambda e, y=y, ps=ps, rows=rows: e.copy(out=y[:rows, :n], in_=ps), reads=[d_ps], writes=[d_y])
        P.op("act", lambda e, sq=sq, ps=ps, rows=rows: e.activation(out=sq[:rows, :n], in_=ps, func=AF.Square),
             reads=[d_ps], writes=[d_sq])
        P.op("pe", lambda e, sq=sq, rows=rows, i=i: e.matmul(ss[:, :n], lhsT=C.onesb[:rows, :], rhs=sq[:rows, :n],
                                                            start=(i == 0), stop=(i == k - 1)),
             reads=[d_sq, C.d_onesb], writes=[d_ss], sig=(i == k - 1))
        ys.append((y, d_y, rows))
    rs, d_rs = R["f32"].get()
    P.op("act", lambda e: e.activation(out=rs[:, :n], in_=ss[:, :n], func=AF.Sqrt, scale=1.0 / nfeat, bias=EPS),
         reads=[d_ss], writes=[d_rs])
    P.op("dve", lambda e: e.reciprocal(out=rs[:, :n], in_=rs[:, :n]), reads=[d_rs], writes=[d_rs])
    for (y, d_y, rows), g, o, d_o in zip(ys, gains, outs, d_outs):
        P.op("dve", lambda e, y=y, g=g, o=o, rows=rows: e.scalar_tensor_tensor(
            out=o, in0=y[:rows, :n], scalar=g, in1=rs[:rows, :n], op0=ALU.mult, op1=ALU.mult),
            reads=[d_y, d_rs], writes=[d_o])


def gla_pass(P, st, C, R, src, direction, chunk_ids, state, d_state, state_bf, d_state_bf, emit=None, asum=None):
    tri = C.triF if direction == 0 else C.triB
    d_tri = C.d_tri
    for ci in chunk_ids:
        t0 = ci * 128
        la, d_la = R["la"].get()
        lk, d_lk = R["lk"].get()
        lv, d_lv = R["lv"].get()
        P.dma(la[:], src["la"][t0:t0 + 128, :], writes=[d_la])
        P.dma(lk[:], src["lk"][t0:t0 + 128, :], writes=[d_lk])
        P.dma(lv[:], src["lv"][t0:t0 + 128, :], writes=[d_lv])
        pb, d_pb = R["mm"].get()
        pe_, d_pe = R["mm"].get()
        P.op("pe", lambda e: e.matmul(pb[:, :512], lhsT=tri[:], rhs=la[:], start=True, stop=True),
             reads=[d_tri, d_la], writes=[d_pb], sig=True)
        P.op("pe", lambda e: e.matmul(pe_[:, :512], lhsT=C.onesf[:], rhs=la[:], start=True, stop=True),
             reads=[C.d_onesf, d_la], writes=[d_pe], sig=True)
        bsb, d_bsb = R["f32"].get()
        P.op("act", lambda e: e.copy(out=bsb[:, :512], in_=pb[:, :512]), reads=[d_pb], writes=[d_bsb])
        dif, d_dif = R["f32"].get()
        P.op("dve", lambda e: e.tensor_tensor(out=dif[:, :512], in0=pe_[:, :512], in1=bsb[:, :512], op=ALU.subtract),
             reads=[d_pe, d_bsb], writes=[d_dif])
        P.op("act", lambda e: e.activation(out=dif[:, :512], in_=dif[:, :512], func=AF.Exp), reads=[d_dif], writes=[d_dif])
        kh, d_kh = R["kh"].get()
        P.op("dve", lambda e: e.tensor_tensor(out=kh[:, :512], in0=lk[:], in1=dif[:, :512], op=ALU.mult),
             reads=[d_lk, d_dif], writes=[d_kh])
        pbe, d_pbe = R["aux"].get()
        for h in range(4):
            P.op("pe", lambda e, h=h: e.matmul(pbe[:, 4 * h:4 * h + 4], lhsT=la[:, h * 128:(h + 1) * 128], rhs=C.onesf[:, 0:4],
                                               start=True, stop=True),
                 reads=[d_la, C.d_onesf], writes=[d_pbe], sig=(h == 3))
        ebe, d_ebe = R["small"].get()
        P.op("act", lambda e: e.activation(out=ebe[:, 0:16], in_=pbe[:, 0:16], func=AF.Exp), reads=[d_pbe], writes=[d_ebe])
        if asum is not None:
            P.op("dve", lambda e: e.tensor_tensor(out=asum[0][:, 0:16], in0=asum[0][:, 0:16], in1=pbe[:, 0:16], op=ALU.add),
                 reads=[d_pbe, asum[1]], writes=[asum[1]])
        if emit is not None:
            lqT, lkT = emit["lqT"], emit["lkT"]
            for h in range(4):
                pbt, d_pbt = R["aux"].get()
                P.op("pe", lambda e, h=h: e.matmul(pbt[:, :128], lhsT=la[:, h * 128:(h + 1) * 128], rhs=tri[:],
                                                   start=True, stop=True),
                     reads=[d_la, d_tri], writes=[d_pbt], sig=True)
                eb, d_eb = R["f32"].get()
                enb, d_enb = R["f32"].get()
                P.op("act", lambda e: e.activation(out=eb[:, :128], in_=pbt[:, :128], func=AF.Exp), reads=[d_pbt], writes=[d_eb])
                P.op("act", lambda e: e.activation(out=enb[:, :128], in_=pbt[:, :128], func=AF.Exp, scale=-1.0),
                     reads=[d_pbt], writes=[d_enb])
                qt, d_qt = R["bf"].get()
                kt, d_kt = R["bf"].get()
                P.op("dve", lambda e, h=h: e.tensor_tensor(out=qt[:, :128], in0=lqT[:, h, t0:t0 + 128], in1=eb[:, :128], op=ALU.mult),
                     reads=[emit["d_lqT"], d_eb], writes=[d_qt])
                P.op("pool", lambda e, h=h: e.tensor_tensor(out=kt[:, :128], in0=lkT[:, h, t0:t0 + 128], in1=enb[:, :128], op=ALU.mult),
                     reads=[emit["d_lkT"], d_enb], writes=[d_kt])
                psc, d_psc = R["aux"].get()
                P.op("pe", lambda e: e.matmul(psc[:, :128], lhsT=kt[:, :128], rhs=qt[:, :128], start=True, stop=True),
                     reads=[d_kt, d_qt], writes=[d_psc], sig=True)
                scm, d_scm = R["bf"].get()
                P.op("dve", lambda e: e.tensor_tensor(out=scm[:, :128], in0=psc[:, :128], in1=tri[:], op=ALU.mult),
                     reads=[d_psc, d_tri], writes=[d_scm])
                po, d_po = R["mm"].get()
                for j in range(2):
                    P.op("pe", lambda e, h=h, j=j: e.matmul(po[:, j * 128:(j + 1) * 128],
                                                            lhsT=lv[:, h * 256 + j * 128:h * 256 + (j + 1) * 128],
                                                            rhs=scm[:, :128], start=True, stop=False),
                         reads=[d_lv, d_scm], writes=[d_po], sig=False)
                    P.op("pe", lambda e, h=h, j=j: e.matmul(po[:, j * 128:(j + 1) * 128],
                                                            lhsT=state_bf[h][:, j * 128:(j + 1) * 128],
                                                            rhs=qt[:, :128], start=False, stop=True),
                         reads=[d_state_bf[h], d_qt], writes=[d_po], sig=(j == 1))
                oT, d_oT = emit["oT"], emit["d_oT"][ci]
                if emit["first"]:
                    P.op("act", lambda e, h=h: e.copy(out=oT[:, 2 * h:2 * h + 2, t0:t0 + 128],
                                                     in_=po[:, 0:256].rearrange("p (j t) -> p j t", j=2)),
                         reads=[d_po], writes=[d_oT])
                else:
                    P.op("dve", lambda e, h=h: e.tensor_tensor(out=oT[:, 2 * h:2 * h + 2, t0:t0 + 128],
                                                               in0=oT[:, 2 * h:2 * h + 2, t0:t0 + 128],
                                                               in1=po[:, 0:256].rearrange("p (j t) -> p j t", j=2), op=ALU.add),
                         reads=[d_po, d_oT], writes=[d_oT])
        for h in range(4):
            pu, d_pu = R["aux"].get()
            P.op("pe", lambda e, h=h: e.matmul(pu[:, :256], lhsT=kh[:, h * 128:(h + 1) * 128], rhs=lv[:, h * 256:(h + 1) * 256],
                                               start=True, stop=True),
                 reads=[d_kh, d_lv], writes=[d_pu], sig=True)
            P.op("dve", lambda e, h=h: e.scalar_tensor_tensor(out=state[h][:], in0=state[h][:], scalar=ebe[:, 4 * h:4 * h + 1],
                                                              in1=pu[:, :256], op0=ALU.mult, op1=ALU.add),
                 reads=[d_pu, d_ebe, d_state[h]] + ([d_state_bf[h]] if state_bf is not None else []), writes=[d_state[h]])
            if state_bf is not None:
                P.op("act", lambda e, h=h: e.copy(out=state_bf[h][:], in_=state[h][:]), reads=[d_state[h]], writes=[d_state_bf[h]])


def mk_rings(P, st, n_mm=3, n_aux=2):
    R = {}
    R["mm"] = Ring(P, st, [128, 512], F32, n_mm, "psmm", psum=True)
    R["aux"] = Ring(P, st, [128, 512], F32, n_aux, "psaux", psum=True)
    R["f32"] = Ring(P, st, [128, 512], F32, 12, "rf32")
    R["bf"] = Ring(P, st, [128, 512], BF16, 8, "rbf")
    R["small"] = Ring(P, st, [128, 16], F32, 8, "rsm")
    R["kh"] = Ring(P, st, [128, 512], BF16, 2, "rkh")
    return R


S1_INPUTS = {
    "xin": ([T, D], F32), "cT": ([128, 16, 2], F32), "w_mod": ([D, 6 * D], F32), "bmodT": ([128, 96], F32),
    "bmod_g": ([2, 2, D], F32), "n1g": ([128, 16], F32), "w_in": ([D, IN_COLS], F32),
    "gq": ([128, 1], F32), "gk": ([128, 1], F32), "waf": ([17, 512], F32), "wab": ([17, 512], F32),
    "gmq": ([128, 4], F32), "gmkv": ([128, 4], F32), "wuq": ([512, 1536], F32), "wukv": ([512, 2048], F32),
    "cosA": ([128, NLAT], F32), "sinA": ([128, NLAT], F32), "cosM": ([64, NLAT], F32), "sinM": ([64, NLAT], F32),
    "ident": ([128, 128], F32), "ones": ([128, 128], F32), "rotA": ([128, 128], F32), "rotM": ([64, 64], F32),
    "triF": ([128, 128], F32), "triB": ([128, 128], F32),
}
S1_OUTPUTS = {
    "mfm": ([128, 96, 2], F32), "grow": ([2, 2, D], F32),
    "qTa": ([8, 128, T], BF16), "kTa": ([2, 128, T], BF16), "va": ([T, 256], BF16),
    "lqT": ([4, 128, T], BF16), "lkT": ([4, 128, T], BF16), "lk": ([T, 512], BF16), "lv": ([T, 1024], BF16),
    "lgT": ([1024, T], BF16), "laf": ([T, 512], F32), "lab": ([T, 512], F32),
    "mqT": ([8, 192, T], BF16), "mkT": ([8, 128, T], BF16), "mkrT": ([64, T], BF16), "vm": ([T, 1024], BF16),
    "gT": ([3, D, T], BF16),
    "glaA": ([2, 128, 16], F32), "glaU": ([2, 4, 128, 256], F32), "glaC": ([2, 4, 128, 256], F32),
}


def stage1(P, I, O):
    with ExitStack() as st:
        C = load_consts(P, st, I)
        C.triF = P.sbuf(st, [128, 128], F32, "triF")
        C.triB = P.sbuf(st, [128, 128], F32, "triB")
        C.d_tri = Dep()
        P.dma(C.triF[:], I["triF"], writes=[C.d_tri])
        P.dma(C.triB[:], I["triB"], writes=[C.d_tri])
        R = mk_rings(P, st)
        R["w"] = Ring(P, st, [128, 16, 512], BF16, 2, "wt")
        hT = P.sbuf(st, [128, 16, T], BF16, "hT")
        d_hT = [(Dep(), Dep()) for _ in range(18)]
        mfm = P.sbuf(st, [128, 96, 2], F32, "mfm")
        d_mfm = Dep()
        gm1 = P.sbuf(st, [128, 16, 2], F32, "gm1")
        d_gm1 = Dep()

        with ExitStack() as sa:
            cT = P.sbuf(sa, [128, 16, 2], F32, "cT")
            d_cT = Dep()
            scT = P.sbuf(sa, [128, 16, 2], BF16, "scT")
            d_scT = Dep()
            bmodT = P.sbuf(sa, [128, 96], F32, "bmodT")
            d_bm = Dep()
            bmg = P.sbuf(sa, [2, 2, D], F32, "bmg")
            d_bmg = Dep()
            grow = P.sbuf(sa, [2, 2, D], F32, "grow")
            d_grow = Dep()
            n1g = P.sbuf(sa, [128, 16], F32, "n1g")
            d_n1g = Dep()
            P.dma(cT[:], I["cT"], writes=[d_cT])
            P.dma(bmodT[:], I["bmodT"], writes=[d_bm])
            P.dma(bmg[:], I["bmod_g"], writes=[d_bmg])
            P.dma(n1g[:], I["n1g"], writes=[d_n1g])
            P.op("act", lambda e: e.activation(out=scT[:], in_=cT[:], func=AF.Silu), reads=[d_cT], writes=[d_scT])
            wmv = I["w_mod"].rearrange("(kc p) c -> p kc c", p=128)
            for g in range(24):
                wt, d_wt = R["w"].get()
                P.dma(wt[:], wmv[:, :, g * 512:(g + 1) * 512], writes=[d_wt], eng="pool")
                for j in range(4):
                    cc = g * 4 + j
                    ps, d_ps = R["aux"].get()
                    for kc in range(16):
                        P.op("pe", lambda e, kc=kc, j=j, wt=wt, ps=ps: e.matmul(
                            ps[:, 0:2], lhsT=wt[:, kc, j * 128:(j + 1) * 128], rhs=scT[:, kc, :],
                            start=(kc == 0), stop=(kc == 15)), reads=[d_wt, d_scT], writes=[d_ps], sig=(kc == 15))
                    P.op("dve", lambda e, cc=cc, ps=ps: e.tensor_scalar(out=mfm[:, cc, :], in0=ps[:, 0:2], scalar1=bmodT[:, cc:cc + 1],
                                                                       scalar2=None, op0=ALU.add),
                         reads=[d_ps, d_bm], writes=[d_mfm])
                gi = {2: 0, 5: 1}.get(g // 4)
                if gi is not None:
                    off = (g % 4) * 512
                    psr, d_psr = R["mm"].get()
                    for kc in range(16):
                        P.op("pe", lambda e, kc=kc, wt=wt, psr=psr: e.matmul(
                            psr[0:2, :], lhsT=scT[:, kc, :], rhs=wt[:, kc, :], start=(kc == 0), stop=(kc == 15)),
                            reads=[d_wt, d_scT], writes=[d_psr], sig=(kc == 15))
                    P.op("dve", lambda e, gi=gi, off=off, psr=psr: e.tensor_tensor(
                        out=grow[0:2, gi, off:off + 512], in0=psr[0:2, :], in1=bmg[0:2, gi, off:off + 512], op=ALU.add),
                        reads=[d_psr, d_bmg], writes=[d_grow])
            P.dma(O["mfm"], mfm[:], reads=[d_mfm])
            P.dma(O["grow"], grow[:], reads=[d_grow])
            P.op("dve", lambda e: e.tensor_scalar(out=gm1[:], in0=mfm[:, 16:32, :], scalar1=1.0, scalar2=None, op0=ALU.add),
                 reads=[d_mfm], writes=[d_gm1])
            P.op("dve", lambda e: e.tensor_tensor(out=gm1[:], in0=gm1[:], in1=n1g[:].unsqueeze(2).to_broadcast([128, 16, 2]),
                                                  op=ALU.mult), reads=[d_gm1, d_n1g], writes=[d_gm1])
            P.barrier()

        if STOP == "a":
            P.barrier()
            return
        norm_to_fm(P, st, C, I["xin"], hT, d_hT, gm1, d_gm1, mfm, d_mfm, 0)
        if STOP == "b":
            for kc in range(8):
                P.dma(O["lgT"][kc * 128:(kc + 1) * 128, :], hT[:, kc, :], reads=[x for p in d_hT for x in p])
            P.barrier()
            return

        w_in_v = I["w_in"].rearrange("(kc p) c -> p kc c", p=128)

        def hdeps(t0, n):
            out = []
            for ti in range(t0 // 128, (t0 + n) // 128):
                out.extend(d_hT[ti])
            return out

        def load_w(off, ncols):
            wt, d_wt = R["w"].get()
            P.dma(wt[:, :, :ncols], w_in_v[:, :, off:off + ncols], writes=[d_wt], eng="pool")
            return wt, d_wt

        def fm_chunk(wt, d_wt, c0, rows, blk):
            t0, n = blk
            ps, d_ps = R["mm"].get()
            for kc in range(16):
                P.op("pe", lambda e, kc=kc: e.matmul(ps[:rows, :n], lhsT=wt[:, kc, c0:c0 + rows], rhs=hT[:, kc, t0:t0 + n],
                                                     start=(kc == 0), stop=(kc == 15)),
                     reads=[d_wt] + hdeps(t0, n), writes=[d_ps], sig=(kc == 15))
            return ps[:rows, :n], d_ps

        def tm_tile(wt, d_wt, c0, ncols, ti):
            ps, d_ps = R["mm"].get()
            for kc in range(16):
                P.op("pe", lambda e, kc=kc: e.matmul(ps[:, :ncols], lhsT=hT[:, kc, ti * 128:(ti + 1) * 128],
                                                     rhs=wt[:, kc, c0:c0 + ncols], start=(kc == 0), stop=(kc == 15)),
                     reads=[d_wt] + list(d_hT[ti]), writes=[d_ps], sig=(kc == 15))
            return ps[:, :ncols], d_ps

        def store(dst, src, dep):
            P.dma(dst, src, reads=[dep])

        def evac_bf(ps, d_ps, rows, n, func=AF.Copy, scale=1.0, eng="act"):
            ob, d_ob = R["bf"].get()
            if eng == "act":
                P.op("act", lambda e: e.activation(out=ob[:rows, :n], in_=ps, func=func, scale=scale), reads=[d_ps], writes=[d_ob])
            else:
                P.op("dve", lambda e: e.tensor_copy(out=ob[:rows, :n], in_=ps), reads=[d_ps], writes=[d_ob])
            return ob, d_ob

        def simple_fm(off, ncols, dst_fn, func=AF.Copy, scale=1.0):
            wt, d_wt = load_w(off, ncols)
            for blk in BLKS:
                for c in range(ncols // 128):
                    ps, d_ps = fm_chunk(wt, d_wt, c * 128, 128, blk)
                    ob, d_ob = evac_bf(ps, d_ps, 128, blk[1], func, scale)
                    store(dst_fn(c, blk), ob[:, :blk[1]], d_ob)

        def simple_tm(off, ncols, dst, dcol, wt_pair=None):
            wt, d_wt = wt_pair or load_w(off, ncols)
            for ti in range(18):
                ps, d_ps = tm_tile(wt, d_wt, 0, ncols, ti)
                ob, d_ob = evac_bf(ps, d_ps, 128, ncols, eng="dve")
                store(dst[ti * 128:(ti + 1) * 128, dcol:dcol + ncols], ob[:, :ncols], d_ob)
            return wt, d_wt

        with ExitStack() as sc:
            def ldc(name, shape, cast=True):
                t = P.sbuf(sc, shape, BF16 if cast else F32, name)
                d = Dep()
                P.dma(t[:], I[name], writes=[d], eng="pool" if cast else "sp")
                return t, d
            cosA, d_cosA = ldc("cosA", [128, NLAT])
            sinA, d_sinA = ldc("sinA", [128, NLAT])
            cosM, d_cosM = ldc("cosM", [64, NLAT])
            sinM, d_sinM = ldc("sinM", [64, NLAT])
            rotA, d_rotA = ldc("rotA", [128, 128])
            rotM, d_rotM = ldc("rotM", [64, 64])
            gq, d_gq = ldc("gq", [128, 1], cast=False)
            gk, d_gk = ldc("gk", [128, 1], cast=False)
            gmq, d_gmq = ldc("gmq", [128, 4], cast=False)
            gmkv, d_gmkv = ldc("gmkv", [128, 4], cast=False)
            P.op("dve", lambda e: e.tensor_scalar(out=gq[:], in0=gq[:], scalar1=128 ** -0.5, scalar2=None, op0=ALU.mult),
                 reads=[d_gq], writes=[d_gq])

            def rope(o, d_o, rows, blk, rot, d_rot, cos, d_cos, sin, d_sin):
                t0, n = blk
                ob, d_ob = R["bf"].get()
                if t0 < NCTX:
                    P.op("act", lambda e: e.copy(out=ob[:rows, :n], in_=o[:rows, :n]), reads=[d_o], writes=[d_ob])
                    return ob, d_ob
                l0 = t0 - NCTX
                P.op("act", lambda e: e.copy(out=ob[:rows, :n], in_=o[:rows, :n]), reads=[d_o], writes=[d_ob])
                pr, d_pr = R["aux"].get()
                P.op("pe", lambda e: e.matmul(pr[:rows, :n], lhsT=rot[:rows, :rows], rhs=ob[:rows, :n], start=True, stop=True),
                     reads=[d_rot, d_ob], writes=[d_pr], sig=True)
                t2, d_t2 = R["f32"].get()
                P.op("dve", lambda e: e.tensor_tensor(out=t2[:rows, :n], in0=pr[:rows, :n], in1=sin[:rows, l0:l0 + n], op=ALU.mult),
                     reads=[d_pr, d_sin], writes=[d_t2])
                P.op("pool", lambda e: e.tensor_tensor(out=o[:rows, :n], in0=o[:rows, :n], in1=cos[:rows, l0:l0 + n], op=ALU.mult),
                     reads=[d_o, d_cos], writes=[d_o])
                ob2, d_ob2 = R["bf"].get()
                P.op("pool", lambda e: e.tensor_tensor(out=ob2[:rows, :n], in0=o[:rows, :n], in1=t2[:rows, :n], op=ALU.add),
                     reads=[d_o, d_t2], writes=[d_ob2])
                return ob2, d_ob2

            def gqa_heads(off, nheads, gain, d_gain, dst):
                for g0 in range(0, nheads, 4):
                    nh = min(4, nheads - g0)
                    wt, d_wt = load_w(off + g0 * 128, nh * 128)
                    for blk in BLKS:
                        for hh in range(nh):
                            o, d_o = R["f32"].get()
                            rms_fm(P, R, C, [lambda hh=hh: fm_chunk(wt, d_wt, hh * 128, 128, blk)], [gain[:, 0:1]], blk[1], 128,
                                   [o[:, :blk[1]]], [d_o])
                            ob, d_ob = rope(o, d_o, 128, blk, rotA, d_rotA, cosA, d_cosA, sinA, d_sinA)
                            store(dst[g0 + hh, :, blk[0]:blk[0] + blk[1]], ob[:, :blk[1]], d_ob)
            gqa_heads(O_AQ, 8, gq, d_gq, O["qTa"])
            wt, d_wt = load_w(O_AK, 512)
            for blk in BLKS:
                for hh in range(2):
                    o, d_o = R["f32"].get()
                    rms_fm(P, R, C, [lambda hh=hh: fm_chunk(wt, d_wt, hh * 128, 128, blk)], [gk[:, 0:1]], blk[1], 128,
                           [o[:, :blk[1]]], [d_o])
                    ob, d_ob = rope(o, d_o, 128, blk, rotA, d_rotA, cosA, d_cosA, sinA, d_sinA)
                    store(O["kTa"][hh, :, blk[0]:blk[0] + blk[1]], ob[:, :blk[1]], d_ob)
            for ti in range(18):
                ps, d_ps = tm_tile(wt, d_wt, 256, 256, ti)
                ob, d_ob = evac_bf(ps, d_ps, 128, 256, eng="dve")
                store(O["va"][ti * 128:(ti + 1) * 128, :], ob[:, :256], d_ob)
            if STOP == "c1":
                P.barrier()
                return
            simple_fm(O_LQ, 512, lambda c, blk: O["lqT"][c, :, blk[0]:blk[0] + blk[1]], scale=128 ** -0.5)
            wt, d_wt = load_w(O_LK, 512)
            for blk in BLKS:
                for c in range(4):
                    ps, d_ps = fm_chunk(wt, d_wt, c * 128, 128, blk)
                    ob, d_ob = evac_bf(ps, d_ps, 128, blk[1])
                    store(O["lkT"][c, :, blk[0]:blk[0] + blk[1]], ob[:, :blk[1]], d_ob)
            simple_tm(O_LK, 512, O["lk"], 0, wt_pair=(wt, d_wt))
            simple_tm(O_LV, 512, O["lv"], 0)
            simple_tm(O_LV + 512, 512, O["lv"], 512)
            simple_fm(O_LG, 512, lambda c, blk: O["lgT"][c * 128:(c + 1) * 128, blk[0]:blk[0] + blk[1]], func=AF.Silu)
            simple_fm(O_LG + 512, 512, lambda c, blk: O["lgT"][512 + c * 128:512 + (c + 1) * 128, blk[0]:blk[0] + blk[1]], func=AF.Silu)
            for g in range(12):
                simple_fm(O_GA + g * 512, 512,
                          lambda c, blk, g=g: O["gT"][g // 4, (g % 4) * 512 + c * 128:(g % 4) * 512 + (c + 1) * 128, blk[0]:blk[0] + blk[1]],
                          func=AF.Sigmoid)
            if STOP == "c2":
                P.barrier()
                return
            with ExitStack() as sl:
                lrA = [P.sbuf(sl, [32, T], F32, f"lrA{d}") for d in range(2)]
                d_lrA = [Dep(), Dep()]
                wa = [P.sbuf(sl, [17, 512], F32, f"wa{d}") for d in range(2)]
                d_wa = [Dep(), Dep()]
                for d in range(2):
                    P.op("pool", lambda e, d=d: e.memset(lrA[d][:], 1.0), writes=[d_lrA[d]])
                    P.dma(wa[d][:], I["waf" if d == 0 else "wab"], writes=[d_wa[d]])
                wt, d_wt = load_w(O_LRF, 32)
                for blk in BLKS:
                    for d in range(2):
                        ps, d_ps = fm_chunk(wt, d_wt, d * 16, 16, blk)
                        P.op("act", lambda e, d=d, ps=ps: e.copy(out=lrA[d][0:16, blk[0]:blk[0] + blk[1]], in_=ps),
                             reads=[d_ps], writes=[d_lrA[d]])
                for d in range(2):
                    for ti in range(18):
                        ps, d_ps = R["mm"].get()
                        P.op("pe", lambda e, d=d, ti=ti, ps=ps: e.matmul(ps[:, :512], lhsT=lrA[d][0:17, ti * 128:(ti + 1) * 128],
                                                                         rhs=wa[d][:], start=True, stop=True),
                             reads=[d_lrA[d], d_wa[d]], writes=[d_ps], sig=True)
                        ex, d_ex = R["f32"].get()
                        P.op("act", lambda e, ps=ps, ex=ex: e.activation(out=ex[:, :512], in_=ps[:, :512], func=AF.Exp, scale=-1.0),
                             reads=[d_ps], writes=[d_ex])
                        P.op("act", lambda e, ex=ex: e.activation(out=ex[:, :512], in_=ex[:, :512], func=AF.Ln, bias=1.0),
                             reads=[d_ex], writes=[d_ex])
                        P.op("pool", lambda e, ex=ex: e.tensor_scalar(out=ex[:, :512], in0=ex[:, :512], scalar1=-1.0 / 16.0, scalar2=None,
                                                                      op0=ALU.mult), reads=[d_ex], writes=[d_ex])
                        store(O["laf" if d == 0 else "lab"][ti * 128:(ti + 1) * 128, :], ex[:, :512], d_ex)
                P.barrier()
            if STOP == "c3":
                P.barrier()
                return
            with ExitStack() as sm:
                wuq = P.sbuf(sm, [128, 4, 1536], BF16, "wuq")
                wukv = P.sbuf(sm, [128, 4, 2048], BF16, "wukv")
                d_wuq, d_wukv = Dep(), Dep()
                P.dma(wuq[:], I["wuq"].rearrange("(kc p) c -> p kc c", p=128), writes=[d_wuq], eng="pool")
                P.dma(wukv[:], I["wukv"].rearrange("(kc p) c -> p kc c", p=128), writes=[d_wukv], eng="pool")
                RC = Ring(P, sm, [128, 4, 512], BF16, 2, "cn")
                wq_t, d_wq_t = load_w(O_MCQ, 512)
                wkv_t, d_wkv_t = load_w(O_MCKV, 512)
                for blk in BLKS:
                    t0, n = blk
                    cqn, d_cqn = RC.get()
                    rms_fm(P, R, C, [lambda c=c: fm_chunk(wq_t, d_wq_t, c * 128, 128, blk) for c in range(4)],
                           [gmq[:, c:c + 1] for c in range(4)], n, 512, [cqn[:, c, :n] for c in range(4)], [d_cqn] * 4)
                    for h in range(8):
                        ps, d_ps = R["mm"].get()
                        for kc in range(4):
                            P.op("pe", lambda e, kc=kc, h=h, ps=ps: e.matmul(ps[:, :n], lhsT=wuq[:, kc, h * 192:h * 192 + 128],
                                                                             rhs=cqn[:, kc, :n], start=(kc == 0), stop=(kc == 3)),
                                 reads=[d_wuq, d_cqn], writes=[d_ps], sig=(kc == 3))
                        ob, d_ob = evac_bf(ps[:, :n], d_ps, 128, n, scale=192 ** -0.5)
                        store(O["mqT"][h, 0:128, t0:t0 + n], ob[:, :n], d_ob)
                        ps2, d_ps2 = R["mm"].get()
                        for kc in range(4):
                            P.op("pe", lambda e, kc=kc, h=h, ps2=ps2: e.matmul(ps2[:64, :n], lhsT=wuq[:, kc, h * 192 + 128:h * 192 + 192],
                                                                               rhs=cqn[:, kc, :n], start=(kc == 0), stop=(kc == 3)),
                                 reads=[d_wuq, d_cqn], writes=[d_ps2], sig=(kc == 3))
                        o, d_o = R["f32"].get()
                        P.op("act", lambda e, o=o, ps2=ps2: e.activation(out=o[:64, :n], in_=ps2[:64, :n], func=AF.Copy, scale=192 ** -0.5),
                             reads=[d_ps2], writes=[d_o])
                        ob, d_ob = rope(o, d_o, 64, blk, rotM, d_rotM, cosM, d_cosM, sinM, d_sinM)
                        store(O["mqT"][h, 128:192, t0:t0 + n], ob[:64, :n], d_ob)
                    ckn, d_ckn = RC.get()
                    rms_fm(P, R, C, [lambda c=c: fm_chunk(wkv_t, d_wkv_t, c * 128, 128, blk) for c in range(4)],
                           [gmkv[:, c:c + 1] for c in range(4)], n, 512, [ckn[:, c, :n] for c in range(4)], [d_ckn] * 4)
                    for h in range(8):
                        ps, d_ps = R["mm"].get()
                        for kc in range(4):
                            P.op("pe", lambda e, kc=kc, h=h, ps=ps: e.matmul(ps[:, :n], lhsT=wukv[:, kc, h * 256:h * 256 + 128],
                                                                             rhs=ckn[:, kc, :n], start=(kc == 0), stop=(kc == 3)),
                                 reads=[d_wukv, d_ckn], writes=[d_ps], sig=(kc == 3))
                        ob, d_ob = evac_bf(ps[:, :n], d_ps, 128, n)
                        store(O["mkT"][h, :, t0:t0 + n], ob[:, :n], d_ob)
                    wv = wukv[:].rearrange("p k (h c) -> p k h c", c=256)
                    for tt in range(n // 128):
                        for half in range(2):
                            ps, d_ps = R["mm"].get()
                            for kc in range(4):
                                P.op("pe", lambda e, kc=kc, half=half, tt=tt, ps=ps: e.matmul(
                                    ps[:, :512].rearrange("p (h c) -> p h c", c=128), lhsT=ckn[:, kc, tt * 128:(tt + 1) * 128],
                                    rhs=wv[:, kc, half * 4:(half + 1) * 4, 128:256], start=(kc == 0), stop=(kc == 3)),
                                    reads=[d_wukv, d_ckn], writes=[d_ps], sig=(kc == 3))
                            ob, d_ob = evac_bf(ps[:, :512], d_ps, 128, 512, eng="dve")
                            store(O["vm"][t0 + tt * 128:t0 + (tt + 1) * 128, half * 512:(half + 1) * 512], ob[:, :512], d_ob)
                wt, d_wt = load_w(O_MKR, 64)
                for blk in BLKS:
                    ps, d_ps = fm_chunk(wt, d_wt, 0, 64, blk)
                    o, d_o = R["f32"].get()
                    P.op("act", lambda e, o=o, ps=ps: e.copy(out=o[:64, :blk[1]], in_=ps), reads=[d_ps], writes=[d_o])
                    ob, d_ob = rope(o, d_o, 64, blk, rotM, d_rotM, cosM, d_cosM, sinM, d_sinM)
                    store(O["mkrT"][:, blk[0]:blk[0] + blk[1]], ob[:64, :blk[1]], d_ob)
                P.barrier()
            P.barrier()
        P.barrier()

    if STOP == "c4":
        return
    with ExitStack() as st:
        C = load_consts(P, st, I)
        C.triF = P.sbuf(st, [128, 128], F32, "triF")
        C.triB = P.sbuf(st, [128, 128], F32, "triB")
        C.d_tri = Dep()
        P.dma(C.triF[:], I["triF"], writes=[C.d_tri])
        P.dma(C.triB[:], I["triB"], writes=[C.d_tri])
        R = mk_rings(P, st)
        R["la"] = Ring(P, st, [128, 512], F32, 2, "la")
        R["lk"] = Ring(P, st, [128, 512], BF16, 2, "lk")
        R["lv"] = Ring(P, st, [128, 1024], BF16, 2, "lv")
        state = [P.sbuf(st, [128, 256], F32, f"st{h}") for h in range(4)]
        d_state = [Dep() for _ in range(4)]
        asum = (P.sbuf(st, [128, 16], F32, "asum"), Dep())
        for d in range(2):
            src = {"la": O["laf" if d == 0 else "lab"], "lk": O["lk"], "lv": O["lv"]}
            for which in ("ctx", "lat"):
                for h in range(4):
                    P.op("pool", lambda e, h=h: e.memset(state[h][:], 0.0), writes=[d_state[h]])
                P.op("pool", lambda e: e.memset(asum[0][:], 0.0), writes=[asum[1]])
                ids = [0, 1] if which == "ctx" else list(range(2, 18))
                if d == 1:
                    ids = ids[::-1]
                gla_pass(P, st, C, R, src, d, ids, state, d_state, None, None, emit=None, asum=asum)
                dst = O["glaC"] if which == "ctx" else O["glaU"]
                for h in range(4):
                    P.dma(dst[d, h], state[h][:], reads=[d_state[h]])
                if which == "lat":
                    ea, d_ea = R["small"].get()
                    P.op("act", lambda e, ea=ea: e.activation(out=ea[:, 0:16], in_=asum[0][:, 0:16], func=AF.Exp),
                         reads=[asum[1]], writes=[d_ea])
                    P.dma(O["glaA"][d], ea[:, 0:16], reads=[d_ea])
        P.barrier()


def norm_to_fm(P, st, C, xsrc, hT, d_hT, gm, d_gm, mfm, d_mfm, shift_group):
    with ExitStack() as sb:
        RX = Ring(P, sb, [128, D], F32, 2, "x")
        RXN = Ring(P, sb, [128, D], BF16, 2, "xn")
        RT = Ring(P, sb, [128, 8, 128], BF16, 2, "tp", psum=True)
        RS = Ring(P, sb, [128, 8], F32, 4, "xs")
        for ti in range(18):
            r = 1 if ti < 2 else 0
            xt, d_xt = RX.get()
            P.dma(xt[:], xsrc[ti * 128:(ti + 1) * 128, :], writes=[d_xt])
            xn, d_xn = RXN.get()
            ssq, d_ssq = RS.get()
            P.op("act", lambda e: e.activation(out=xn[:], in_=xt[:], func=AF.Square, accum_out=ssq[:, 0:1]),
                 reads=[d_xt], writes=[d_xn, d_ssq])
            P.op("act", lambda e: e.activation(out=ssq[:, 1:2], in_=ssq[:, 0:1], func=AF.Sqrt, scale=1.0 / D, bias=EPS),
                 reads=[d_ssq], writes=[d_ssq])
            P.op("dve", lambda e: e.reciprocal(out=ssq[:, 2:3], in_=ssq[:, 1:2]), reads=[d_ssq], writes=[d_ssq])
            P.op("act", lambda e: e.activation(out=xn[:], in_=xt[:], func=AF.Copy, scale=ssq[:, 2:3]),
                 reads=[d_xt, d_ssq, d_xn], writes=[d_xn])
            tps = [RT.get(), RT.get()]
            for kc in range(16):
                tp, d_tp = tps[kc // 8]
                P.op("pe", lambda e, kc=kc: e.transpose(out=tp[:, kc % 8, :], in_=xn[:, kc * 128:(kc + 1) * 128], identity=C.identb[:]),
                     reads=[d_xn, C.d_identb], writes=[d_tp], sig=(kc % 8 == 7))
            for kc in range(16):
                tp, d_tp = tps[kc // 8]
                sl = hT[:, kc, ti * 128:(ti + 1) * 128]
                if kc < 8:
                    P.op("dve", lambda e, kc=kc, sl=sl: e.tensor_scalar(out=sl, in0=tp[:, kc % 8, :], scalar1=gm[:, kc, r:r + 1],
                                                                       scalar2=mfm[:, shift_group * 16 + kc, r:r + 1],
                                                                       op0=ALU.mult, op1=ALU.add),
                         reads=[d_tp, d_gm, d_mfm], writes=[d_hT[ti][0]])
                else:
                    P.op("act", lambda e, kc=kc, sl=sl: e.activation(out=sl, in_=tp[:, kc % 8, :], func=AF.Identity,
                                                                    scale=gm[:, kc, r:r + 1], bias=mfm[:, shift_group * 16 + kc, r:r + 1]),
                         reads=[d_tp, d_gm, d_mfm], writes=[d_hT[ti][1]])
        P.barrier()


def _fm16(v):
    return np.ascontiguousarray(np.asarray(v, np.float32).reshape(16, 128).T)


def _rope_tables(dim, pos0):
    quarter = dim // 4
    inv = (10000.0 ** (-np.arange(quarter, dtype=np.float32) / quarter)).astype(np.float32)
    t = np.arange(pos0, pos0 + NLAT)
    row = (t // 64).astype(np.float32)
    col = (t % 64).astype(np.float32)
    ang = np.concatenate([row[:, None] * inv, col[:, None] * inv], axis=-1)
    cos = np.cos(ang).astype(np.float32).T
    sin = np.sin(ang).astype(np.float32).T
    return (np.ascontiguousarray(np.concatenate([cos, cos], 0)), np.ascontiguousarray(np.concatenate([sin, sin], 0)))


def _rot_mat(dim):
    half = dim // 2
    L = np.zeros((dim, dim), np.float32)
    for m in range(half):
        L[m + half, m] = -1.0
        L[m, m + half] = 1.0
    return L


def consts_np():
    tri = np.triu(np.ones((128, 128), np.float32))
    return {"ident": np.eye(128, dtype=np.float32), "ones": np.ones((128, 128), np.float32),
            "rotA": _rot_mat(128), "rotM": _rot_mat(64), "triF": tri, "triB": np.ascontiguousarray(tri.T)}


def s1_inputs(core, layer, xs, ctxs, inp):
    b, q = core // 4, core % 4
    L = layer
    m = {}
    m["xin"] = np.ascontiguousarray(np.concatenate([ctxs[b], xs[b][q * NLAT:(q + 1) * NLAT]], 0))
    cc = np.stack([inp["c"][b], inp["c_ctx"]], -1)
    m["cT"] = np.ascontiguousarray(cc.reshape(16, 128, 2).transpose(1, 0, 2))
    m["w_mod"] = inp["w_mod"][L]
    bm = inp["b_mod"][L]
    m["bmodT"] = np.ascontiguousarray(bm.reshape(96, 128).T)
    g = np.stack([bm[2 * D:3 * D], bm[5 * D:6 * D]], 0)
    m["bmod_g"] = np.ascontiguousarray(np.stack([g, g], 0))
    m["n1g"] = _fm16(inp["norm1_g"][L])
    m["w_in"] = inp["w_in"][L]
    m["gq"] = np.ascontiguousarray(inp["gqa_qn_g"][L].reshape(128, 1))
    m["gk"] = np.ascontiguousarray(inp["gqa_kn_g"][L].reshape(128, 1))
    m["waf"] = np.ascontiguousarray(np.concatenate([inp["gla_wa2_f"][L], inp["gla_ba_f"][L][None]], 0))
    m["wab"] = np.ascontiguousarray(np.concatenate([inp["gla_wa2_b"][L], inp["gla_ba_b"][L][None]], 0))
    m["gmq"] = np.ascontiguousarray(inp["mla_qn_g"][L].reshape(4, 128).T)
    m["gmkv"] = np.ascontiguousarray(inp["mla_kvn_g"][L].reshape(4, 128).T)
    m["wuq"] = inp["mla_wuq"][L]
    m["wukv"] = inp["mla_wukv"][L]
    m["cosA"], m["sinA"] = _rope_tables(128, q * NLAT)
    m["cosM"], m["sinM"] = _rope_tables(64, q * NLAT)
    m.update(consts_np())
    return m


def build_s1():
    P = Prog()
    I = {k: P.dram(k, sh, dt, kind="ExternalInput") for k, (sh, dt) in S1_INPUTS.items()}
    O = {k: P.dram(k, sh, dt, kind="ExternalOutput") for k, (sh, dt) in S1_OUTPUTS.items()}
    stage1(P, I, O)
    return P.finish(), P


NK = NCTX + 4 * NLAT
NKT = NK // 128
NEXP = 16384

S2_INPUTS = {
    "xin": ([T, D], F32), "mfm": ([128, 96, 2], F32), "grow": ([2, 2, D], F32),
    "qTa": ([8, 128, T], BF16), "mqT": ([8, 192, T], BF16),
    "lqT": ([4, 128, T], BF16), "lkT": ([4, 128, T], BF16), "lk": ([T, 512], BF16), "lv": ([T, 1024], BF16),
    "lgT": ([1024, T], BF16), "laf": ([T, 512], F32), "lab": ([T, 512], F32), "gT": ([3, D, T], BF16),
    "kTa_all": ([2, 128, NK], BF16), "va_all": ([NK, 256], BF16),
    "mkT_all": ([8, 128, NK], BF16), "mkrT_all": ([64, NK], BF16), "vm_all": ([NK, 1024], BF16),
    "glaC": ([2, 4, 128, 256], F32), "glaA3": ([2, 3, 128, 4], F32), "glaU3": ([2, 3, 4, 128, 256], F32),
    "w_br": ([3, 1024, D], F32), "w_out": ([D, D], F32), "n2g": ([128, 16], F32), "gon": ([128, 2], F32),
    "peer_wq": ([D, D], F32), "k12T": ([16, 128, 128], F32), "peer_uT": ([D, NEXP], F32), "peer_v": ([NEXP, D], F32),
    "fing": ([1, D], F32), "sel": ([2, 2, 128], F32),
    "ident": ([128, 128], F32), "ones": ([128, 128], F32), "triF": ([128, 128], F32), "triB": ([128, 128], F32),
}
S2_OUTPUTS = {"xout": ([T, D], F32)}

QBLKS = [(0, 256, 2)] + [(256 + i * 512, 512, NKT) for i in range(4)]


def attention(P, I, oaT, omT):
    with ExitStack() as st:
        C = load_consts(P, st, I)
        RS = Ring(P, st, [128, 512], F32, 3, "ps_s", psum=True)
        RO = Ring(P, st, [128, 512], F32, 2, "ps_o", psum=True)
        RD = Ring(P, st, [128, 512], F32, 2, "ps_d", psum=True)
        RP = Ring(P, st, [128, 512], BF16, 4, "pT")
        RQ = Ring(P, st, [128, 512], BF16, 3, "qT")
        RQ2 = Ring(P, st, [64, 512], BF16, 3, "qT2")
        RF = Ring(P, st, [128, 512], F32, 3, "rden")
        ROB = Ring(P, st, [128, 512], BF16, 3, "ob")
        RK = Ring(P, st, [128, NK], BF16, 2, "kT")
        RV = Ring(P, st, [128, NKT, 128], BF16, 2, "V")
        kr = P.sbuf(st, [64, NK], BF16, "krope")
        d_kr = Dep()
        xd = I.get("_xdeps", {})

        def xdeps(*names):
            return [xd[n] for n in names if n in xd]

        def run_head(kT, d_kT, V, d_V, qsrc, dst, rope_q=None):
            for (t0, n, nkt) in QBLKS:
                q, d_q = RQ.get()
                P.dma(q[:, :n], qsrc[0:128, t0:t0 + n], writes=[d_q])
                if rope_q is not None:
                    q2, d_q2 = RQ2.get()
                    P.dma(q2[:, :n], qsrc[128:192, t0:t0 + n], writes=[d_q2])
                po, d_po = RO.get()
                pd, d_pd = RD.get()

                def qk(kt):
                    ps, d_ps = RS.get()
                    if rope_q is None:
                        P.op("pe", lambda e: e.matmul(ps[:, :n], lhsT=kT[:, kt * 128:(kt + 1) * 128], rhs=q[:, :n], start=True, stop=True),
                             reads=[d_kT, d_q], writes=[d_ps], sig=True)
                    else:
                        P.op("pe", lambda e: e.matmul(ps[:, :n], lhsT=kT[:, kt * 128:(kt + 1) * 128], rhs=q[:, :n], start=True, stop=False),
                             reads=[d_kT, d_q], writes=[d_ps], sig=False)
                        P.op("pe", lambda e: e.matmul(ps[:, :n], lhsT=kr[:, kt * 128:(kt + 1) * 128], rhs=q2[:, :n], start=False, stop=True),
                             reads=[d_kr, d_q2], writes=[d_ps], sig=True)
                    return ps, d_ps
                nxt = qk(0)
                for kt in range(nkt):
                    ps, d_ps = nxt
                    if kt + 1 < nkt:
                        nxt = qk(kt + 1)
                    pT, d_pT = RP.get()
                    P.op("act", lambda e: e.activation(out=pT[:, :n], in_=ps[:, :n], func=AF.Exp), reads=[d_ps], writes=[d_pT])
                    P.op("pe", lambda e: e.matmul(po[:, :n], lhsT=V[:, kt, :], rhs=pT[:, :n], start=(kt == 0), stop=(kt == nkt - 1)),
                         reads=[d_V, d_pT], writes=[d_po], sig=False)
                    P.op("pe", lambda e: e.matmul(pd[:, :n], lhsT=C.onesb[:], rhs=pT[:, :n], start=(kt == 0), stop=(kt == nkt - 1)),
                         reads=[C.d_onesb, d_pT], writes=[d_pd], sig=True)
                rd, d_rd = RF.get()
                P.op("dve", lambda e: e.reciprocal(out=rd[:, :n], in_=pd[:, :n]), reads=[d_pd], writes=[d_rd])
                ob, d_ob = ROB.get()
                P.op("dve", lambda e: e.tensor_tensor(out=ob[:, :n], in0=po[:, :n], in1=rd[:, :n], op=ALU.mult),
                     reads=[d_po, d_rd], writes=[d_ob])
                P.dma(dst[:, t0:t0 + n], ob[:, :n], reads=[d_ob])

        for kvh in range(2):
            kT, d_kT = RK.get()
            V, d_V = RV.get()
            P.dma(kT[:], I["kTa_all"][kvh], reads=xdeps("kTa_all"), writes=[d_kT])
            P.dma(V[:], I["va_all"].rearrange("(kt p) c -> p kt c", p=128)[:, :, kvh * 128:(kvh + 1) * 128], reads=xdeps("va_all"), writes=[d_V])
            for g in range(4):
                h = kvh * 4 + g
                run_head(kT, d_kT, V, d_V, I["qTa"][h], oaT[h])
        P.dma(kr[:], I["mkrT_all"], reads=xdeps("mkrT_all"), writes=[d_kr])
        for h in range(8):
            kT, d_kT = RK.get()
            V, d_V = RV.get()
            P.dma(kT[:], I["mkT_all"][h], reads=xdeps("mkT_all"), writes=[d_kT])
            P.dma(V[:], I["vm_all"].rearrange("(kt p) c -> p kt c", p=128)[:, :, h * 128:(h + 1) * 128], reads=xdeps("vm_all"), writes=[d_V])
            run_head(kT, d_kT, V, d_V, I["mqT"][h], omT[h], rope_q=True)
        P.barrier()


def load_tri(P, st, C, I):
    C.triF = P.sbuf(st, [128, 128], F32, "triF")
    C.triB = P.sbuf(st, [128, 128], F32, "triB")
    C.d_tri = Dep()
    P.dma(C.triF[:], I["triF"], writes=[C.d_tri])
    P.dma(C.triB[:], I["triB"], writes=[C.d_tri])


def gla_stage2(P, I, olT):
    with ExitStack() as st:
        C = load_consts(P, st, I)
        load_tri(P, st, C, I)
        R = mk_rings(P, st)
        R["la"] = Ring(P, st, [128, 512], F32, 2, "la")
        R["lk"] = Ring(P, st, [128, 512], BF16, 2, "lk")
        R["lv"] = Ring(P, st, [128, 1024], BF16, 2, "lv")
        lqT = P.sbuf(st, [128, 4, T], BF16, "lqT")
        lkT = P.sbuf(st, [128, 4, T], BF16, "lkT")
        d_lqT, d_lkT = Dep(), Dep()
        P.dma(lqT[:], I["lqT"].rearrange("h p t -> p h t"), writes=[d_lqT])
        P.dma(lkT[:], I["lkT"].rearrange("h p t -> p h t"), writes=[d_lkT])
        oT = P.sbuf(st, [128, 8, T], F32, "oT")
        d_oT = [Dep() for _ in range(18)]
        state = [P.sbuf(st, [128, 256], F32, f"st{h}") for h in range(4)]
        state_bf = [P.sbuf(st, [128, 256], BF16, f"stb{h}") for h in range(4)]
        d_state = [Dep() for _ in range(4)]
        d_state_bf = [Dep() for _ in range(4)]
        gon = P.sbuf(st, [128, 2], F32, "gon")
        d_gon = Dep()
        P.dma(gon[:], I["gon"], writes=[d_gon])
        RA = Ring(P, st, [128, 4], F32, 2, "trA")
        RA16 = Ring(P, st, [128, 16], F32, 2, "trA16")
        RU = Ring(P, st, [128, 256], F32, 3, "trU")
        if "recvF" in I:
            qmt = P.sbuf(st, [128, 16], F32, "qmask")
            d_qmt = Dep()
            P.dma(qmt[:], I["qmask"], writes=[d_qmt])
            I = dict(I)
            I["qmask_sb"] = (qmt, d_qmt)
        for d in range(2):
            src = {"la": I["laf" if d == 0 else "lab"], "lk": I["lk"], "lv": I["lv"]}
            emit = dict(oT=oT, d_oT=d_oT, first=(d == 0), lqT=lqT, lkT=lkT, d_lqT=d_lqT, d_lkT=d_lkT)
            for h in range(4):
                P.op("pool", lambda e, h=h: e.memset(state[h][:], 0.0), writes=[d_state[h]])
                P.op("pool", lambda e, h=h: e.memset(state_bf[h][:], 0.0), writes=[d_state_bf[h]])
            ids = [0, 1] if d == 0 else [1, 0]
            gla_pass(P, st, C, R, src, d, ids, state, d_state, state_bf, d_state_bf, emit=emit)
            if "recvF" in I:
                qm = I["qmask_sb"]
                order = range(4) if d == 0 else range(3, -1, -1)
                for j in order:
                    A, d_A = RA16.get()
                    P.dma(A[:], I["recvF"][16, j * 128:(j + 1) * 128, d * 16:(d + 1) * 16], reads=[I["_xdeps"]["recvF"]], writes=[d_A])
                    mcol = d * 4 + j
                    P.op("dve", lambda e, A=A: e.tensor_scalar(out=A[:], in0=A[:], scalar1=qm[0][:, mcol:mcol + 1], scalar2=qm[0][:, 8 + mcol:9 + mcol],
                                                               op0=ALU.mult, op1=ALU.add), reads=[d_A, qm[1]], writes=[d_A])
                    for h in range(4):
                        U, d_U = RU.get()
                        for f in range(2):
                            P.dma(U[:, f * 128:(f + 1) * 128], I["recvF"][(d * 4 + h) * 2 + f, j * 128:(j + 1) * 128, :],
                                  reads=[I["_xdeps"]["recvF"]], writes=[d_U])
                        P.op("pool", lambda e, U=U: e.tensor_scalar(out=U[:], in0=U[:], scalar1=qm[0][:, mcol:mcol + 1], scalar2=None, op0=ALU.mult),
                             reads=[d_U, qm[1]], writes=[d_U])
                        P.op("dve", lambda e, h=h, A=A, U=U: e.scalar_tensor_tensor(out=state[h][:], in0=state[h][:], scalar=A[:, 4 * h:4 * h + 1],
                                                                                    in1=U[:], op0=ALU.mult, op1=ALU.add),
                             reads=[d_A, d_U, d_state[h]], writes=[d_state[h]])
            for k in (range(3) if "recvF" not in I else ()):
                A, d_A = RA.get()
                P.dma(A[:], I["glaA3"][d, k], writes=[d_A])
                for h in range(4):
                    U, d_U = RU.get()
                    P.dma(U[:], I["glaU3"][d, k, h], writes=[d_U])
                    P.op("dve", lambda e, h=h, A=A, U=U: e.scalar_tensor_tensor(out=state[h][:], in0=state[h][:], scalar=A[:, h:h + 1],
                                                                                in1=U[:], op0=ALU.mult, op1=ALU.add),
                         reads=[d_A, d_U, d_state[h]], writes=[d_state[h]])
            for h in range(4):
                P.op("act", lambda e, h=h: e.copy(out=state_bf[h][:], in_=state[h][:]), reads=[d_state[h]], writes=[d_state_bf[h]])
            ids = list(range(2, 18)) if d == 0 else list(range(17, 1, -1))
            gla_pass(P, st, C, R, src, d, ids, state, d_state, state_bf, d_state_bf, emit=emit)
        RG = Ring(P, st, [128, 512], BF16, 3, "lg")
        for (t0, n) in BLKS:
            deps = [d_oT[ti] for ti in range(t0 // 128, (t0 + n) // 128)]
            dd = Dep()
            for h in range(4):
                outs = [R["f32"].get() for _ in range(2)]
                prods = []
                for j in range(2):
                    def prod(j=j, h=h):
                        d0 = Dep()
                        return oT[:, 2 * h + j, t0:t0 + n], deps
                    prods.append(prod)
                rms_fm_multi(P, R, C, prods, [gon[:, j:j + 1] for j in range(2)], n, 256,
                             [outs[j][0][:, :n] for j in range(2)], [outs[j][1] for j in range(2)])
                for j in range(2):
                    lg, d_lg = RG.get()
                    c = 2 * h + j
                    P.dma(lg[:, :n], I["lgT"][c * 128:(c + 1) * 128, t0:t0 + n], writes=[d_lg])
                    ob, d_ob = R["bf"].get()
                    P.op("dve", lambda e, j=j, lg=lg, ob=ob: e.tensor_tensor(out=ob[:, :n], in0=outs[j][0][:, :n], in1=lg[:, :n], op=ALU.mult),
                         reads=[outs[j][1], d_lg], writes=[d_ob])
                    P.dma(olT[c * 128:(c + 1) * 128, t0:t0 + n], ob[:, :n], reads=[d_ob])
        P.barrier()


def rms_fm_multi(P, R, C, producers, gains, n, nfeat, outs, d_outs):
    ys = []
    ss, d_ss = R["aux"].get()
    k = len(producers)
    for i, prod in enumerate(producers):
        src, deps = prod()
        rows = src.shape[0]
        sq, d_sq = R["bf"].get()
        P.op("act", lambda e: e.activation(out=sq[:rows, :n], in_=src, func=AF.Square), reads=list(deps), writes=[d_sq])
        P.op("pe", lambda e: e.matmul(ss[:, :n], lhsT=C.onesb[:rows, :], rhs=sq[:rows, :n], start=(i == 0), stop=(i == k - 1)),
             reads=[d_sq, C.d_onesb], writes=[d_ss], sig=(i == k - 1))
        ys.append((src, deps, rows))
    rs, d_rs = R["f32"].get()
    P.op("act", lambda e: e.activation(out=rs[:, :n], in_=ss[:, :n], func=AF.Sqrt, scale=1.0 / nfeat, bias=EPS),
         reads=[d_ss], writes=[d_rs])
    P.op("dve", lambda e: e.reciprocal(out=rs[:, :n], in_=rs[:, :n]), reads=[d_rs], writes=[d_rs])
    for (src, deps, rows), g, o, d_o in zip(ys, gains, outs, d_outs):
        P.op("dve", lambda e: e.scalar_tensor_tensor(out=o, in0=src, scalar=g, in1=rs[:rows, :n], op0=ALU.mult, op1=ALU.mult),
             reads=list(deps) + [d_rs], writes=[d_o])


def merge_stage(P, I, oaT, olT, omT, yT):
    with ExitStack() as st:
        R = mk_rings(P, st)
        acts = [P.sbuf(st, [128, 8, T], BF16, f"act{b}") for b in range(3)]
        d_acts = [Dep() for _ in range(3)]
        P.dma(acts[0][:], oaT.rearrange("h p t -> p h t"), writes=[d_acts[0]])
        P.dma(acts[1][:], olT.rearrange("(c p) t -> p c t", p=128), writes=[d_acts[1]])
        P.dma(acts[2][:], omT.rearrange("h p t -> p h t"), writes=[d_acts[2]])
        RW = Ring(P, st, [128, 8, 128], BF16, 6, "wbr")
        RG = Ring(P, st, [128, 512], BF16, 4, "gate")
        for nch in range(16):
            wb = []
            for br in range(3):
                w, d_w = RW.get()
                P.dma(w[:], I["w_br"][br].rearrange("(kc p) n -> p kc n", p=128)[:, :, nch * 128:(nch + 1) * 128], writes=[d_w], eng="pool")
                wb.append((w, d_w))
            for (t0, n) in BLKS:
                acc, d_acc = R["f32"].get()
                for br in range(3):
                    ps, d_ps = R["mm"].get()
                    for kc in range(8):
                        P.op("pe", lambda e, kc=kc: e.matmul(ps[:, :n], lhsT=wb[br][0][:, kc, :], rhs=acts[br][:, kc, t0:t0 + n],
                                                             start=(kc == 0), stop=(kc == 7)),
                             reads=[wb[br][1], d_acts[br]], writes=[d_ps], sig=(kc == 7))
                    g, d_g = RG.get()
                    P.dma(g[:, :n], I["gT"][br, nch * 128:(nch + 1) * 128, t0:t0 + n], writes=[d_g])
                    if br == 0:
                        P.op("dve", lambda e: e.tensor_tensor(out=acc[:, :n], in0=ps[:, :n], in1=g[:, :n], op=ALU.mult),
                             reads=[d_ps, d_g], writes=[d_acc])
                    else:
                        tmp, d_tmp = R["f32"].get()
                        P.op("dve", lambda e: e.tensor_tensor(out=tmp[:, :n], in0=ps[:, :n], in1=g[:, :n], op=ALU.mult),
                             reads=[d_ps, d_g], writes=[d_tmp])
                        P.op("pool", lambda e: e.tensor_tensor(out=acc[:, :n], in0=acc[:, :n], in1=tmp[:, :n], op=ALU.add),
                             reads=[d_tmp, d_acc], writes=[d_acc])
                ob, d_ob = R["bf"].get()
                P.op("act", lambda e: e.copy(out=ob[:, :n], in_=acc[:, :n]), reads=[d_acc], writes=[d_ob])
                P.dma(yT[nch, :, t0:t0 + n], ob[:, :n], reads=[d_ob])
        P.barrier()


def bcast_rows(P, R, sel, d_sel, rowsrc, d_rowsrc, gi, dst, d_dst):
    for r in range(2):
        for cb in range(4):
            ps, d_ps = R["aux"].get()
            P.op("pe", lambda e: e.matmul(ps[:, :512], lhsT=sel[0:2, r, :], rhs=rowsrc[0:2, gi, cb * 512:(cb + 1) * 512], start=True, stop=True),
                 reads=[d_sel, d_rowsrc], writes=[d_ps], sig=True)
            P.op("act", lambda e: e.copy(out=dst[r][:, cb * 512:(cb + 1) * 512], in_=ps[:, :512]), reads=[d_ps], writes=[d_dst[r]])


def wout_stage(P, I, yT, x1):
    with ExitStack() as st:
        R = mk_rings(P, st)
        wo = P.sbuf(st, [128, 16, D], BF16, "wo")
        d_wo = Dep()
        wv = I["w_out"].rearrange("(kc p) n -> p kc n", p=128)
        for q4 in range(4):
            P.dma(wo[:, q4 * 4:(q4 + 1) * 4, :], wv[:, q4 * 4:(q4 + 1) * 4, :], writes=[d_wo], eng="pool")
        sel = P.sbuf(st, [2, 2, 128], F32, "sel")
        grow = P.sbuf(st, [2, 2, D], F32, "grow")
        d_sel, d_grow = Dep(), Dep()
        P.dma(sel[:], I["sel"], writes=[d_sel])
        P.dma(grow[:], I["grow"], writes=[d_grow])
        gbc = [P.sbuf(st, [128, D], F32, f"g1bc{r}") for r in range(2)]
        d_gbc = [Dep(), Dep()]
        bcast_rows(P, R, sel, d_sel, grow, d_grow, 0, gbc, d_gbc)
        RX = Ring(P, st, [128, D], F32, 2, "x")
        RY = Ring(P, st, [128, 16, 128], BF16, 2, "yTt")
        for ti in range(18):
            r = 1 if ti < 2 else 0
            xt, d_xt = RX.get()
            P.dma(xt[:], I["xin"][ti * 128:(ti + 1) * 128, :], writes=[d_xt])
            yt, d_yt = RY.get()
            P.dma(yt[:], yT[:, :, ti * 128:(ti + 1) * 128].rearrange("c p t -> p c t"), writes=[d_yt])
            for cb in range(4):
                ps, d_ps = R["mm"].get()
                for kc in range(16):
                    P.op("pe", lambda e, kc=kc: e.matmul(ps[:, :512], lhsT=yt[:, kc, :], rhs=wo[:, kc, cb * 512:(cb + 1) * 512],
                                                         start=(kc == 0), stop=(kc == 15)),
                         reads=[d_yt, d_wo], writes=[d_ps], sig=(kc == 15))
                t, d_t = R["f32"].get()
                P.op("dve", lambda e: e.tensor_tensor(out=t[:, :512], in0=ps[:, :512], in1=gbc[r][:, cb * 512:(cb + 1) * 512], op=ALU.mult),
                     reads=[d_ps, d_gbc[r]], writes=[d_t])
                P.op("pool", lambda e: e.tensor_tensor(out=xt[:, cb * 512:(cb + 1) * 512], in0=xt[:, cb * 512:(cb + 1) * 512],
                                                       in1=t[:, :512], op=ALU.add), reads=[d_t, d_xt], writes=[d_xt])
            P.dma(x1[ti * 128:(ti + 1) * 128, :], xt[:], reads=[d_xt])
        P.barrier()


def convert_tables(P, I, ubf, vbf):
    for i in range(16):
        P.dma(ubf[i * 128:(i + 1) * 128, :], I["peer_uT"][i * 128:(i + 1) * 128, :], eng="pool")
        P.dma(vbf[i * 1024:(i + 1) * 1024, :], I["peer_v"][i * 1024:(i + 1) * 1024, :], eng="pool")


def peer_stage(P, I, x1, xout, last):
    S = P.dram("peerS", [T, 16, 128], F32)
    AB = P.dram("peerAB", [T, 2, 8, 128], F32)
    ZI = P.dram("peerZI", [T, 8], F32)
    h2T = P.dram("h2T", [16, 128, T], BF16)
    if "ubf" in I:
        ubf, vbf = I["ubf"], I["vbf"]
    else:
        ubf = P.dram("ubf", [D, NEXP], BF16)
        vbf = P.dram("vbf", [NEXP, D], BF16)
        convert_tables(P, I, ubf, vbf)
    with ExitStack() as st:
        C = load_consts(P, st, I)
        hT = P.sbuf(st, [128, 16, T], BF16, "h2Ts")
        d_hT = [(Dep(), Dep()) for _ in range(18)]
        mfm = P.sbuf(st, [128, 96, 2], F32, "mfm")
        n2g = P.sbuf(st, [128, 16], F32, "n2g")
        gm2 = P.sbuf(st, [128, 16, 2], F32, "gm2")
        d_mfm, d_n2g, d_gm2 = Dep(), Dep(), Dep()
        P.dma(mfm[:], I["mfm"], writes=[d_mfm])
        P.dma(n2g[:], I["n2g"], writes=[d_n2g])
        P.op("dve", lambda e: e.tensor_scalar(out=gm2[:], in0=mfm[:, 64:80, :], scalar1=1.0, scalar2=None, op0=ALU.add),
             reads=[d_mfm], writes=[d_gm2])
        P.op("dve", lambda e: e.tensor_tensor(out=gm2[:], in0=gm2[:], in1=n2g[:].unsqueeze(2).to_broadcast([128, 16, 2]), op=ALU.mult),
             reads=[d_gm2, d_n2g], writes=[d_gm2])
        norm_to_fm(P, st, C, x1, hT, d_hT, gm2, d_gm2, mfm, d_mfm, 3)
        for kc in range(16):
            P.dma(h2T[kc], hT[:, kc, :], reads=[x for p in d_hT for x in p])
        P.barrier()
    with ExitStack() as st:
        R = mk_rings(P, st)
        kT = P.sbuf(st, [128, 16, 128], F32, "k12T")
        d_kT = Dep()
        P.dma(kT[:], I["k12T"].rearrange("c d n -> d c n"), writes=[d_kT])
        wq = P.sbuf(st, [128, 16, D], BF16, "wq")
        d_wq = Dep()
        wv = I["peer_wq"].rearrange("(kc p) n -> p kc n", p=128)
        for q4 in range(4):
            P.dma(wq[:, q4 * 4:(q4 + 1) * 4, :], wv[:, q4 * 4:(q4 + 1) * 4, :], writes=[d_wq], eng="pool")
        RH = Ring(P, st, [128, 16, 512], BF16, 2, "h2blk")
        RSC = Ring(P, st, [128, 4, 16, 128], F32, 1, "sc")
        for (t0, n) in BLKS:
            hb, d_hb = RH.get()
            P.dma(hb[:, :, :n], h2T[:, :, t0:t0 + n].rearrange("c p t -> p c t"), writes=[d_hb])
            sc, d_sc = RSC.get()
            for c in range(16):
                ps, d_ps = R["mm"].get()
                for kc in range(16):
                    P.op("pe", lambda e, kc=kc: e.matmul(ps[:, :n], lhsT=wq[:, kc, c * 128:(c + 1) * 128], rhs=hb[:, kc, :n],
                                                         start=(kc == 0), stop=(kc == 15)),
                         reads=[d_wq, d_hb], writes=[d_ps], sig=(kc == 15))
                qc, d_qc = R["f32"].get()
                P.op("act", lambda e: e.copy(out=qc[:, :n], in_=ps[:, :n]), reads=[d_ps], writes=[d_qc])
                for tt in range(n // 128):
                    ps2, d_ps2 = R["aux"].get()
                    P.op("pe", lambda e: e.matmul(ps2[:, :128], lhsT=qc[:, tt * 128:(tt + 1) * 128], rhs=kT[:, c, :], start=True, stop=True),
                         reads=[d_qc, d_kT], writes=[d_ps2], sig=True)
                    P.op("dve", lambda e: e.tensor_copy(out=sc[:, tt, c, :], in_=ps2[:, :128]), reads=[d_ps2], writes=[d_sc])
            for tt in range(n // 128):
                P.dma(S[t0 + tt * 128:t0 + (tt + 1) * 128], sc[:, tt], reads=[d_sc])
        P.barrier()
    NEG = -1.0e30
    with ExitStack() as st:
        RSC = Ring(P, st, [128, 16, 128], F32, 2, "sct")
        RAB = Ring(P, st, [128, 2, 8, 128], F32, 2, "abt")
        RZ = Ring(P, st, [128, 8], F32, 2, "zit")
        RW = Ring(P, st, [128, 256], F32, 4, "wk")
        RV = Ring(P, st, [128, 2, 16], F32, 2, "v12")
        RM = Ring(P, st, [128, 32], F32, 4, "m8")
        for ti in range(18):
            sc, d_sc = RSC.get()
            P.dma(sc[:], S[ti * 128:(ti + 1) * 128], writes=[d_sc])
            ab, d_ab = RAB.get()
            zi, d_zi = RZ.get()
            for h in range(8):
                v12, d_v = RV.get()
                for half in range(2):
                    s = sc[:, 2 * h + half, :]
                    wk, d_wk = RW.get()
                    P.op("dve", lambda e: e.max(out=v12[:, half, 0:8], in_=s), reads=[d_sc], writes=[d_v])
                    P.op("dve", lambda e: e.match_replace(out=wk[:, :128], in_to_replace=v12[:, half, 0:8], in_values=s, imm_value=NEG),
                         reads=[d_sc, d_v], writes=[d_wk])
                    P.op("dve", lambda e: e.max(out=v12[:, half, 8:16], in_=wk[:, :128]), reads=[d_wk], writes=[d_v])
                cand, d_cand = RW.get()
                P.op("dve", lambda e: e.tensor_tensor(out=cand[:].rearrange("p (a b) -> p a b", a=16),
                                                      in0=v12[:, 0, :].unsqueeze(2).to_broadcast([128, 16, 16]),
                                                      in1=v12[:, 1, :].unsqueeze(1).to_broadcast([128, 16, 16]), op=ALU.add),
                     reads=[d_v], writes=[d_cand])
                m8, d_m8 = RM.get()
                wk2, d_wk2 = RW.get()
                P.op("dve", lambda e: e.max(out=m8[:, 0:8], in_=cand[:]), reads=[d_cand], writes=[d_m8])
                P.op("dve", lambda e: e.match_replace(out=wk2[:], in_to_replace=m8[:, 0:8], in_values=cand[:], imm_value=NEG),
                     reads=[d_cand, d_m8], writes=[d_wk2])
                P.op("dve", lambda e: e.max(out=m8[:, 8:16], in_=wk2[:]), reads=[d_wk2], writes=[d_m8])
                P.op("dve", lambda e: e.match_replace(out=wk2[:], in_to_replace=m8[:, 8:16], in_values=wk2[:], imm_value=NEG),
                     reads=[d_m8, d_wk2], writes=[d_wk2])
                P.op("dve", lambda e: e.max(out=m8[:, 16:24], in_=wk2[:]), reads=[d_wk2], writes=[d_m8])
                P.op("dve", lambda e: e.tensor_tensor(out=m8[:, 25:26], in0=m8[:, 15:16], in1=m8[:, 16:17], op=ALU.add),
                     reads=[d_m8], writes=[d_m8])
                P.op("dve", lambda e: e.tensor_scalar(out=m8[:, 24:25], in0=m8[:, 25:26], scalar1=-0.5, scalar2=None, op0=ALU.mult),
                     reads=[d_m8], writes=[d_m8])
                ex, d_ex = RM.get()
                P.op("act", lambda e: e.activation(out=ex[:, 0:16], in_=m8[:, 0:16], func=AF.Exp, bias=m8[:, 24:25], accum_out=ex[:, 16:17]),
                     reads=[d_m8], writes=[d_ex])
                P.op("dve", lambda e: e.reciprocal(out=zi[:, h:h + 1], in_=ex[:, 16:17]), reads=[d_ex], writes=[d_zi])
                P.op("dve", lambda e: e.tensor_scalar(out=ab[:, 0, h, :], in0=sc[:, 2 * h, :], scalar1=m8[:, 24:25], scalar2=None, op0=ALU.add),
                     reads=[d_sc, d_m8], writes=[d_ab])
                P.op("pool", lambda e: e.tensor_copy(out=ab[:, 1, h, :], in_=sc[:, 2 * h + 1, :]), reads=[d_sc], writes=[d_ab])
            P.dma(AB[ti * 128:(ti + 1) * 128], ab[:], reads=[d_ab])
            P.dma(ZI[ti * 128:(ti + 1) * 128], zi[:], reads=[d_zi])
        P.barrier()
    with ExitStack() as st:
        C = load_consts(P, st, I)
        RO = Ring(P, st, [128, 512], F32, 4, "po", psum=True)
        RA = Ring(P, st, [128, 512], F32, 2, "paT", psum=True)
        RG = Ring(P, st, [128, 512], F32, 2, "pgT", psum=True)
        RU = Ring(P, st, [128, 16, 512], BF16, 2, "ublk")
        RVB = Ring(P, st, [128, 4, D], BF16, 2, "vblk")
        RSP = Ring(P, st, [128, 8, 4, 128], F32, 1, "sp")
        RE = Ring(P, st, [128, 8, 4, 128], BF16, 1, "ee")
        RGH = Ring(P, st, [128, 8, 4, 128], BF16, 2, "gh")
        RGL = Ring(P, st, [128, 512], F32, 2, "gelu")
        RWA = Ring(P, st, [128, 4, 128], BF16, 2, "wact")
        RAB = Ring(P, st, [128, 2, 8, 128], F32, 1, "ab")
        RZ = Ring(P, st, [128, 8], F32, 2, "zi")
        RDZ = Ring(P, st, [128, 8, 128], BF16, 2, "dz")
        RH = Ring(P, st, [128, 16, 128], BF16, 2, "h2t")
        RX = Ring(P, st, [128, D], F32, 2, "x1t")
        RF = Ring(P, st, [128, 512], F32, 3, "tmpf")
        RS8 = Ring(P, st, [128, 8], F32, 4, "s8")
        sel = P.sbuf(st, [2, 2, 128], F32, "sel")
        grow = P.sbuf(st, [2, 2, D], F32, "grow")
        d_sel, d_grow = Dep(), Dep()
        P.dma(sel[:], I["sel"], writes=[d_sel])
        P.dma(grow[:], I["grow"], writes=[d_grow])
        gbc = [P.sbuf(st, [128, D], F32, f"g2bc{r}") for r in range(2)]
        d_gbc = [Dep(), Dep()]
        Rtmp = {"aux": RA}
        bcast_rows(P, Rtmp, sel, d_sel, grow, d_grow, 1, gbc, d_gbc)
        if last:
            fg = P.sbuf(st, [1, D], F32, "fg")
            d_fg = Dep()
            P.dma(fg[:], I["fing"], writes=[d_fg])
            fgbc = P.sbuf(st, [128, D], F32, "fgbc")
            d_fgbc = Dep()
            for cb in range(4):
                ps, d_ps = RA.get()
                P.op("pe", lambda e: e.matmul(ps[:, :512], lhsT=C.onesf[0:1, :], rhs=fg[0:1, cb * 512:(cb + 1) * 512], start=True, stop=True),
                     reads=[C.d_onesf, d_fg], writes=[d_ps], sig=True)
                P.op("act", lambda e: e.copy(out=fgbc[:, cb * 512:(cb + 1) * 512], in_=ps[:, :512]), reads=[d_ps], writes=[d_fgbc])
        uv = ubf.rearrange("(kc p) e -> p kc e", p=128)
        vv = vbf.rearrange("(c p) d -> p c d", p=128)
        for ti in range(18):
            r = 1 if ti < 2 else 0
            ht, d_ht = RH.get()
            P.dma(ht[:], h2T[:, :, ti * 128:(ti + 1) * 128].rearrange("c p t -> p c t"), writes=[d_ht])
            ab, d_ab = RAB.get()
            P.dma(ab[:], AB[ti * 128:(ti + 1) * 128], writes=[d_ab])
            zi, d_zi = RZ.get()
            P.dma(zi[:], ZI[ti * 128:(ti + 1) * 128], writes=[d_zi])
            dz, d_dz = RDZ.get()
            for h in range(8):
                P.op("pool", lambda e, h=h: e.tensor_scalar(out=dz[:, h, :], in0=C.identf[:], scalar1=zi[:, h:h + 1], scalar2=None, op0=ALU.mult),
                     reads=[C.d_identf, d_zi], writes=[d_dz])
            pos = [RO.get() for _ in range(4)]

            def stageA(eb):
                sp, d_sp = RSP.get()
                P.op("pool", lambda e: e.tensor_tensor(out=sp[:],
                                                       in0=ab[:, 0, :, eb * 4:(eb + 1) * 4].unsqueeze(3).to_broadcast([128, 8, 4, 128]),
                                                       in1=ab[:, 1, :, :].unsqueeze(2).to_broadcast([128, 8, 4, 128]), op=ALU.add),
                     reads=[d_ab], writes=[d_sp])
                ee, d_ee = RE.get()
                P.op("act", lambda e: e.activation(out=ee[:], in_=sp[:], func=AF.Exp), reads=[d_sp], writes=[d_ee])
                gh, d_gh = RGH.get()
                P.op("dve", lambda e: e.scalar_tensor_tensor(out=gh[:], in0=sp[:], scalar=0.0, in1=ee[:], op0=ALU.is_ge, op1=ALU.mult),
                     reads=[d_sp, d_ee], writes=[d_gh])
                return gh, d_gh

            def stageB(eb):
                ub, d_ub = RU.get()
                P.dma(ub[:], uv[:, :, eb * 512:(eb + 1) * 512], writes=[d_ub])
                vb, d_vb = RVB.get()
                P.dma(vb[:], vv[:, eb * 4:(eb + 1) * 4, :], writes=[d_vb])
                pa, d_pa = RA.get()
                for c in range(4):
                    for kc in range(16):
                        P.op("pe", lambda e, c=c, kc=kc: e.matmul(pa[:, c * 128:(c + 1) * 128], lhsT=ub[:, kc, c * 128:(c + 1) * 128], rhs=ht[:, kc, :],
                                                                  start=(kc == 0), stop=(kc == 15)),
                             reads=[d_ub, d_ht], writes=[d_pa], sig=(c == 3 and kc == 15))
                gl, d_gl = RGL.get()
                P.op("act", lambda e: e.activation(out=gl[:], in_=pa[:], func=AF.Gelu), reads=[d_pa], writes=[d_gl])
                return gl, d_gl, vb, d_vb

            nA = stageA(0)
            nB = stageB(0)
            for eb in range(32):
                gh, d_gh = nA
                gl, d_gl, vb, d_vb = nB
                if eb + 1 < 32:
                    nA = stageA(eb + 1)
                pg, d_pg = RG.get()
                for c in range(4):
                    for h in range(8):
                        P.op("pe", lambda e, c=c, h=h: e.matmul(pg[:, c * 128:(c + 1) * 128], lhsT=gh[:, h, c, :], rhs=dz[:, h, :],
                                                                start=(h == 0), stop=(h == 7)),
                             reads=[d_gh, d_dz], writes=[d_pg], sig=(c == 3 and h == 7))
                if eb + 1 < 32:
                    nB = stageB(eb + 1)
                wa, d_wa = RWA.get()
                P.op("dve", lambda e: e.tensor_tensor(out=wa[:].rearrange("p c t -> p (c t)"), in0=pg[:], in1=gl[:], op=ALU.mult),
                     reads=[d_pg, d_gl], writes=[d_wa])
                for db in range(4):
                    po, d_po = pos[db]
                    for c in range(4):
                        P.op("pe", lambda e, c=c, db=db, po=po: e.matmul(po[:, :512], lhsT=wa[:, c, :], rhs=vb[:, c, db * 512:(db + 1) * 512],
                                                                         start=(eb == 0 and c == 0), stop=(eb == 31 and c == 3)),
                             reads=[d_wa, d_vb], writes=[d_po], sig=(c == 3))
            xt, d_xt = RX.get()
            P.dma(xt[:], x1[ti * 128:(ti + 1) * 128, :], writes=[d_xt])
            for db in range(4):
                po, d_po = pos[db]
                t, d_t = RF.get()
                P.op("dve", lambda e: e.tensor_tensor(out=t[:, :512], in0=po[:, :512], in1=gbc[r][:, db * 512:(db + 1) * 512], op=ALU.mult),
                     reads=[d_po, d_gbc[r]], writes=[d_t])
                P.op("pool", lambda e: e.tensor_tensor(out=xt[:, db * 512:(db + 1) * 512], in0=xt[:, db * 512:(db + 1) * 512], in1=t[:, :512], op=ALU.add),
                     reads=[d_t, d_xt], writes=[d_xt])
            if last:
                s8, d_s8 = RS8.get()
                xo, d_xo = RX.get()
                P.op("act", lambda e: e.activation(out=xo[:], in_=xt[:], func=AF.Square, accum_out=s8[:, 0:1]), reads=[d_xt], writes=[d_xo, d_s8])
                P.op("act", lambda e: e.activation(out=s8[:, 1:2], in_=s8[:, 0:1], func=AF.Sqrt, scale=1.0 / D, bias=EPS), reads=[d_s8], writes=[d_s8])
                P.op("dve", lambda e: e.reciprocal(out=s8[:, 2:3], in_=s8[:, 1:2]), reads=[d_s8], writes=[d_s8])
                P.op("dve", lambda e: e.scalar_tensor_tensor(out=xo[:], in0=xt[:], scalar=s8[:, 2:3], in1=fgbc[:], op0=ALU.mult, op1=ALU.mult),
                     reads=[d_xt, d_s8, d_fgbc, d_xo], writes=[d_xo])
                P.dma(xout[ti * 128:(ti + 1) * 128, :], xo[:], reads=[d_xo])
            else:
                P.dma(xout[ti * 128:(ti + 1) * 128, :], xt[:], reads=[d_xt])
        P.barrier()


def stage2(P, I, O, last):
    oaT = P.dram("oaT", [8, 128, T], BF16)
    omT = P.dram("omT", [8, 128, T], BF16)
    olT = P.dram("olT", [1024, T], BF16)
    yT = P.dram("yT", [16, 128, T], BF16)
    x1 = P.dram("x1", [T, D], F32)
    attention(P, I, oaT, omT)
    gla_stage2(P, I, olT)
    merge_stage(P, I, oaT, olT, omT, yT)
    wout_stage(P, I, yT, x1)
    peer_stage(P, I, x1, O["xout"], last)


def build_s2(last):
    P = Prog()
    I = {k: P.dram(k, sh, dt, kind="ExternalInput") for k, (sh, dt) in S2_INPUTS.items()}
    O = {k: P.dram(k, sh, dt, kind="ExternalOutput") for k, (sh, dt) in S2_OUTPUTS.items()}
    stage2(P, I, O, last)
    return P.finish(), P


OWN_KEYS = ["mfm", "grow", "qTa", "mqT", "lqT", "lkT", "lk", "lv", "lgT", "laf", "lab", "gT"]


def s2_inputs(core, layer, xin_core, s1out, inp, shared):
    b, q = core // 4, core % 4
    L = layer
    own = s1out[core]
    grp = [s1out[b * 4 + j] for j in range(4)]
    m = {"xin": xin_core}
    for k in OWN_KEYS:
        m[k] = own[k]
    m["kTa_all"] = np.ascontiguousarray(np.concatenate([own["kTa"][:, :, :NCTX]] + [g["kTa"][:, :, NCTX:] for g in grp], axis=2))
    m["mkT_all"] = np.ascontiguousarray(np.concatenate([own["mkT"][:, :, :NCTX]] + [g["mkT"][:, :, NCTX:] for g in grp], axis=2))
    m["mkrT_all"] = np.ascontiguousarray(np.concatenate([own["mkrT"][:, :NCTX]] + [g["mkrT"][:, NCTX:] for g in grp], axis=1))
    m["va_all"] = np.ascontiguousarray(np.concatenate([own["va"][:NCTX]] + [g["va"][NCTX:] for g in grp], axis=0))
    m["vm_all"] = np.ascontiguousarray(np.concatenate([own["vm"][:NCTX]] + [g["vm"][NCTX:] for g in grp], axis=0))
    m["glaC"] = own["glaC"]
    A3 = np.ones((2, 3, 128, 4), np.float32)
    U3 = np.zeros((2, 3, 4, 128, 256), np.float32)
    for k in range(3):
        qq = q - 3 + k
        if qq >= 0:
            A3[0, k] = grp[qq]["glaA"][0][:, 0::4]
            U3[0, k] = grp[qq]["glaU"][0]
        qq = q + 3 - k
        if qq <= 3:
            A3[1, k] = grp[qq]["glaA"][1][:, 0::4]
            U3[1, k] = grp[qq]["glaU"][1]
    m["glaA3"], m["glaU3"] = A3, U3
    m.update(shared)
    return m


def s2_shared(layer, inp):
    L = layer
    sh = {}
    sh["w_br"] = np.ascontiguousarray(np.stack([inp["w_br_gqa"][L], inp["w_br_gla"][L], inp["w_br_mla"][L]], 0))
    sh["w_out"] = inp["w_out"][L]
    sh["n2g"] = _fm16(inp["norm2_g"][L])
    sh["gon"] = np.ascontiguousarray(inp["gla_on_g"][L].reshape(2, 128).T)
    sh["peer_wq"] = inp["peer_wq"][L]
    k12 = np.stack([inp["peer_k1"][L], inp["peer_k2"][L]], 1)
    sh["k12T"] = np.ascontiguousarray(k12.reshape(16, 128, 128).transpose(0, 2, 1))
    sh["peer_uT"] = np.ascontiguousarray(inp["peer_u"][L].T)
    sh["peer_v"] = inp["peer_v"][L]
    sh["fing"] = np.ascontiguousarray(inp["final_g"].reshape(1, D))
    sel = np.zeros((2, 2, 128), np.float32)
    sel[0, 0] = 1.0
    sel[1, 1] = 1.0
    sh["sel"] = sel
    c = consts_np()
    for k in ("ident", "ones", "triF", "triB"):
        sh[k] = c[k]
    return sh


_PROGS = {}


def _prog(name):
    if name not in _PROGS:
        if name == "s1":
            _PROGS[name] = build_s1()[0]
        elif name == "s2":
            _PROGS[name] = build_s2(False)[0]
        else:
            _PROGS[name] = build_s2(True)[0]
    return _PROGS[name]


def kernel_unfused(**inputs):
    inp = {k: np.asarray(v) for k, v in inputs.items()}
    xs = [np.asarray(inp["x"][b], np.float32) for b in range(2)]
    ctxs = [np.asarray(inp["ctx"][b], np.float32) for b in range(2)]
    cores = list(range(8))
    for L in range(2):
        maps1 = [s1_inputs(c, L, xs, ctxs, inp) for c in cores]
        r1 = run_bass_kernel_spmd(_prog("s1"), maps1, core_ids=cores)
        s1out = r1.results
        shared = s2_shared(L, inp)
        maps2 = [s2_inputs(c, L, maps1[c]["xin"], s1out, inp, shared) for c in cores]
        del maps1
        r2 = run_bass_kernel_spmd(_prog("s2" if L == 0 else "s2last"), maps2, core_ids=cores)
        del maps2
        new_xs = []
        for b in range(2):
            new_xs.append(np.concatenate([np.asarray(r2.results[b * 4 + j]["xout"])[NCTX:] for j in range(4)], 0))
            ctxs[b] = np.asarray(r2.results[b * 4]["xout"])[:NCTX]
        xs = new_xs
    return np.stack(xs, 0).astype(np.float32)


GROUPS4 = [[0, 1, 2, 3], [4, 5, 6, 7]]
NCHB = 164
NCHF = 17

S1_W = ["w_mod", "bmodT", "bmod_g", "n1g", "w_in", "gq", "gk", "waf", "wab", "gmq", "gmkv", "wuq", "wukv"]
S2_W = ["w_br", "w_out", "n2g", "gon", "peer_wq", "k12T", "peer_uT", "peer_v"]
SHARED_IN = ["cT", "cosA", "sinA", "cosM", "sinM", "ident", "ones", "rotA", "rotM", "triF", "triB", "sel", "fing"]


def exchange(P, O1, X, conv=None):
    sendB = P.dram("sendB", [NCHB, 128, 256], BF16)
    recvB = P.dram("recvB", [NCHB, 512, 256], BF16)
    sendF = P.dram("sendF", [NCHF, 128, 128], F32)
    recvF = P.dram("recvF", [NCHF, 512, 128], F32)
    d_s = Dep()
    dX = {k: Dep() for k in ("kTa_all", "va_all", "mkT_all", "mkrT_all", "vm_all", "recvF")}
    L0 = NCTX
    for kvh in range(2):
        P.dma(sendB[kvh * 8:(kvh + 1) * 8], O1["kTa"][kvh][:, L0:].rearrange("p (t c) -> t p c", c=256), writes=[d_s])
    P.dma(sendB[16:32], O1["va"][L0:, :].rearrange("(i p) c -> i p c", p=128), writes=[d_s])
    for h in range(8):
        P.dma(sendB[32 + h * 8:40 + h * 8], O1["mkT"][h][:, L0:].rearrange("p (t c) -> t p c", c=256), writes=[d_s])
    vmv = sendB[96:160].rearrange("(i q) p c -> i q p c", q=4)
    for cq in range(4):
        P.dma(vmv[:, cq], O1["vm"][L0:, cq * 256:(cq + 1) * 256].rearrange("(i p) c -> i p c", p=128), writes=[d_s])
    for j in range(4):
        P.dma(sendB[160 + j].rearrange("(h p) c -> h p c", h=2),
              O1["mkrT"][:, L0 + j * 512:L0 + (j + 1) * 512].rearrange("p (h c) -> h p c", h=2), writes=[d_s])
    for d in range(2):
        for h in range(4):
            P.dma(sendF[(d * 4 + h) * 2:(d * 4 + h) * 2 + 2], O1["glaU"][d, h].rearrange("p (f c) -> f p c", f=2), writes=[d_s])
    P.dma(sendF[16][:, 0:32].rearrange("p (d c) -> p d c", d=2), O1["glaA"].rearrange("d p c -> p d c"), writes=[d_s])
    recs = []
    WIN = 4

    def gather(src, dst):
        if len(recs) >= WIN:
            P._wait("pool", recs[-WIN])
        recs.append(P.collective("AllGather", GROUPS4, src, dst, reads=[d_s], writes=[Dep()]))
    for i in range(32):
        gather(sendB[i].bitcast(F32), recvB[i].bitcast(F32))
    d_r1 = Dep()
    d_r1.w = recs[-1]
    P._wait("pool", recs[-1])
    P.dma(X["kTa_all"][:, :, 0:L0], O1["kTa"][:, :, 0:L0], writes=[dX["kTa_all"]])
    P.dma(X["va_all"][0:L0, :], O1["va"][0:L0, :], writes=[dX["va_all"]])
    for r in range(4):
        rr = slice(r * 128, (r + 1) * 128)
        t0 = L0 + r * NLAT
        for kvh in range(2):
            P.dma(X["kTa_all"][kvh][:, t0:t0 + NLAT].rearrange("p (t c) -> t p c", c=256), recvB[kvh * 8:(kvh + 1) * 8, rr, :],
                  reads=[d_r1], writes=[dX["kTa_all"]])
        P.dma(X["va_all"][t0:t0 + NLAT, :].rearrange("(i p) c -> i p c", p=128), recvB[16:32, rr, :], reads=[d_r1], writes=[dX["va_all"]])
    for i in range(32, NCHB):
        gather(sendB[i].bitcast(F32), recvB[i].bitcast(F32))
    for i in range(NCHF):
        gather(sendF[i], recvF[i])
    d_r2 = Dep()
    d_r2.w = recs[-1]
    if conv is not None:
        convert_tables(P, *conv)
    Q = "pool"
    P.dma(X["mkT_all"][:, :, 0:L0], O1["mkT"][:, :, 0:L0], writes=[dX["mkT_all"]], eng=Q)
    P.dma(X["mkrT_all"][:, 0:L0], O1["mkrT"][:, 0:L0], writes=[dX["mkrT_all"]], eng=Q)
    P.dma(X["vm_all"][0:L0, :], O1["vm"][0:L0, :], writes=[dX["vm_all"]], eng=Q)
    for r in range(4):
        rr = slice(r * 128, (r + 1) * 128)
        t0 = L0 + r * NLAT
        for h in range(8):
            P.dma(X["mkT_all"][h][:, t0:t0 + NLAT].rearrange("p (t c) -> t p c", c=256), recvB[32 + h * 8:40 + h * 8, rr, :],
                  reads=[d_r2], writes=[dX["mkT_all"]], eng=Q)
        rv = recvB[96:160].rearrange("(i q) m c -> i q m c", q=4)
        for cq in range(4):
            P.dma(X["vm_all"][t0:t0 + NLAT, cq * 256:(cq + 1) * 256].rearrange("(i p) c -> i p c", p=128), rv[:, cq, rr, :],
                  reads=[d_r2], writes=[dX["vm_all"]], eng=Q)
        for j in range(4):
            P.dma(X["mkrT_all"][:, t0 + j * 512:t0 + (j + 1) * 512].rearrange("p (h c) -> h p c", h=2),
                  recvB[160 + j, rr, :].rearrange("(h p) c -> h p c", h=2), reads=[d_r2], writes=[dX["mkrT_all"]], eng=Q)
    dX["recvF"].w = recs[-1]
    return recvF, dX


def fused_input_specs():
    specs = {"xin0": ([T, D], F32), "qmask": ([128, 16], F32)}
    for k in SHARED_IN:
        specs[k] = (S1_INPUTS.get(k) or S2_INPUTS[k])
    for L in range(2):
        for k in S1_W:
            specs[f"{k}_{L}"] = S1_INPUTS[k]
        for k in S2_W:
            specs[f"{k}_{L}"] = S2_INPUTS[k]
    return specs


def build_fused():
    P = Prog()
    specs = fused_input_specs()
    E = {k: P.dram(k, sh, dt, kind="ExternalInput") for k, (sh, dt) in specs.items()}
    xout = P.dram("xout", [T, D], F32, kind="ExternalOutput")
    xmid = P.dram("xmid", [T, D], F32)
    O1 = {k: P.dram("s1_" + k, sh, dt) for k, (sh, dt) in S1_OUTPUTS.items()}
    X = {k: P.dram(k, S2_INPUTS[k][0], S2_INPUTS[k][1]) for k in ("kTa_all", "va_all", "mkT_all", "mkrT_all", "vm_all")}
    for L in range(2):
        I1 = {k: E[k] for k in SHARED_IN if k in S1_INPUTS}
        I1["xin"] = E["xin0"] if L == 0 else xmid
        for k in S1_W:
            I1[k] = E[f"{k}_{L}"]
        stage1(P, I1, O1)
        ubf = P.dram("ubf", [D, NEXP], BF16)
        vbf = P.dram("vbf", [NEXP, D], BF16)
        recvF, dX = exchange(P, O1, X, conv=({"peer_uT": E[f"peer_uT_{L}"], "peer_v": E[f"peer_v_{L}"]}, ubf, vbf))
        I2 = {k: E[k] for k in SHARED_IN if k in S2_INPUTS}
        I2["xin"] = I1["xin"]
        for k in OWN_KEYS:
            I2[k] = O1[k]
        I2.update(X)
        I2["recvF"] = recvF
        I2["_xdeps"] = dX
        I2["ubf"], I2["vbf"] = ubf, vbf
        I2["qmask"] = E["qmask"]
        for k in S2_W:
            I2[k] = E[f"{k}_{L}"]
        stage2(P, I2, {"xout": xmid if L == 0 else xout}, last=(L == 1))
    return P.finish(), P


def fused_inputs(core, inp, shared2):
    b, q = core // 4, core % 4
    m = {}
    xs = [inp["x"][bb] for bb in range(2)]
    ctxs = [inp["ctx"][bb] for bb in range(2)]
    for L in range(2):
        s1 = s1_inputs(core, L, xs, ctxs, inp)
        if L == 0:
            m["xin0"] = s1["xin"]
            for k in SHARED_IN:
                if k in s1:
                    m[k] = s1[k]
        for k in S1_W:
            m[f"{k}_{L}"] = s1[k]
        for k in S2_W:
            m[f"{k}_{L}"] = shared2[L][k]
    m["sel"] = shared2[0]["sel"]
    m["fing"] = shared2[0]["fing"]
    qm = np.zeros((128, 16), np.float32)
    for j in range(4):
        qm[:, j] = 1.0 if j < q else 0.0
        qm[:, 4 + j] = 1.0 if j > q else 0.0
    qm[:, 8:16] = 1.0 - qm[:, 0:8]
    m["qmask"] = qm
    return m


def kernel_fused(**inputs):
    inp = {k: np.asarray(v) for k, v in inputs.items()}
    if "fused" not in _PROGS:
        _PROGS["fused"] = build_fused()[0]
    shared2 = [s2_shared(L, inp) for L in range(2)]
    cores = list(range(8))
    maps = [fused_inputs(c, inp, shared2) for c in cores]
    r = run_bass_kernel_spmd(_PROGS["fused"], maps, core_ids=cores)
    xs = []
    for b in range(2):
        xs.append(np.concatenate([np.asarray(r.results[b * 4 + j]["xout"])[NCTX:] for j in range(4)], 0))
    return np.stack(xs, 0).astype(np.float32)


def kernel(**inputs):
    return kernel_fused(**inputs)
```

```python
from contextlib import ExitStack
import numpy as np
import ml_dtypes
import concourse.bass as bass
import concourse.mybir as mybir
from concourse.bass_utils import run_bass_kernel_spmd

F32 = mybir.dt.float32
BF16 = mybir.dt.bfloat16
ALU = mybir.AluOpType
AF = mybir.ActivationFunctionType
AX = mybir.AxisListType

ENGS = ("pe", "act", "dve", "pool", "sp")
SEMQ = ENGS + ("cc",)
SEM_ROT = 12000
NSEM = {"pe": 8, "act": 10, "dve": 14, "pool": 8, "sp": 1, "cc": 1}
NSLOT = {"sp": 16, "pool": 12, "act": 4}

D = 2048
T = 2304
NCTX = 256
NLAT = 2048
KC = 16
EPS = 1e-6
import os
STOP = os.environ.get('K_STOP', '')
BLKS = [(0, 256), (256, 512), (768, 512), (1280, 512), (1792, 512)]
IN_COLS = 11872
O_AQ, O_AK, O_AV, O_LQ, O_LK, O_LV, O_LG, O_LRF, O_LRB, O_MCQ, O_MCKV, O_MKR, O_GA = (
    0, 1024, 1280, 1536, 2048, 2560, 3584, 4608, 4624, 4640, 5152, 5664, 5728)


class Dep:
    __slots__ = ("w", "r", "x")

    def __init__(self, x=False):
        self.w = None
        self.r = []
        self.x = x


class Rec:
    __slots__ = ("eng", "cnt", "is_dma", "dma_idx", "q")

    def __init__(self, eng, is_dma=False):
        self.eng = eng
        self.cnt = None
        self.is_dma = is_dma
        self.dma_idx = None


class Prog:
    def __init__(self):
        self.nc = bass.Bass("TRN2", target_bir_lowering=False)
        nc = self.nc
        self.E = {"pe": nc.tensor, "act": nc.scalar, "dve": nc.vector, "pool": nc.gpsimd, "sp": nc.sync}
        self.stack = ExitStack()
        self.sems = {e: [self.stack.enter_context(nc.semaphore(f"s_{e}_{i}")) for i in range(NSEM[e])] for e in SEMQ}
        self.slots = {q: [self.stack.enter_context(nc.semaphore(f"s_dma_{q}_{i}")) for i in range(n)] for q, n in NSLOT.items()}
        self.count = {e: 0 for e in SEMQ}
        self.last_cc = None
        self.last = {e: None for e in ENGS}
        self.known = {e: {} for e in ENGS}
        self.pe_unsig = []
        self.ndma = 0
        self.dma_recs = {q: [] for q in NSLOT}
        self.ntile = 0
        self.ninstr = {e: 0 for e in ENGS}
        self.nwait = 0

    def dram(self, name, shape, dtype, kind="Internal"):
        if kind == "Internal":
            self.ntile += 1
            name = f"{name}_i{self.ntile}"
        return self.nc.dram_tensor(name, list(shape), dtype, kind=kind).ap()

    def sbuf(self, stack, shape, dtype, name="sb"):
        self.ntile += 1
        return stack.enter_context(self.nc.sbuf_tensor(f"{name}_{self.ntile}", list(shape), dtype))

    def psum(self, stack, shape, dtype, name="ps"):
        self.ntile += 1
        return stack.enter_context(self.nc.psum_tensor(f"{name}_{self.ntile}", list(shape), dtype))

    def _sem_of(self, rec):
        if rec.is_dma:
            k = rec.dma_idx
            ns = NSLOT[rec.q]
            return ("d", rec.q, k % ns), k // ns + 1, self.slots[rec.q][k % ns], 16 * (k // ns + 1)
        if rec.cnt is None:
            raise RuntimeError("dependency on unsignaled PE instruction")
        c = rec.cnt - 1
        return ("c", rec.eng), rec.cnt, self.sems[rec.eng][c // SEM_ROT], (c % SEM_ROT) + 1

    def _wait(self, engname, rec):
        key, val, s, v = self._sem_of(rec)
        kn = self.known[engname]
        if kn.get(key, 0) >= val:
            return
        kn[key] = val
        self.E[engname].wait_ge(s, v)
        self.nwait += 1

    def _deps(self, rec, reads, writes):
        deps = []
        for d in reads:
            if d.w is not None:
                deps.append(d.w)
            if d.x:
                deps.extend(r for r in d.r if r.eng != rec.eng)
        for d in writes:
            if d.w is not None:
                deps.append(d.w)
            deps.extend(d.r)
        seen = set()
        for x in deps:
            if id(x) in seen:
                continue
            seen.add(id(x))
            if x.eng == "pe" and rec.eng == "pe" and not x.is_dma and not rec.is_dma:
                continue
            self._wait("pool" if rec.eng == "cc" else rec.eng, x)
        for d in reads:
            d.r.append(rec)
        for d in writes:
            d.w = rec
            d.r = []

    def op(self, eng, fn, reads=(), writes=(), sig=None):
        rec = Rec(eng)
        self._deps(rec, reads, writes)
        ins = fn(self.E[eng])
        self.ninstr[eng] += 1
        if sig is None:
            sig = eng != "pe"
        if sig:
            self.count[eng] += 1
            rec.cnt = self.count[eng]
            c = rec.cnt - 1
            ins.then_inc(self.sems[eng][c // SEM_ROT], 1)
            if eng == "pe":
                for r in self.pe_unsig:
                    r.cnt = rec.cnt
                self.pe_unsig = []
        else:
            self.pe_unsig.append(rec)
        self.last[eng] = rec
        return rec

    def dma(self, out, in_, reads=(), writes=(), eng="sp", **kw):
        rec = Rec(eng, is_dma=True)
        rec.q = eng
        ns = NSLOT[eng]
        rec.dma_idx = len(self.dma_recs[eng])
        self.ndma += 1
        if rec.dma_idx >= ns:
            self._wait(eng, self.dma_recs[eng][rec.dma_idx - ns])
        self._deps(rec, reads, writes)
        ins = self.E[eng].dma_start(out=out, in_=in_, **kw)
        ins.then_inc(self.slots[eng][rec.dma_idx % ns], 16)
        self.ninstr[eng] += 1
        self.dma_recs[eng].append(rec)
        return rec

    def collective(self, kind, groups, src, dst, reads=(), writes=()):
        rec = Rec("cc")
        self._deps(rec, reads, writes)
        ins = self.nc.gpsimd.collective_compute(kind, ALU.bypass, replica_groups=groups, ins=[src.opt()], outs=[dst.opt()])
        self.count["cc"] += 1
        rec.cnt = self.count["cc"]
        ins.then_inc(self.sems["cc"][0])
        self.ninstr["pool"] += 1
        self.last_cc = rec
        return rec

    def barrier(self):
        if self.pe_unsig:
            raise RuntimeError("barrier with unsignaled PE instrs")
        lasts = [self.last[e] for e in ENGS if self.last[e] is not None and e != "sp"]
        if self.last_cc is not None:
            lasts.append(self.last_cc)
        pend = [r for q in NSLOT for r in self.dma_recs[q][-NSLOT[q]:]]
        for e in ENGS:
            for x in lasts + pend:
                if (not x.is_dma) and x.eng == e and e == "pe":
                    continue
                self._wait(e, x)

    def finish(self):
        self.barrier()
        self.stack.close()
        return self.nc


class Ring:
    def __init__(self, P, stack, shape, dtype, n, name, psum=False):
        mk = P.psum if psum else P.sbuf
        self.tiles = [(mk(stack, shape, dtype, name), Dep(x=psum)) for _ in range(n)]
        self.i = 0

    def get(self):
        t = self.tiles[self.i % len(self.tiles)]
        self.i += 1
        return t


class Consts:
    pass


def load_consts(P, st, I):
    C = Consts()

    def ld(name, shape, dtype=F32, cast=False):
        t = P.sbuf(st, shape, BF16 if cast else dtype, name)
        d = Dep()
        P.dma(t[:], I[name], writes=[d], eng="pool" if cast else "sp")
        return t, d

    C.identf, C.d_identf = ld("ident", [128, 128])
    C.identb, C.d_identb = ld("ident", [128, 128], cast=True)
    C.onesb, C.d_onesb = ld("ones", [128, 128], cast=True)
    C.onesf, C.d_onesf = ld("ones", [128, 128])
    return C


def rms_fm(P, R, C, producers, gains, n, nfeat, outs, d_outs):
    ys = []
    ss, d_ss = R["aux"].get()
    k = len(producers)
    for i, prod in enumerate(producers):
        ps, d_ps = prod()
        rows = ps.shape[0]
        y, d_y = R["f32"].get()
        sq, d_sq = R["bf"].get()
        P.op("act", lambda e, y=y, ps=ps, rows=rows: e.copy(out=y[:rows, :n], in_=ps), reads=[d_ps], writes=[d_y])
        P.op("act", lambda e, sq=sq, ps=ps, rows=rows: e.activation(out=sq[:rows, :n], in_=ps, func=AF.Square),
             reads=[d_ps], writes=[d_sq])
        P.op("pe", lambda e, sq=sq, rows=rows, i=i: e.matmul(ss[:, :n], lhsT=C.onesb[:rows, :], rhs=sq[:rows, :n],
                                                            start=(i == 0), stop=(i == k - 1)),
             reads=[d_sq, C.d_onesb], writes=[d_ss], sig=(i == k - 1))
        ys.append((y, d_y, rows))
    rs, d_rs = R["f32"].get()
    P.op("act", lambda e: e.activation(out=rs[:, :n], in_=ss[:, :n], func=AF.Sqrt, scale=1.0 / nfeat, bias=EPS),
         reads=[d_ss], writes=[d_rs])
    P.op("dve", lambda e: e.reciprocal(out=rs[:, :n], in_=rs[:, :n]), reads=[d_rs], writes=[d_rs])
    for (y, d_y, rows), g, o, d_o in zip(ys, gains, outs, d_outs):
        P.op("dve", lambda e, y=y, g=g, o=o, rows=rows: e.scalar_tensor_tensor(
            out=o, in0=y[:rows, :n], scalar=g, in1=rs[:rows, :n], op0=ALU.mult, op1=ALU.mult),
            reads=[d_y, d_rs], writes=[d_o])


def gla_pass(P, st, C, R, src, direction, chunk_ids, state, d_state, state_bf, d_state_bf, emit=None, asum=None):
    tri = C.triF if direction == 0 else C.triB
    d_tri = C.d_tri
    for ci in chunk_ids:
        t0 = ci * 128
        la, d_la = R["la"].get()
        lk, d_lk = R["lk"].get()
        lv, d_lv = R["lv"].get()
        P.dma(la[:], src["la"][t0:t0 + 128, :], writes=[d_la])
        P.dma(lk[:], src["lk"][t0:t0 + 128, :], writes=[d_lk])
        P.dma(lv[:], src["lv"][t0:t0 + 128, :], writes=[d_lv])
        pb, d_pb = R["mm"].get()
        pe_, d_pe = R["mm"].get()
        P.op("pe", lambda e: e.matmul(pb[:, :512], lhsT=tri[:], rhs=la[:], start=True, stop=True),
             reads=[d_tri, d_la], writes=[d_pb], sig=True)
        P.op("pe", lambda e: e.matmul(pe_[:, :512], lhsT=C.onesf[:], rhs=la[:], start=True, stop=True),
             reads=[C.d_onesf, d_la], writes=[d_pe], sig=True)
        bsb, d_bsb = R["f32"].get()
        P.op("act", lambda e: e.copy(out=bsb[:, :512], in_=pb[:, :512]), reads=[d_pb], writes=[d_bsb])
        dif, d_dif = R["f32"].get()
        P.op("dve", lambda e: e.tensor_tensor(out=dif[:, :512], in0=pe_[:, :512], in1=bsb[:, :512], op=ALU.subtract),
             reads=[d_pe, d_bsb], writes=[d_dif])
        P.op("act", lambda e: e.activation(out=dif[:, :512], in_=dif[:, :512], func=AF.Exp), reads=[d_dif], writes=[d_dif])
        kh, d_kh = R["kh"].get()
        P.op("dve", lambda e: e.tensor_tensor(out=kh[:, :512], in0=lk[:], in1=dif[:, :512], op=ALU.mult),
             reads=[d_lk, d_dif], writes=[d_kh])
        pbe, d_pbe = R["aux"].get()
        for h in range(4):
            P.op("pe", lambda e, h=h: e.matmul(pbe[:, 4 * h:4 * h + 4], lhsT=la[:, h * 128:(h + 1) * 128], rhs=C.onesf[:, 0:4],
                                               start=True, stop=True),
                 reads=[d_la, C.d_onesf], writes=[d_pbe], sig=(h == 3))
        ebe, d_ebe = R["small"].get()
        P.op("act", lambda e: e.activation(out=ebe[:, 0:16], in_=pbe[:, 0:16], func=AF.Exp), reads=[d_pbe], writes=[d_ebe])
        if asum is not None:
            P.op("dve", lambda e: e.tensor_tensor(out=asum[0][:, 0:16], in0=asum[0][:, 0:16], in1=pbe[:, 0:16], op=ALU.add),
                 reads=[d_pbe, asum[1]], writes=[asum[1]])
        if emit is not None:
            lqT, lkT = emit["lqT"], emit["lkT"]
            for h in range(4):
                pbt, d_pbt = R["aux"].get()
                P.op("pe", lambda e, h=h: e.matmul(pbt[:, :128], lhsT=la[:, h * 128:(h + 1) * 128], rhs=tri[:],
                                                   start=True, stop=True),
                     reads=[d_la, d_tri], writes=[d_pbt], sig=True)
                eb, d_eb = R["f32"].get()
                enb, d_enb = R["f32"].get()
                P.op("act", lambda e: e.activation(out=eb[:, :128], in_=pbt[:, :128], func=AF.Exp), reads=[d_pbt], writes=[d_eb])
                P.op("act", lambda e: e.activation(out=enb[:, :128], in_=pbt[:, :128], func=AF.Exp, scale=-1.0),
                     reads=[d_pbt], writes=[d_enb])
                qt, d_qt = R["bf"].get()
                kt, d_kt = R["bf"].get()
                P.op("dve", lambda e, h=h: e.tensor_tensor(out=qt[:, :128], in0=lqT[:, h, t0:t0 + 128], in1=eb[:, :128], op=ALU.mult),
                     reads=[emit["d_lqT"], d_eb], writes=[d_qt])
                P.op("pool", lambda e, h=h: e.tensor_tensor(out=kt[:, :128], in0=lkT[:, h, t0:t0 + 128], in1=enb[:, :128], op=ALU.mult),
                     reads=[emit["d_lkT"], d_enb], writes=[d_kt])
                psc, d_psc = R["aux"].get()
                P.op("pe", lambda e: e.matmul(psc[:, :128], lhsT=kt[:, :128], rhs=qt[:, :128], start=True, stop=True),
                     reads=[d_kt, d_qt], writes=[d_psc], sig=True)
                scm, d_scm = R["bf"].get()
                P.op("dve", lambda e: e.tensor_tensor(out=scm[:, :128], in0=psc[:, :128], in1=tri[:], op=ALU.mult),
                     reads=[d_psc, d_tri], writes=[d_scm])
                po, d_po = R["mm"].get()
                for j in range(2):
                    P.op("pe", lambda e, h=h, j=j: e.matmul(po[:, j * 128:(j + 1) * 128],
                                                            lhsT=lv[:, h * 256 + j * 128:h * 256 + (j + 1) * 128],
                                                            rhs=scm[:, :128], start=True, stop=False),
                         reads=[d_lv, d_scm], writes=[d_po], sig=False)
                    P.op("pe", lambda e, h=h, j=j: e.matmul(po[:, j * 128:(j + 1) * 128],
                                                            lhsT=state_bf[h][:, j * 128:(j + 1) * 128],
                                                            rhs=qt[:, :128], start=False, stop=True),
                         reads=[d_state_bf[h], d_qt], writes=[d_po], sig=(j == 1))
                oT, d_oT = emit["oT"], emit["d_oT"][ci]
                if emit["first"]:
                    P.op("act", lambda e, h=h: e.copy(out=oT[:, 2 * h:2 * h + 2, t0:t0 + 128],
                                                     in_=po[:, 0:256].rearrange("p (j t) -> p j t", j=2)),
                         reads=[d_po], writes=[d_oT])
                else:
                    P.op("dve", lambda e, h=h: e.tensor_tensor(out=oT[:, 2 * h:2 * h + 2, t0:t0 + 128],
                                                               in0=oT[:, 2 * h:2 * h + 2, t0:t0 + 128],
                                                               in1=po[:, 0:256].rearrange("p (j t) -> p j t", j=2), op=ALU.add),
                         reads=[d_po, d_oT], writes=[d_oT])
        for h in range(4):
            pu, d_pu = R["aux"].get()
            P.op("pe", lambda e, h=h: e.matmul(pu[:, :256], lhsT=kh[:, h * 128:(h + 1) * 128], rhs=lv[:, h * 256:(h + 1) * 256],
                                               start=True, stop=True),
                 reads=[d_kh, d_lv], writes=[d_pu], sig=True)
            P.op("dve", lambda e, h=h: e.scalar_tensor_tensor(out=state[h][:], in0=state[h][:], scalar=ebe[:, 4 * h:4 * h + 1],
                                                              in1=pu[:, :256], op0=ALU.mult, op1=ALU.add),
                 reads=[d_pu, d_ebe, d_state[h]] + ([d_state_bf[h]] if state_bf is not None else []), writes=[d_state[h]])
            if state_bf is not None:
                P.op("act", lambda e, h=h: e.copy(out=state_bf[h][:], in_=state[h][:]), reads=[d_state[h]], writes=[d_state_bf[h]])


def mk_rings(P, st, n_mm=3, n_aux=2):
    R = {}
    R["mm"] = Ring(P, st, [128, 512], F32, n_mm, "psmm", psum=True)
    R["aux"] = Ring(P, st, [128, 512], F32, n_aux, "psaux", psum=True)
    R["f32"] = Ring(P, st, [128, 512], F32, 12, "rf32")
    R["bf"] = Ring(P, st, [128, 512], BF16, 8, "rbf")
    R["small"] = Ring(P, st, [128, 16], F32, 8, "rsm")
    R["kh"] = Ring(P, st, [128, 512], BF16, 2, "rkh")
    return R


S1_INPUTS = {
    "xin": ([T, D], F32), "cT": ([128, 16, 2], F32), "w_mod": ([D, 6 * D], F32), "bmodT": ([128, 96], F32),
    "bmod_g": ([2, 2, D], F32), "n1g": ([128, 16], F32), "w_in": ([D, IN_COLS], F32),
    "gq": ([128, 1], F32), "gk": ([128, 1], F32), "waf": ([17, 512], F32), "wab": ([17, 512], F32),
    "gmq": ([128, 4], F32), "gmkv": ([128, 4], F32), "wuq": ([512, 1536], F32), "wukv": ([512, 2048], F32),
    "cosA": ([128, NLAT], F32), "sinA": ([128, NLAT], F32), "cosM": ([64, NLAT], F32), "sinM": ([64, NLAT], F32),
    "ident": ([128, 128], F32), "ones": ([128, 128], F32), "rotA": ([128, 128], F32), "rotM": ([64, 64], F32),
    "triF": ([128, 128], F32), "triB": ([128, 128], F32),
}
S1_OUTPUTS = {
    "mfm": ([128, 96, 2], F32), "grow": ([2, 2, D], F32),
    "qTa": ([8, 128, T], BF16), "kTa": ([2, 128, T], BF16), "va": ([T, 256], BF16),
    "lqT": ([4, 128, T], BF16), "lkT": ([4, 128, T], BF16), "lk": ([T, 512], BF16), "lv": ([T, 1024], BF16),
    "lgT": ([1024, T], BF16), "laf": ([T, 512], F32), "lab": ([T, 512], F32),
    "mqT": ([8, 192, T], BF16), "mkT": ([8, 128, T], BF16), "mkrT": ([64, T], BF16), "vm": ([T, 1024], BF16),
    "gT": ([3, D, T], BF16),
    "glaA": ([2, 128, 16], F32), "glaU": ([2, 4, 128, 256], F32), "glaC": ([2, 4, 128, 256], F32),
}


def stage1(P, I, O):
    with ExitStack() as st:
        C = load_consts(P, st, I)
        C.triF = P.sbuf(st, [128, 128], F32, "triF")
        C.triB = P.sbuf(st, [128, 128], F32, "triB")
        C.d_tri = Dep()
        P.dma(C.triF[:], I["triF"], writes=[C.d_tri])
        P.dma(C.triB[:], I["triB"], writes=[C.d_tri])
        R = mk_rings(P, st)
        R["w"] = Ring(P, st, [128, 16, 512], BF16, 2, "wt")
        hT = P.sbuf(st, [128, 16, T], BF16, "hT")
        d_hT = [(Dep(), Dep()) for _ in range(18)]
        mfm = P.sbuf(st, [128, 96, 2], F32, "mfm")
        d_mfm = Dep()
        gm1 = P.sbuf(st, [128, 16, 2], F32, "gm1")
        d_gm1 = Dep()

        with ExitStack() as sa:
            cT = P.sbuf(sa, [128, 16, 2], F32, "cT")
            d_cT = Dep()
            scT = P.sbuf(sa, [128, 16, 2], BF16, "scT")
            d_scT = Dep()
            bmodT = P.sbuf(sa, [128, 96], F32, "bmodT")
            d_bm = Dep()
            bmg = P.sbuf(sa, [2, 2, D], F32, "bmg")
            d_bmg = Dep()
            grow = P.sbuf(sa, [2, 2, D], F32, "grow")
            d_grow = Dep()
            n1g = P.sbuf(sa, [128, 16], F32, "n1g")
            d_n1g = Dep()
            P.dma(cT[:], I["cT"], writes=[d_cT])
            P.dma(bmodT[:], I["bmodT"], writes=[d_bm])
            P.dma(bmg[:], I["bmod_g"], writes=[d_bmg])
            P.dma(n1g[:], I["n1g"], writes=[d_n1g])
            P.op("act", lambda e: e.activation(out=scT[:], in_=cT[:], func=AF.Silu), reads=[d_cT], writes=[d_scT])
            wmv = I["w_mod"].rearrange("(kc p) c -> p kc c", p=128)
            for g in range(24):
                wt, d_wt = R["w"].get()
                P.dma(wt[:], wmv[:, :, g * 512:(g + 1) * 512], writes=[d_wt], eng="pool")
                for j in range(4):
                    cc = g * 4 + j
                    ps, d_ps = R["aux"].get()
                    for kc in range(16):
                        P.op("pe", lambda e, kc=kc, j=j, wt=wt, ps=ps: e.matmul(
                            ps[:, 0:2], lhsT=wt[:, kc, j * 128:(j + 1) * 128], rhs=scT[:, kc, :],
                            start=(kc == 0), stop=(kc == 15)), reads=[d_wt, d_scT], writes=[d_ps], sig=(kc == 15))
                    P.op("dve", lambda e, cc=cc, ps=ps: e.tensor_scalar(out=mfm[:, cc, :], in0=ps[:, 0:2], scalar1=bmodT[:, cc:cc + 1],
                                                                       scalar2=None, op0=ALU.add),
                         reads=[d_ps, d_bm], writes=[d_mfm])
                gi = {2: 0, 5: 1}.get(g // 4)
                if gi is not None:
                    off = (g % 4) * 512
                    psr, d_psr = R["mm"].get()
                    for kc in range(16):
                        P.op("pe", lambda e, kc=kc, wt=wt, psr=psr: e.matmul(
                            psr[0:2, :], lhsT=scT[:, kc, :], rhs=wt[:, kc, :], start=(kc == 0), stop=(kc == 15)),
                            reads=[d_wt, d_scT], writes=[d_psr], sig=(kc == 15))
                    P.op("dve", lambda e, gi=gi, off=off, psr=psr: e.tensor_tensor(
                        out=grow[0:2, gi, off:off + 512], in0=psr[0:2, :], in1=bmg[0:2, gi, off:off + 512], op=ALU.add),
                        reads=[d_psr, d_bmg], writes=[d_grow])
            P.dma(O["mfm"], mfm[:], reads=[d_mfm])
            P.dma(O["grow"], grow[:], reads=[d_grow])
            P.op("dve", lambda e: e.tensor_scalar(out=gm1[:], in0=mfm[:, 16:32, :], scalar1=1.0, scalar2=None, op0=ALU.add),
                 reads=[d_mfm], writes=[d_gm1])
            P.op("dve", lambda e: e.tensor_tensor(out=gm1[:], in0=gm1[:], in1=n1g[:].unsqueeze(2).to_broadcast([128, 16, 2]),
                                                  op=ALU.mult), reads=[d_gm1, d_n1g], writes=[d_gm1])
            P.barrier()

        if STOP == "a":
            P.barrier()
            return
        norm_to_fm(P, st, C, I["xin"], hT, d_hT, gm1, d_gm1, mfm, d_mfm, 0)
        if STOP == "b":
            for kc in range(8):
                P.dma(O["lgT"][kc * 128:(kc + 1) * 128, :], hT[:, kc, :], reads=[x for p in d_hT for x in p])
            P.barrier()
            return

        w_in_v = I["w_in"].rearrange("(kc p) c -> p kc c", p=128)

        def hdeps(t0, n):
            out = []
            for ti in range(t0 // 128, (t0 + n) // 128):
                out.extend(d_hT[ti])
            return out

        def load_w(off, ncols):
            wt, d_wt = R["w"].get()
            P.dma(wt[:, :, :ncols], w_in_v[:, :, off:off + ncols], writes=[d_wt], eng="pool")
            return wt, d_wt

        def fm_chunk(wt, d_wt, c0, rows, blk):
            t0, n = blk
            ps, d_ps = R["mm"].get()
            for kc in range(16):
                P.op("pe", lambda e, kc=kc: e.matmul(ps[:rows, :n], lhsT=wt[:, kc, c0:c0 + rows], rhs=hT[:, kc, t0:t0 + n],
                                                     start=(kc == 0), stop=(kc == 15)),
                     reads=[d_wt] + hdeps(t0, n), writes=[d_ps], sig=(kc == 15))
            return ps[:rows, :n], d_ps

        def tm_tile(wt, d_wt, c0, ncols, ti):
            ps, d_ps = R["mm"].get()
            for kc in range(16):
                P.op("pe", lambda e, kc=kc: e.matmul(ps[:, :ncols], lhsT=hT[:, kc, ti * 128:(ti + 1) * 128],
                                                     rhs=wt[:, kc, c0:c0 + ncols], start=(kc == 0), stop=(kc == 15)),
                     reads=[d_wt] + list(d_hT[ti]), writes=[d_ps], sig=(kc == 15))
            return ps[:, :ncols], d_ps

        def store(dst, src, dep):
            P.dma(dst, src, reads=[dep])

        def evac_bf(ps, d_ps, rows, n, func=AF.Copy, scale=1.0, eng="act"):
            ob, d_ob = R["bf"].get()
            if eng == "act":
                P.op("act", lambda e: e.activation(out=ob[:rows, :n], in_=ps, func=func, scale=scale), reads=[d_ps], writes=[d_ob])
            else:
                P.op("dve", lambda e: e.tensor_copy(out=ob[:rows, :n], in_=ps), reads=[d_ps], writes=[d_ob])
            return ob, d_ob

        def simple_fm(off, ncols, dst_fn, func=AF.Copy, scale=1.0):
            wt, d_wt = load_w(off, ncols)
            for blk in BLKS:
                for c in range(ncols // 128):
                    ps, d_ps = fm_chunk(wt, d_wt, c * 128, 128, blk)
                    ob, d_ob = evac_bf(ps, d_ps, 128, blk[1], func, scale)
                    store(dst_fn(c, blk), ob[:, :blk[1]], d_ob)

        def simple_tm(off, ncols, dst, dcol, wt_pair=None):
            wt, d_wt = wt_pair or load_w(off, ncols)
            for ti in range(18):
                ps, d_ps = tm_tile(wt, d_wt, 0, ncols, ti)
                ob, d_ob = evac_bf(ps, d_ps, 128, ncols, eng="dve")
                store(dst[ti * 128:(ti + 1) * 128, dcol:dcol + ncols], ob[:, :ncols], d_ob)
            return wt, d_wt

        with ExitStack() as sc:
            def ldc(name, shape, cast=True):
                t = P.sbuf(sc, shape, BF16 if cast else F32, name)
                d = Dep()
                P.dma(t[:], I[name], writes=[d], eng="pool" if cast else "sp")
                return t, d
            cosA, d_cosA = ldc("cosA", [128, NLAT])
            sinA, d_sinA = ldc("sinA", [128, NLAT])
            cosM, d_cosM = ldc("cosM", [64, NLAT])
            sinM, d_sinM = ldc("sinM", [64, NLAT])
            rotA, d_rotA = ldc("rotA", [128, 128])
            rotM, d_rotM = ldc("rotM", [64, 64])
            gq, d_gq = ldc("gq", [128, 1], cast=False)
            gk, d_gk = ldc("gk", [128, 1], cast=False)
            gmq, d_gmq = ldc("gmq", [128, 4], cast=False)
            gmkv, d_gmkv = ldc("gmkv", [128, 4], cast=False)
            P.op("dve", lambda e: e.tensor_scalar(out=gq[:], in0=gq[:], scalar1=128 ** -0.5, scalar2=None, op0=ALU.mult),
                 reads=[d_gq], writes=[d_gq])

            def rope(o, d_o, rows, blk, rot, d_rot, cos, d_cos, sin, d_sin):
                t0, n = blk
                ob, d_ob = R["bf"].get()
                if t0 < NCTX:
                    P.op("act", lambda e: e.copy(out=ob[:rows, :n], in_=o[:rows, :n]), reads=[d_o], writes=[d_ob])
                    return ob, d_ob
                l0 = t0 - NCTX
                P.op("act", lambda e: e.copy(out=ob[:rows, :n], in_=o[:rows, :n]), reads=[d_o], writes=[d_ob])
                pr, d_pr = R["aux"].get()
                P.op("pe", lambda e: e.matmul(pr[:rows, :n], lhsT=rot[:rows, :rows], rhs=ob[:rows, :n], start=True, stop=True),
                     reads=[d_rot, d_ob], writes=[d_pr], sig=True)
                t2, d_t2 = R["f32"].get()
                P.op("dve", lambda e: e.tensor_tensor(out=t2[:rows, :n], in0=pr[:rows, :n], in1=sin[:rows, l0:l0 + n], op=ALU.mult),
                     reads=[d_pr, d_sin], writes=[d_t2])
                P.op("pool", lambda e: e.tensor_tensor(out=o[:rows, :n], in0=o[:rows, :n], in1=cos[:rows, l0:l0 + n], op=ALU.mult),
                     reads=[d_o, d_cos], writes=[d_o])
                ob2, d_ob2 = R["bf"].get()
                P.op("pool", lambda e: e.tensor_tensor(out=ob2[:rows, :n], in0=o[:rows, :n], in1=t2[:rows, :n], op=ALU.add),
                     reads=[d_o, d_t2], writes=[d_ob2])
                return ob2, d_ob2

            def gqa_heads(off, nheads, gain, d_gain, dst):
                for g0 in range(0, nheads, 4):
                    nh = min(4, nheads - g0)
                    wt, d_wt = load_w(off + g0 * 128, nh * 128)
                    for blk in BLKS:
                        for hh in range(nh):
                            o, d_o = R["f32"].get()
                            rms_fm(P, R, C, [lambda hh=hh: fm_chunk(wt, d_wt, hh * 128, 128, blk)], [gain[:, 0:1]], blk[1], 128,
                                   [o[:, :blk[1]]], [d_o])
                            ob, d_ob = rope(o, d_o, 128, blk, rotA, d_rotA, cosA, d_cosA, sinA, d_sinA)
                            store(dst[g0 + hh, :, blk[0]:blk[0] + blk[1]], ob[:, :blk[1]], d_ob)
            gqa_heads(O_AQ, 8, gq, d_gq, O["qTa"])
            wt, d_wt = load_w(O_AK, 512)
            for blk in BLKS:
                for hh in range(2):
                    o, d_o = R["f32"].get()
                    rms_fm(P, R, C, [lambda hh=hh: fm_chunk(wt, d_wt, hh * 128, 128, blk)], [gk[:, 0:1]], blk[1], 128,
                           [o[:, :blk[1]]], [d_o])
                    ob, d_ob = rope(o, d_o, 128, blk, rotA, d_rotA, cosA, d_cosA, sinA, d_sinA)
                    store(O["kTa"][hh, :, blk[0]:blk[0] + blk[1]], ob[:, :blk[1]], d_ob)
            for ti in range(18):
                ps, d_ps = tm_tile(wt, d_wt, 256, 256, ti)
                ob, d_ob = evac_bf(ps, d_ps, 128, 256, eng="dve")
                store(O["va"][ti * 128:(ti + 1) * 128, :], ob[:, :256], d_ob)
            if STOP == "c1":
                P.barrier()
                return
            simple_fm(O_LQ, 512, lambda c, blk: O["lqT"][c, :, blk[0]:blk[0] + blk[1]], scale=128 ** -0.5)
            wt, d_wt = load_w(O_LK, 512)
            for blk in BLKS:
                for c in range(4):
                    ps, d_ps = fm_chunk(wt, d_wt, c * 128, 128, blk)
                    ob, d_ob = evac_bf(ps, d_ps, 128, blk[1])
                    store(O["lkT"][c, :, blk[0]:blk[0] + blk[1]], ob[:, :blk[1]], d_ob)
            simple_tm(O_LK, 512, O["lk"], 0, wt_pair=(wt, d_wt))
            simple_tm(O_LV, 512, O["lv"], 0)
            simple_tm(O_LV + 512, 512, O["lv"], 512)
            simple_fm(O_LG, 512, lambda c, blk: O["lgT"][c * 128:(c + 1) * 128, blk[0]:blk[0] + blk[1]], func=AF.Silu)
            simple_fm(O_LG + 512, 512, lambda c, blk: O["lgT"][512 + c * 128:512 + (c + 1) * 128, blk[0]:blk[0] + blk[1]], func=AF.Silu)
            for g in range(12):
                simple_fm(O_GA + g * 512, 512,
                          lambda c, blk, g=g: O["gT"][g // 4, (g % 4) * 512 + c * 128:(g % 4) * 512 + (c + 1) * 128, blk[0]:blk[0] + blk[1]],
                          func=AF.Sigmoid)
            if STOP == "c2":
                P.barrier()
                return
            with ExitStack() as sl:
                lrA = [P.sbuf(sl, [32, T], F32, f"lrA{d}") for d in range(2)]
                d_lrA = [Dep(), Dep()]
                wa = [P.sbuf(sl, [17, 512], F32, f"wa{d}") for d in range(2)]
                d_wa = [Dep(), Dep()]
                for d in range(2):
                    P.op("pool", lambda e, d=d: e.memset(lrA[d][:], 1.0), writes=[d_lrA[d]])
                    P.dma(wa[d][:], I["waf" if d == 0 else "wab"], writes=[d_wa[d]])
                wt, d_wt = load_w(O_LRF, 32)
                for blk in BLKS:
                    for d in range(2):
                        ps, d_ps = fm_chunk(wt, d_wt, d * 16, 16, blk)
                        P.op("act", lambda e, d=d, ps=ps: e.copy(out=lrA[d][0:16, blk[0]:blk[0] + blk[1]], in_=ps),
                             reads=[d_ps], writes=[d_lrA[d]])
                for d in range(2):
                    for ti in range(18):
                        ps, d_ps = R["mm"].get()
                        P.op("pe", lambda e, d=d, ti=ti, ps=ps: e.matmul(ps[:, :512], lhsT=lrA[d][0:17, ti * 128:(ti + 1) * 128],
                                                                         rhs=wa[d][:], start=True, stop=True),
                             reads=[d_lrA[d], d_wa[d]], writes=[d_ps], sig=True)
                        ex, d_ex = R["f32"].get()
                        P.op("act", lambda e, ps=ps, ex=ex: e.activation(out=ex[:, :512], in_=ps[:, :512], func=AF.Exp, scale=-1.0),
                             reads=[d_ps], writes=[d_ex])
                        P.op("act", lambda e, ex=ex: e.activation(out=ex[:, :512], in_=ex[:, :512], func=AF.Ln, bias=1.0),
                             reads=[d_ex], writes=[d_ex])
                        P.op("pool", lambda e, ex=ex: e.tensor_scalar(out=ex[:, :512], in0=ex[:, :512], scalar1=-1.0 / 16.0, scalar2=None,
                                                                      op0=ALU.mult), reads=[d_ex], writes=[d_ex])
                        store(O["laf" if d == 0 else "lab"][ti * 128:(ti + 1) * 128, :], ex[:, :512], d_ex)
                P.barrier()
            if STOP == "c3":
                P.barrier()
                return
            with ExitStack() as sm:
                wuq = P.sbuf(sm, [128, 4, 1536], BF16, "wuq")
                wukv = P.sbuf(sm, [128, 4, 2048], BF16, "wukv")
                d_wuq, d_wukv = Dep(), Dep()
                P.dma(wuq[:], I["wuq"].rearrange("(kc p) c -> p kc c", p=128), writes=[d_wuq], eng="pool")
                P.dma(wukv[:], I["wukv"].rearrange("(kc p) c -> p kc c", p=128), writes=[d_wukv], eng="pool")
                RC = Ring(P, sm, [128, 4, 512], BF16, 2, "cn")
                wq_t, d_wq_t = load_w(O_MCQ, 512)
                wkv_t, d_wkv_t = load_w(O_MCKV, 512)
                for blk in BLKS:
                    t0, n = blk
                    cqn, d_cqn = RC.get()
                    rms_fm(P, R, C, [lambda c=c: fm_chunk(wq_t, d_wq_t, c * 128, 128, blk) for c in range(4)],
                           [gmq[:, c:c + 1] for c in range(4)], n, 512, [cqn[:, c, :n] for c in range(4)], [d_cqn] * 4)
                    for h in range(8):
                        ps, d_ps = R["mm"].get()
                        for kc in range(4):
                            P.op("pe", lambda e, kc=kc, h=h, ps=ps: e.matmul(ps[:, :n], lhsT=wuq[:, kc, h * 192:h * 192 + 128],
                                                                             rhs=cqn[:, kc, :n], start=(kc == 0), stop=(kc == 3)),
                                 reads=[d_wuq, d_cqn], writes=[d_ps], sig=(kc == 3))
                        ob, d_ob = evac_bf(ps[:, :n], d_ps, 128, n, scale=192 ** -0.5)
                        store(O["mqT"][h, 0:128, t0:t0 + n], ob[:, :n], d_ob)
                        ps2, d_ps2 = R["mm"].get()
                        for kc in range(4):
                            P.op("pe", lambda e, kc=kc, h=h, ps2=ps2: e.matmul(ps2[:64, :n], lhsT=wuq[:, kc, h * 192 + 128:h * 192 + 192],
                                                                               rhs=cqn[:, kc, :n], start=(kc == 0), stop=(kc == 3)),
                                 reads=[d_wuq, d_cqn], writes=[d_ps2], sig=(kc == 3))
                        o, d_o = R["f32"].get()
                        P.op("act", lambda e, o=o, ps2=ps2: e.activation(out=o[:64, :n], in_=ps2[:64, :n], func=AF.Copy, scale=192 ** -0.5),
                             reads=[d_ps2], writes=[d_o])
                        ob, d_ob = rope(o, d_o, 64, blk, rotM, d_rotM, cosM, d_cosM, sinM, d_sinM)
                        store(O["mqT"][h, 128:192, t0:t0 + n], ob[:64, :n], d_ob)
                    ckn, d_ckn = RC.get()
                    rms_fm(P, R, C, [lambda c=c: fm_chunk(wkv_t, d_wkv_t, c * 128, 128, blk) for c in range(4)],
                           [gmkv[:, c:c + 1] for c in range(4)], n, 512, [ckn[:, c, :n] for c in range(4)], [d_ckn] * 4)
                    for h in range(8):
                        ps, d_ps = R["mm"].get()
                        for kc in range(4):
                            P.op("pe", lambda e, kc=kc, h=h, ps=ps: e.matmul(ps[:, :n], lhsT=wukv[:, kc, h * 256:h * 256 + 128],
                                                                             rhs=ckn[:, kc, :n], start=(kc == 0), stop=(kc == 3)),
                                 reads=[d_wukv, d_ckn], writes=[d_ps], sig=(kc == 3))
                        ob, d_ob = evac_bf(ps[:, :n], d_ps, 128, n)
                        store(O["mkT"][h, :, t0:t0 + n], ob[:, :n], d_ob)
                    wv = wukv[:].rearrange("p k (h c) -> p k h c", c=256)
                    for tt in range(n // 128):
                        for half in range(2):
                            ps, d_ps = R["mm"].get()
                            for kc in range(4):
                                P.op("pe", lambda e, kc=kc, half=half, tt=tt, ps=ps: e.matmul(
                                    ps[:, :512].rearrange("p (h c) -> p h c", c=128), lhsT=ckn[:, kc, tt * 128:(tt + 1) * 128],
                                    rhs=wv[:, kc, half * 4:(half + 1) * 4, 128:256], start=(kc == 0), stop=(kc == 3)),
                                    reads=[d_wukv, d_ckn], writes=[d_ps], sig=(kc == 3))
                            ob, d_ob = evac_bf(ps[:, :512], d_ps, 128, 512, eng="dve")
                            store(O["vm"][t0 + tt * 128:t0 + (tt + 1) * 128, half * 512:(half + 1) * 512], ob[:, :512], d_ob)
                wt, d_wt = load_w(O_MKR, 64)
                for blk in BLKS:
                    ps, d_ps = fm_chunk(wt, d_wt, 0, 64, blk)
                    o, d_o = R["f32"].get()
                    P.op("act", lambda e, o=o, ps=ps: e.copy(out=o[:64, :blk[1]], in_=ps), reads=[d_ps], writes=[d_o])
                    ob, d_ob = rope(o, d_o, 64, blk, rotM, d_rotM, cosM, d_cosM, sinM, d_sinM)
                    store(O["mkrT"][:, blk[0]:blk[0] + blk[1]], ob[:64, :blk[1]], d_ob)
                P.barrier()
            P.barrier()
        P.barrier()

    if STOP == "c4":
        return
    with ExitStack() as st:
        C = load_consts(P, st, I)
        C.triF = P.sbuf(st, [128, 128], F32, "triF")
        C.triB = P.sbuf(st, [128, 128], F32, "triB")
        C.d_tri = Dep()
        P.dma(C.triF[:], I["triF"], writes=[C.d_tri])
        P.dma(C.triB[:], I["triB"], writes=[C.d_tri])
        R = mk_rings(P, st)
        R["la"] = Ring(P, st, [128, 512], F32, 2, "la")
        R["lk"] = Ring(P, st, [128, 512], BF16, 2, "lk")
        R["lv"] = Ring(P, st, [128, 1024], BF16, 2, "lv")
        state = [P.sbuf(st, [128, 256], F32, f"st{h}") for h in range(4)]
        d_state = [Dep() for _ in range(4)]
        asum = (P.sbuf(st, [128, 16], F32, "asum"), Dep())
        for d in range(2):
            src = {"la": O["laf" if d == 0 else "lab"], "lk": O["lk"], "lv": O["lv"]}
            for which in ("ctx", "lat"):
                for h in range(4):
                    P.op("pool", lambda e, h=h: e.memset(state[h][:], 0.0), writes=[d_state[h]])
                P.op("pool", lambda e: e.memset(asum[0][:], 0.0), writes=[asum[1]])
                ids = [0, 1] if which == "ctx" else list(range(2, 18))
                if d == 1:
                    ids = ids[::-1]
                gla_pass(P, st, C, R, src, d, ids, state, d_state, None, None, emit=None, asum=asum)
                dst = O["glaC"] if which == "ctx" else O["glaU"]
                for h in range(4):
                    P.dma(dst[d, h], state[h][:], reads=[d_state[h]])
                if which == "lat":
                    ea, d_ea = R["small"].get()
                    P.op("act", lambda e, ea=ea: e.activation(out=ea[:, 0:16], in_=asum[0][:, 0:16], func=AF.Exp),
                         reads=[asum[1]], writes=[d_ea])
                    P.dma(O["glaA"][d], ea[:, 0:16], reads=[d_ea])
        P.barrier()


def norm_to_fm(P, st, C, xsrc, hT, d_hT, gm, d_gm, mfm, d_mfm, shift_group):
    with ExitStack() as sb:
        RX = Ring(P, sb, [128, D], F32, 2, "x")
        RXN = Ring(P, sb, [128, D], BF16, 2, "xn")
        RT = Ring(P, sb, [128, 8, 128], BF16, 2, "tp", psum=True)
        RS = Ring(P, sb, [128, 8], F32, 4, "xs")
        for ti in range(18):
            r = 1 if ti < 2 else 0
            xt, d_xt = RX.get()
            P.dma(xt[:], xsrc[ti * 128:(ti + 1) * 128, :], writes=[d_xt])
            xn, d_xn = RXN.get()
            ssq, d_ssq = RS.get()
            P.op("act", lambda e: e.activation(out=xn[:], in_=xt[:], func=AF.Square, accum_out=ssq[:, 0:1]),
                 reads=[d_xt], writes=[d_xn, d_ssq])
            P.op("act", lambda e: e.activation(out=ssq[:, 1:2], in_=ssq[:, 0:1], func=AF.Sqrt, scale=1.0 / D, bias=EPS),
                 reads=[d_ssq], writes=[d_ssq])
            P.op("dve", lambda e: e.reciprocal(out=ssq[:, 2:3], in_=ssq[:, 1:2]), reads=[d_ssq], writes=[d_ssq])
            P.op("act", lambda e: e.activation(out=xn[:], in_=xt[:], func=AF.Copy, scale=ssq[:, 2:3]),
                 reads=[d_xt, d_ssq, d_xn], writes=[d_xn])
            tps = [RT.get(), RT.get()]
            for kc in range(16):
                tp, d_tp = tps[kc // 8]
                P.op("pe", lambda e, kc=kc: e.transpose(out=tp[:, kc % 8, :], in_=xn[:, kc * 128:(kc + 1) * 128], identity=C.identb[:]),
                     reads=[d_xn, C.d_identb], writes=[d_tp], sig=(kc % 8 == 7))
            for kc in range(16):
                tp, d_tp = tps[kc // 8]
                sl = hT[:, kc, ti * 128:(ti + 1) * 128]
                if kc < 8:
                    P.op("dve", lambda e, kc=kc, sl=sl: e.tensor_scalar(out=sl, in0=tp[:, kc % 8, :], scalar1=gm[:, kc, r:r + 1],
                                                                       scalar2=mfm[:, shift_group * 16 + kc, r:r + 1],
                                                                       op0=ALU.mult, op1=ALU.add),
                         reads=[d_tp, d_gm, d_mfm], writes=[d_hT[ti][0]])
                else:
                    P.op("act", lambda e, kc=kc, sl=sl: e.activation(out=sl, in_=tp[:, kc % 8, :], func=AF.Identity,
                                                                    scale=gm[:, kc, r:r + 1], bias=mfm[:, shift_group * 16 + kc, r:r + 1]),
                         reads=[d_tp, d_gm, d_mfm], writes=[d_hT[ti][1]])
        P.barrier()


def _fm16(v):
    return np.ascontiguousarray(np.asarray(v, np.float32).reshape(16, 128).T)


def _rope_tables(dim, pos0):
    quarter = dim // 4
    inv = (10000.0 ** (-np.arange(quarter, dtype=np.float32) / quarter)).astype(np.float32)
    t = np.arange(pos0, pos0 + NLAT)
    row = (t // 64).astype(np.float32)
    col = (t % 64).astype(np.float32)
    ang = np.concatenate([row[:, None] * inv, col[:, None] * inv], axis=-1)
    cos = np.cos(ang).astype(np.float32).T
    sin = np.sin(ang).astype(np.float32).T
    return (np.ascontiguousarray(np.concatenate([cos, cos], 0)), np.ascontiguousarray(np.concatenate([sin, sin], 0)))


def _rot_mat(dim):
    half = dim // 2
    L = np.zeros((dim, dim), np.float32)
    for m in range(half):
        L[m + half, m] = -1.0
        L[m, m + half] = 1.0
    return L


def consts_np():
    tri = np.triu(np.ones((128, 128), np.float32))
    return {"ident": np.eye(128, dtype=np.float32), "ones": np.ones((128, 128), np.float32),
            "rotA": _rot_mat(128), "rotM": _rot_mat(64), "triF": tri, "triB": np.ascontiguousarray(tri.T)}


def s1_inputs(core, layer, xs, ctxs, inp):
    b, q = core // 4, core % 4
    L = layer
    m = {}
    m["xin"] = np.ascontiguousarray(np.concatenate([ctxs[b], xs[b][q * NLAT:(q + 1) * NLAT]], 0))
    cc = np.stack([inp["c"][b], inp["c_ctx"]], -1)
    m["cT"] = np.ascontiguousarray(cc.reshape(16, 128, 2).transpose(1, 0, 2))
    m["w_mod"] = inp["w_mod"][L]
    bm = inp["b_mod"][L]
    m["bmodT"] = np.ascontiguousarray(bm.reshape(96, 128).T)
    g = np.stack([bm[2 * D:3 * D], bm[5 * D:6 * D]], 0)
    m["bmod_g"] = np.ascontiguousarray(np.stack([g, g], 0))
    m["n1g"] = _fm16(inp["norm1_g"][L])
    m["w_in"] = inp["w_in"][L]
    m["gq"] = np.ascontiguousarray(inp["gqa_qn_g"][L].reshape(128, 1))
    m["gk"] = np.ascontiguousarray(inp["gqa_kn_g"][L].reshape(128, 1))
    m["waf"] = np.ascontiguousarray(np.concatenate([inp["gla_wa2_f"][L], inp["gla_ba_f"][L][None]], 0))
    m["wab"] = np.ascontiguousarray(np.concatenate([inp["gla_wa2_b"][L], inp["gla_ba_b"][L][None]], 0))
    m["gmq"] = np.ascontiguousarray(inp["mla_qn_g"][L].reshape(4, 128).T)
    m["gmkv"] = np.ascontiguousarray(inp["mla_kvn_g"][L].reshape(4, 128).T)
    m["wuq"] = inp["mla_wuq"][L]
    m["wukv"] = inp["mla_wukv"][L]
    m["cosA"], m["sinA"] = _rope_tables(128, q * NLAT)
    m["cosM"], m["sinM"] = _rope_tables(64, q * NLAT)
    m.update(consts_np())
    return m


def build_s1():
    P = Prog()
    I = {k: P.dram(k, sh, dt, kind="ExternalInput") for k, (sh, dt) in S1_INPUTS.items()}
    O = {k: P.dram(k, sh, dt, kind="ExternalOutput") for k, (sh, dt) in S1_OUTPUTS.items()}
    stage1(P, I, O)
    return P.finish(), P


NK = NCTX + 4 * NLAT
NKT = NK // 128
NEXP = 16384

S2_INPUTS = {
    "xin": ([T, D], F32), "mfm": ([128, 96, 2], F32), "grow": ([2, 2, D], F32),
    "qTa": ([8, 128, T], BF16), "mqT": ([8, 192, T], BF16),
    "lqT": ([4, 128, T], BF16), "lkT": ([4, 128, T], BF16), "lk": ([T, 512], BF16), "lv": ([T, 1024], BF16),
    "lgT": ([1024, T], BF16), "laf": ([T, 512], F32), "lab": ([T, 512], F32), "gT": ([3, D, T], BF16),
    "kTa_all": ([2, 128, NK], BF16), "va_all": ([NK, 256], BF16),
    "mkT_all": ([8, 128, NK], BF16), "mkrT_all": ([64, NK], BF16), "vm_all": ([NK, 1024], BF16),
    "glaC": ([2, 4, 128, 256], F32), "glaA3": ([2, 3, 128, 4], F32), "glaU3": ([2, 3, 4, 128, 256], F32),
    "w_br": ([3, 1024, D], F32), "w_out": ([D, D], F32), "n2g": ([128, 16], F32), "gon": ([128, 2], F32),
    "peer_wq": ([D, D], F32), "k12T": ([16, 128, 128], F32), "peer_uT": ([D, NEXP], F32), "peer_v": ([NEXP, D], F32),
    "fing": ([1, D], F32), "sel": ([2, 2, 128], F32),
    "ident": ([128, 128], F32), "ones": ([128, 128], F32), "triF": ([128, 128], F32), "triB": ([128, 128], F32),
}
S2_OUTPUTS = {"xout": ([T, D], F32)}

QBLKS = [(0, 256, 2)] + [(256 + i * 512, 512, NKT) for i in range(4)]


def attention(P, I, oaT, omT):
    with ExitStack() as st:
        C = load_consts(P, st, I)
        RS = Ring(P, st, [128, 512], F32, 3, "ps_s", psum=True)
        RO = Ring(P, st, [128, 512], F32, 2, "ps_o", psum=True)
        RD = Ring(P, st, [128, 512], F32, 2, "ps_d", psum=True)
        RP = Ring(P, st, [128, 512], BF16, 4, "pT")
        RQ = Ring(P, st, [128, 512], BF16, 3, "qT")
        RQ2 = Ring(P, st, [64, 512], BF16, 3, "qT2")
        RF = Ring(P, st, [128, 512], F32, 3, "rden")
        ROB = Ring(P, st, [128, 512], BF16, 3, "ob")
        RK = Ring(P, st, [128, NK], BF16, 2, "kT")
        RV = Ring(P, st, [128, NKT, 128], BF16, 2, "V")
        kr = P.sbuf(st, [64, NK], BF16, "krope")
        d_kr = Dep()
        P.dma(kr[:], I["mkrT_all"], writes=[d_kr])

        def run_head(kT, d_kT, V, d_V, qsrc, dst, rope_q=None):
            for (t0, n, nkt) in QBLKS:
                q, d_q = RQ.get()
                P.dma(q[:, :n], qsrc[0:128, t0:t0 + n], writes=[d_q])
                if rope_q is not None:
                    q2, d_q2 = RQ2.get()
                    P.dma(q2[:, :n], qsrc[128:192, t0:t0 + n], writes=[d_q2])
                po, d_po = RO.get()
                pd, d_pd = RD.get()

                def qk(kt):
                    ps, d_ps = RS.get()
                    if rope_q is None:
                        P.op("pe", lambda e: e.matmul(ps[:, :n], lhsT=kT[:, kt * 128:(kt + 1) * 128], rhs=q[:, :n], start=True, stop=True),
                             reads=[d_kT, d_q], writes=[d_ps], sig=True)
                    else:
                        P.op("pe", lambda e: e.matmul(ps[:, :n], lhsT=kT[:, kt * 128:(kt + 1) * 128], rhs=q[:, :n], start=True, stop=False),
                             reads=[d_kT, d_q], writes=[d_ps], sig=False)
                        P.op("pe", lambda e: e.matmul(ps[:, :n], lhsT=kr[:, kt * 128:(kt + 1) * 128], rhs=q2[:, :n], start=False, stop=True),
                             reads=[d_kr, d_q2], writes=[d_ps], sig=True)
                    return ps, d_ps
                nxt = qk(0)
                for kt in range(nkt):
                    ps, d_ps = nxt
                    if kt + 1 < nkt:
                        nxt = qk(kt + 1)
                    pT, d_pT = RP.get()
                    P.op("act", lambda e: e.activation(out=pT[:, :n], in_=ps[:, :n], func=AF.Exp), reads=[d_ps], writes=[d_pT])
                    P.op("pe", lambda e: e.matmul(po[:, :n], lhsT=V[:, kt, :], rhs=pT[:, :n], start=(kt == 0), stop=(kt == nkt - 1)),
                         reads=[d_V, d_pT], writes=[d_po], sig=False)
                    P.op("pe", lambda e: e.matmul(pd[:, :n], lhsT=C.onesb[:], rhs=pT[:, :n], start=(kt == 0), stop=(kt == nkt - 1)),
                         reads=[C.d_onesb, d_pT], writes=[d_pd], sig=True)
                rd, d_rd = RF.get()
                P.op("dve", lambda e: e.reciprocal(out=rd[:, :n], in_=pd[:, :n]), reads=[d_pd], writes=[d_rd])
                ob, d_ob = ROB.get()
                P.op("dve", lambda e: e.tensor_tensor(out=ob[:, :n], in0=po[:, :n], in1=rd[:, :n], op=ALU.mult),
                     reads=[d_po, d_rd], writes=[d_ob])
                P.dma(dst[:, t0:t0 + n], ob[:, :n], reads=[d_ob])

        for kvh in range(2):
            kT, d_kT = RK.get()
            V, d_V = RV.get()
            P.dma(kT[:], I["kTa_all"][kvh], writes=[d_kT])
            P.dma(V[:], I["va_all"].rearrange("(kt p) c -> p kt c", p=128)[:, :, kvh * 128:(kvh + 1) * 128], writes=[d_V])
            for g in range(4):
                h = kvh * 4 + g
                run_head(kT, d_kT, V, d_V, I["qTa"][h], oaT[h])
        for h in range(8):
            kT, d_kT = RK.get()
            V, d_V = RV.get()
            P.dma(kT[:], I["mkT_all"][h], writes=[d_kT])
            P.dma(V[:], I["vm_all"].rearrange("(kt p) c -> p kt c", p=128)[:, :, h * 128:(h + 1) * 128], writes=[d_V])
            run_head(kT, d_kT, V, d_V, I["mqT"][h], omT[h], rope_q=True)
        P.barrier()


def load_tri(P, st, C, I):
    C.triF = P.sbuf(st, [128, 128], F32, "triF")
    C.triB = P.sbuf(st, [128, 128], F32, "triB")
    C.d_tri = Dep()
    P.dma(C.triF[:], I["triF"], writes=[C.d_tri])
    P.dma(C.triB[:], I["triB"], writes=[C.d_tri])


def gla_stage2(P, I, olT):
    with ExitStack() as st:
        C = load_consts(P, st, I)
        load_tri(P, st, C, I)
        R = mk_rings(P, st)
        R["la"] = Ring(P, st, [128, 512], F32, 2, "la")
        R["lk"] = Ring(P, st, [128, 512], BF16, 2, "lk")
        R["lv"] = Ring(P, st, [128, 1024], BF16, 2, "lv")
        lqT = P.sbuf(st, [128, 4, T], BF16, "lqT")
        lkT = P.sbuf(st, [128, 4, T], BF16, "lkT")
        d_lqT, d_lkT = Dep(), Dep()
        P.dma(lqT[:], I["lqT"].rearrange("h p t -> p h t"), writes=[d_lqT])
        P.dma(lkT[:], I["lkT"].rearrange("h p t -> p h t"), writes=[d_lkT])
        oT = P.sbuf(st, [128, 8, T], F32, "oT")
        d_oT = [Dep() for _ in range(18)]
        state = [P.sbuf(st, [128, 256], F32, f"st{h}") for h in range(4)]
        state_bf = [P.sbuf(st, [128, 256], BF16, f"stb{h}") for h in range(4)]
        d_state = [Dep() for _ in range(4)]
        d_state_bf = [Dep() for _ in range(4)]
        gon = P.sbuf(st, [128, 2], F32, "gon")
        d_gon = Dep()
        P.dma(gon[:], I["gon"], writes=[d_gon])
        RA = Ring(P, st, [128, 4], F32, 2, "trA")
        RA16 = Ring(P, st, [128, 16], F32, 2, "trA16")
        RU = Ring(P, st, [128, 256], F32, 3, "trU")
        if "recvF" in I:
            qmt = P.sbuf(st, [128, 16], F32, "qmask")
            d_qmt = Dep()
            P.dma(qmt[:], I["qmask"], writes=[d_qmt])
            I = dict(I)
            I["qmask_sb"] = (qmt, d_qmt)
        for d in range(2):
            src = {"la": I["laf" if d == 0 else "lab"], "lk": I["lk"], "lv": I["lv"]}
            emit = dict(oT=oT, d_oT=d_oT, first=(d == 0), lqT=lqT, lkT=lkT, d_lqT=d_lqT, d_lkT=d_lkT)
            for h in range(4):
                P.op("pool", lambda e, h=h: e.memset(state[h][:], 0.0), writes=[d_state[h]])
                P.op("pool", lambda e, h=h: e.memset(state_bf[h][:], 0.0), writes=[d_state_bf[h]])
            ids = [0, 1] if d == 0 else [1, 0]
            gla_pass(P, st, C, R, src, d, ids, state, d_state, state_bf, d_state_bf, emit=emit)
            if "recvF" in I:
                qm = I["qmask_sb"]
                order = range(4) if d == 0 else range(3, -1, -1)
                for j in order:
                    A, d_A = RA16.get()
                    P.dma(A[:], I["recvF"][16, j * 128:(j + 1) * 128, d * 16:(d + 1) * 16], writes=[d_A])
                    mcol = d * 4 + j
                    P.op("dve", lambda e, A=A: e.tensor_scalar(out=A[:], in0=A[:], scalar1=qm[0][:, mcol:mcol + 1], scalar2=qm[0][:, 8 + mcol:9 + mcol],
                                                               op0=ALU.mult, op1=ALU.add), reads=[d_A, qm[1]], writes=[d_A])
                    for h in range(4):
                        U, d_U = RU.get()
                        for f in range(2):
                            P.dma(U[:, f * 128:(f + 1) * 128], I["recvF"][(d * 4 + h) * 2 + f, j * 128:(j + 1) * 128, :], writes=[d_U])
                        P.op("pool", lambda e, U=U: e.tensor_scalar(out=U[:], in0=U[:], scalar1=qm[0][:, mcol:mcol + 1], scalar2=None, op0=ALU.mult),
                             reads=[d_U, qm[1]], writes=[d_U])
                        P.op("dve", lambda e, h=h, A=A, U=U: e.scalar_tensor_tensor(out=state[h][:], in0=state[h][:], scalar=A[:, 4 * h:4 * h + 1],
                                                                                    in1=U[:], op0=ALU.mult, op1=ALU.add),
                             reads=[d_A, d_U, d_state[h]], writes=[d_state[h]])
            for k in (range(3) if "recvF" not in I else ()):
                A, d_A = RA.get()
                P.dma(A[:], I["glaA3"][d, k], writes=[d_A])
                for h in range(4):
                    U, d_U = RU.get()
                    P.dma(U[:], I["glaU3"][d, k, h], writes=[d_U])
                    P.op("dve", lambda e, h=h, A=A, U=U: e.scalar_tensor_tensor(out=state[h][:], in0=state[h][:], scalar=A[:, h:h + 1],
                                                                                in1=U[:], op0=ALU.mult, op1=ALU.add),
                         reads=[d_A, d_U, d_state[h]], writes=[d_state[h]])
            for h in range(4):
                P.op("act", lambda e, h=h: e.copy(out=state_bf[h][:], in_=state[h][:]), reads=[d_state[h]], writes=[d_state_bf[h]])
            ids = list(range(2, 18)) if d == 0 else list(range(17, 1, -1))
            gla_pass(P, st, C, R, src, d, ids, state, d_state, state_bf, d_state_bf, emit=emit)
        RG = Ring(P, st, [128, 512], BF16, 3, "lg")
        for (t0, n) in BLKS:
            deps = [d_oT[ti] for ti in range(t0 // 128, (t0 + n) // 128)]
            dd = Dep()
            for h in range(4):
                outs = [R["f32"].get() for _ in range(2)]
                prods = []
                for j in range(2):
                    def prod(j=j, h=h):
                        d0 = Dep()
                        return oT[:, 2 * h + j, t0:t0 + n], deps
                    prods.append(prod)
                rms_fm_multi(P, R, C, prods, [gon[:, j:j + 1] for j in range(2)], n, 256,
                             [outs[j][0][:, :n] for j in range(2)], [outs[j][1] for j in range(2)])
                for j in range(2):
                    lg, d_lg = RG.get()
                    c = 2 * h + j
                    P.dma(lg[:, :n], I["lgT"][c * 128:(c + 1) * 128, t0:t0 + n], writes=[d_lg])
                    ob, d_ob = R["bf"].get()
                    P.op("dve", lambda e, j=j, lg=lg, ob=ob: e.tensor_tensor(out=ob[:, :n], in0=outs[j][0][:, :n], in1=lg[:, :n], op=ALU.mult),
                         reads=[outs[j][1], d_lg], writes=[d_ob])
                    P.dma(olT[c * 128:(c + 1) * 128, t0:t0 + n], ob[:, :n], reads=[d_ob])
        P.barrier()


def rms_fm_multi(P, R, C, producers, gains, n, nfeat, outs, d_outs):
    ys = []
    ss, d_ss = R["aux"].get()
    k = len(producers)
    for i, prod in enumerate(producers):
        src, deps = prod()
        rows = src.shape[0]
        sq, d_sq = R["bf"].get()
        P.op("act", lambda e: e.activation(out=sq[:rows, :n], in_=src, func=AF.Square), reads=list(deps), writes=[d_sq])
        P.op("pe", lambda e: e.matmul(ss[:, :n], lhsT=C.onesb[:rows, :], rhs=sq[:rows, :n], start=(i == 0), stop=(i == k - 1)),
             reads=[d_sq, C.d_onesb], writes=[d_ss], sig=(i == k - 1))
        ys.append((src, deps, rows))
    rs, d_rs = R["f32"].get()
    P.op("act", lambda e: e.activation(out=rs[:, :n], in_=ss[:, :n], func=AF.Sqrt, scale=1.0 / nfeat, bias=EPS),
         reads=[d_ss], writes=[d_rs])
    P.op("dve", lambda e: e.reciprocal(out=rs[:, :n], in_=rs[:, :n]), reads=[d_rs], writes=[d_rs])
    for (src, deps, rows), g, o, d_o in zip(ys, gains, outs, d_outs):
        P.op("dve", lambda e: e.scalar_tensor_tensor(out=o, in0=src, scalar=g, in1=rs[:rows, :n], op0=ALU.mult, op1=ALU.mult),
             reads=list(deps) + [d_rs], writes=[d_o])


def merge_stage(P, I, oaT, olT, omT, yT):
    with ExitStack() as st:
        R = mk_rings(P, st)
        acts = [P.sbuf(st, [128, 8, T], BF16, f"act{b}") for b in range(3)]
        d_acts = [Dep() for _ in range(3)]
        P.dma(acts[0][:], oaT.rearrange("h p t -> p h t"), writes=[d_acts[0]])
        P.dma(acts[1][:], olT.rearrange("(c p) t -> p c t", p=128), writes=[d_acts[1]])
        P.dma(acts[2][:], omT.rearrange("h p t -> p h t"), writes=[d_acts[2]])
        RW = Ring(P, st, [128, 8, 128], BF16, 6, "wbr")
        RG = Ring(P, st, [128, 512], BF16, 4, "gate")
        for nch in range(16):
            wb = []
            for br in range(3):
                w, d_w = RW.get()
                P.dma(w[:], I["w_br"][br].rearrange("(kc p) n -> p kc n", p=128)[:, :, nch * 128:(nch + 1) * 128], writes=[d_w], eng="pool")
                wb.append((w, d_w))
            for (t0, n) in BLKS:
                acc, d_acc = R["f32"].get()
                for br in range(3):
                    ps, d_ps = R["mm"].get()
                    for kc in range(8):
                        P.op("pe", lambda e, kc=kc: e.matmul(ps[:, :n], lhsT=wb[br][0][:, kc, :], rhs=acts[br][:, kc, t0:t0 + n],
                                                             start=(kc == 0), stop=(kc == 7)),
                             reads=[wb[br][1], d_acts[br]], writes=[d_ps], sig=(kc == 7))
                    g, d_g = RG.get()
                    P.dma(g[:, :n], I["gT"][br, nch * 128:(nch + 1) * 128, t0:t0 + n], writes=[d_g])
                    if br == 0:
                        P.op("dve", lambda e: e.tensor_tensor(out=acc[:, :n], in0=ps[:, :n], in1=g[:, :n], op=ALU.mult),
                             reads=[d_ps, d_g], writes=[d_acc])
                    else:
                        tmp, d_tmp = R["f32"].get()
                        P.op("dve", lambda e: e.tensor_tensor(out=tmp[:, :n], in0=ps[:, :n], in1=g[:, :n], op=ALU.mult),
                             reads=[d_ps, d_g], writes=[d_tmp])
                        P.op("pool", lambda e: e.tensor_tensor(out=acc[:, :n], in0=acc[:, :n], in1=tmp[:, :n], op=ALU.add),
                             reads=[d_tmp, d_acc], writes=[d_acc])
                ob, d_ob = R["bf"].get()
                P.op("act", lambda e: e.copy(out=ob[:, :n], in_=acc[:, :n]), reads=[d_acc], writes=[d_ob])
                P.dma(yT[nch, :, t0:t0 + n], ob[:, :n], reads=[d_ob])
        P.barrier()


def bcast_rows(P, R, sel, d_sel, rowsrc, d_rowsrc, gi, dst, d_dst):
    for r in range(2):
        for cb in range(4):
            ps, d_ps = R["aux"].get()
            P.op("pe", lambda e: e.matmul(ps[:, :512], lhsT=sel[0:2, r, :], rhs=rowsrc[0:2, gi, cb * 512:(cb + 1) * 512], start=True, stop=True),
                 reads=[d_sel, d_rowsrc], writes=[d_ps], sig=True)
            P.op("act", lambda e: e.copy(out=dst[r][:, cb * 512:(cb + 1) * 512], in_=ps[:, :512]), reads=[d_ps], writes=[d_dst[r]])


def wout_stage(P, I, yT, x1):
    with ExitStack() as st:
        R = mk_rings(P, st)
        wo = P.sbuf(st, [128, 16, D], BF16, "wo")
        d_wo = Dep()
        wv = I["w_out"].rearrange("(kc p) n -> p kc n", p=128)
        for q4 in range(4):
            P.dma(wo[:, q4 * 4:(q4 + 1) * 4, :], wv[:, q4 * 4:(q4 + 1) * 4, :], writes=[d_wo], eng="pool")
        sel = P.sbuf(st, [2, 2, 128], F32, "sel")
        grow = P.sbuf(st, [2, 2, D], F32, "grow")
        d_sel, d_grow = Dep(), Dep()
        P.dma(sel[:], I["sel"], writes=[d_sel])
        P.dma(grow[:], I["grow"], writes=[d_grow])
        gbc = [P.sbuf(st, [128, D], F32, f"g1bc{r}") for r in range(2)]
        d_gbc = [Dep(), Dep()]
        bcast_rows(P, R, sel, d_sel, grow, d_grow, 0, gbc, d_gbc)
        RX = Ring(P, st, [128, D], F32, 2, "x")
        RY = Ring(P, st, [128, 16, 128], BF16, 2, "yTt")
        for ti in range(18):
            r = 1 if ti < 2 else 0
            xt, d_xt = RX.get()
            P.dma(xt[:], I["xin"][ti * 128:(ti + 1) * 128, :], writes=[d_xt])
            yt, d_yt = RY.get()
            P.dma(yt[:], yT[:, :, ti * 128:(ti + 1) * 128].rearrange("c p t -> p c t"), writes=[d_yt])
            for cb in range(4):
                ps, d_ps = R["mm"].get()
                for kc in range(16):
                    P.op("pe", lambda e, kc=kc: e.matmul(ps[:, :512], lhsT=yt[:, kc, :], rhs=wo[:, kc, cb * 512:(cb + 1) * 512],
                                                         start=(kc == 0), stop=(kc == 15)),
                         reads=[d_yt, d_wo], writes=[d_ps], sig=(kc == 15))
                t, d_t = R["f32"].get()
                P.op("dve", lambda e: e.tensor_tensor(out=t[:, :512], in0=ps[:, :512], in1=gbc[r][:, cb * 512:(cb + 1) * 512], op=ALU.mult),
                     reads=[d_ps, d_gbc[r]], writes=[d_t])
                P.op("pool", lambda e: e.tensor_tensor(out=xt[:, cb * 512:(cb + 1) * 512], in0=xt[:, cb * 512:(cb + 1) * 512],
                                                       in1=t[:, :512], op=ALU.add), reads=[d_t, d_xt], writes=[d_xt])
            P.dma(x1[ti * 128:(ti + 1) * 128, :], xt[:], reads=[d_xt])
        P.barrier()


def peer_stage(P, I, x1, xout, last):
    S = P.dram("peerS", [T, 16, 128], F32)
    AB = P.dram("peerAB", [T, 2, 8, 128], F32)
    ZI = P.dram("peerZI", [T, 8], F32)
    h2T = P.dram("h2T", [16, 128, T], BF16)
    ubf, vbf = I["ubf"], I["vbf"]
    with ExitStack() as st:
        C = load_consts(P, st, I)
        hT = P.sbuf(st, [128, 16, T], BF16, "h2Ts")
        d_hT = [(Dep(), Dep()) for _ in range(18)]
        mfm = P.sbuf(st, [128, 96, 2], F32, "mfm")
        n2g = P.sbuf(st, [128, 16], F32, "n2g")
        gm2 = P.sbuf(st, [128, 16, 2], F32, "gm2")
        d_mfm, d_n2g, d_gm2 = Dep(), Dep(), Dep()
        P.dma(mfm[:], I["mfm"], writes=[d_mfm])
        P.dma(n2g[:], I["n2g"], writes=[d_n2g])
        P.op("dve", lambda e: e.tensor_scalar(out=gm2[:], in0=mfm[:, 64:80, :], scalar1=1.0, scalar2=None, op0=ALU.add),
             reads=[d_mfm], writes=[d_gm2])
        P.op("dve", lambda e: e.tensor_tensor(out=gm2[:], in0=gm2[:], in1=n2g[:].unsqueeze(2).to_broadcast([128, 16, 2]), op=ALU.mult),
             reads=[d_gm2, d_n2g], writes=[d_gm2])
        norm_to_fm(P, st, C, x1, hT, d_hT, gm2, d_gm2, mfm, d_mfm, 3)
        for kc in range(16):
            P.dma(h2T[kc], hT[:, kc, :], reads=[x for p in d_hT for x in p])
        P.barrier()
    with ExitStack() as st:
        R = mk_rings(P, st)
        kT = P.sbuf(st, [128, 16, 128], F32, "k12T")
        d_kT = Dep()
        P.dma(kT[:], I["k12T"].rearrange("c d n -> d c n"), writes=[d_kT])
        wq = P.sbuf(st, [128, 16, D], BF16, "wq")
        d_wq = Dep()
        wv = I["peer_wq"].rearrange("(kc p) n -> p kc n", p=128)
        for q4 in range(4):
            P.dma(wq[:, q4 * 4:(q4 + 1) * 4, :], wv[:, q4 * 4:(q4 + 1) * 4, :], writes=[d_wq], eng="pool")
        RH = Ring(P, st, [128, 16, 512], BF16, 2, "h2blk")
        RSC = Ring(P, st, [128, 4, 16, 128], F32, 1, "sc")
        for (t0, n) in BLKS:
            hb, d_hb = RH.get()
            P.dma(hb[:, :, :n], h2T[:, :, t0:t0 + n].rearrange("c p t -> p c t"), writes=[d_hb])
            sc, d_sc = RSC.get()
            for c in range(16):
                ps, d_ps = R["mm"].get()
                for kc in range(16):
                    P.op("pe", lambda e, kc=kc: e.matmul(ps[:, :n], lhsT=wq[:, kc, c * 128:(c + 1) * 128], rhs=hb[:, kc, :n],
                                                         start=(kc == 0), stop=(kc == 15)),
                         reads=[d_wq, d_hb], writes=[d_ps], sig=(kc == 15))
                qc, d_qc = R["f32"].get()
                P.op("act", lambda e: e.copy(out=qc[:, :n], in_=ps[:, :n]), reads=[d_ps], writes=[d_qc])
                for tt in range(n // 128):
                    ps2, d_ps2 = R["aux"].get()
                    P.op("pe", lambda e: e.matmul(ps2[:, :128], lhsT=qc[:, tt * 128:(tt + 1) * 128], rhs=kT[:, c, :], start=True, stop=True),
                         reads=[d_qc, d_kT], writes=[d_ps2], sig=True)
                    P.op("dve", lambda e: e.tensor_copy(out=sc[:, tt, c, :], in_=ps2[:, :128]), reads=[d_ps2], writes=[d_sc])
            for tt in range(n // 128):
                P.dma(S[t0 + tt * 128:t0 + (tt + 1) * 128], sc[:, tt], reads=[d_sc])
        P.barrier()
    NEG = -1.0e30
    with ExitStack() as st:
        RSC = Ring(P, st, [128, 16, 128], F32, 2, "sct")
        RAB = Ring(P, st, [128, 2, 8, 128], F32, 2, "abt")
        RZ = Ring(P, st, [128, 8], F32, 2, "zit")
        RW = Ring(P, st, [128, 256], F32, 4, "wk")
        RV = Ring(P, st, [128, 2, 16], F32, 2, "v12")
        RM = Ring(P, st, [128, 32], F32, 4, "m8")
        for ti in range(18):
            sc, d_sc = RSC.get()
            P.dma(sc[:], S[ti * 128:(ti + 1) * 128], writes=[d_sc])
            ab, d_ab = RAB.get()
            zi, d_zi = RZ.get()
            for h in range(8):
                v12, d_v = RV.get()
                for half in range(2):
                    s = sc[:, 2 * h + half, :]
                    wk, d_wk = RW.get()
                    P.op("dve", lambda e: e.max(out=v12[:, half, 0:8], in_=s), reads=[d_sc], writes=[d_v])
                    P.op("dve", lambda e: e.match_replace(out=wk[:, :128], in_to_replace=v12[:, half, 0:8], in_values=s, imm_value=NEG),
                         reads=[d_sc, d_v], writes=[d_wk])
                    P.op("dve", lambda e: e.max(out=v12[:, half, 8:16], in_=wk[:, :128]), reads=[d_wk], writes=[d_v])
                cand, d_cand = RW.get()
                P.op("dve", lambda e: e.tensor_tensor(out=cand[:].rearrange("p (a b) -> p a b", a=16),
                                                      in0=v12[:, 0, :].unsqueeze(2).to_broadcast([128, 16, 16]),
                                                      in1=v12[:, 1, :].unsqueeze(1).to_broadcast([128, 16, 16]), op=ALU.add),
                     reads=[d_v], writes=[d_cand])
                m8, d_m8 = RM.get()
                wk2, d_wk2 = RW.get()
                P.op("dve", lambda e: e.max(out=m8[:, 0:8], in_=cand[:]), reads=[d_cand], writes=[d_m8])
                P.op("dve", lambda e: e.match_replace(out=wk2[:], in_to_replace=m8[:, 0:8], in_values=cand[:], imm_value=NEG),
                     reads=[d_cand, d_m8], writes=[d_wk2])
                P.op("dve", lambda e: e.max(out=m8[:, 8:16], in_=wk2[:]), reads=[d_wk2], writes=[d_m8])
                P.op("dve", lambda e: e.match_replace(out=wk2[:], in_to_replace=m8[:, 8:16], in_values=wk2[:], imm_value=NEG),
                     reads=[d_m8, d_wk2], writes=[d_wk2])
                P.op("dve", lambda e: e.max(out=m8[:, 16:24], in_=wk2[:]), reads=[d_wk2], writes=[d_m8])
                P.op("dve", lambda e: e.tensor_tensor(out=m8[:, 25:26], in0=m8[:, 15:16], in1=m8[:, 16:17], op=ALU.add),
                     reads=[d_m8], writes=[d_m8])
                P.op("dve", lambda e: e.tensor_scalar(out=m8[:, 24:25], in0=m8[:, 25:26], scalar1=-0.5, scalar2=None, op0=ALU.mult),
                     reads=[d_m8], writes=[d_m8])
                ex, d_ex = RM.get()
                P.op("act", lambda e: e.activation(out=ex[:, 0:16], in_=m8[:, 0:16], func=AF.Exp, bias=m8[:, 24:25], accum_out=ex[:, 16:17]),
                     reads=[d_m8], writes=[d_ex])
                P.op("dve", lambda e: e.reciprocal(out=zi[:, h:h + 1], in_=ex[:, 16:17]), reads=[d_ex], writes=[d_zi])
                P.op("dve", lambda e: e.tensor_scalar(out=ab[:, 0, h, :], in0=sc[:, 2 * h, :], scalar1=m8[:, 24:25], scalar2=None, op0=ALU.add),
                     reads=[d_sc, d_m8], writes=[d_ab])
                P.op("pool", lambda e: e.tensor_copy(out=ab[:, 1, h, :], in_=sc[:, 2 * h + 1, :]), reads=[d_sc], writes=[d_ab])
            P.dma(AB[ti * 128:(ti + 1) * 128], ab[:], reads=[d_ab])
            P.dma(ZI[ti * 128:(ti + 1) * 128], zi[:], reads=[d_zi])
        P.barrier()
    with ExitStack() as st:
        C = load_consts(P, st, I)
        RO = Ring(P, st, [128, 512], F32, 4, "po", psum=True)
        RA = Ring(P, st, [128, 512], F32, 2, "paT", psum=True)
        RG = Ring(P, st, [128, 512], F32, 2, "pgT", psum=True)
        RU = Ring(P, st, [128, 16, 512], BF16, 2, "ublk")
        RVB = Ring(P, st, [128, 4, D], BF16, 2, "vblk")
        RSP = Ring(P, st, [128, 8, 4, 128], F32, 1, "sp")
        RE = Ring(P, st, [128, 8, 4, 128], BF16, 1, "ee")
        RGH = Ring(P, st, [128, 8, 4, 128], BF16, 2, "gh")
        RGL = Ring(P, st, [128, 512], F32, 2, "gelu")
        RWA = Ring(P, st, [128, 4, 128], BF16, 2, "wact")
        RAB = Ring(P, st, [128, 2, 8, 128], F32, 1, "ab")
        RZ = Ring(P, st, [128, 8], F32, 2, "zi")
        RDZ = Ring(P, st, [128, 8, 128], BF16, 2, "dz")
        RH = Ring(P, st, [128, 16, 128], BF16, 2, "h2t")
        RX = Ring(P, st, [128, D], F32, 2, "x1t")
        RF = Ring(P, st, [128, 512], F32, 3, "tmpf")
        RS8 = Ring(P, st, [128, 8], F32, 4, "s8")
        sel = P.sbuf(st, [2, 2, 128], F32, "sel")
        grow = P.sbuf(st, [2, 2, D], F32, "grow")
        d_sel, d_grow = Dep(), Dep()
        P.dma(sel[:], I["sel"], writes=[d_sel])
        P.dma(grow[:], I["grow"], writes=[d_grow])
        gbc = [P.sbuf(st, [128, D], F32, f"g2bc{r}") for r in range(2)]
        d_gbc = [Dep(), Dep()]
        Rtmp = {"aux": RA}
        bcast_rows(P, Rtmp, sel, d_sel, grow, d_grow, 1, gbc, d_gbc)
        if last:
            fg = P.sbuf(st, [1, D], F32, "fg")
            d_fg = Dep()
            P.dma(fg[:], I["fing"], writes=[d_fg])
            fgbc = P.sbuf(st, [128, D], F32, "fgbc")
            d_fgbc = Dep()
            for cb in range(4):
                ps, d_ps = RA.get()
                P.op("pe", lambda e: e.matmul(ps[:, :512], lhsT=C.onesf[0:1, :], rhs=fg[0:1, cb * 512:(cb + 1) * 512], start=True, stop=True),
                     reads=[C.d_onesf, d_fg], writes=[d_ps], sig=True)
                P.op("act", lambda e: e.copy(out=fgbc[:, cb * 512:(cb + 1) * 512], in_=ps[:, :512]), reads=[d_ps], writes=[d_fgbc])
        uv = ubf.rearrange("(kc p) e -> p kc e", p=128)
        vv = vbf.rearrange("(c p) d -> p c d", p=128)
        for ti in range(18):
            r = 1 if ti < 2 else 0
            ht, d_ht = RH.get()
            P.dma(ht[:], h2T[:, :, ti * 128:(ti + 1) * 128].rearrange("c p t -> p c t"), writes=[d_ht])
            ab, d_ab = RAB.get()
            P.dma(ab[:], AB[ti * 128:(ti + 1) * 128], writes=[d_ab])
            zi, d_zi = RZ.get()
            P.dma(zi[:], ZI[ti * 128:(ti + 1) * 128], writes=[d_zi])
            dz, d_dz = RDZ.get()
            for h in range(8):
                P.op("pool", lambda e, h=h: e.tensor_scalar(out=dz[:, h, :], in0=C.identf[:], scalar1=zi[:, h:h + 1], scalar2=None, op0=ALU.mult),
                     reads=[C.d_identf, d_zi], writes=[d_dz])
            pos = [RO.get() for _ in range(4)]

            def stageA(eb):
                sp, d_sp = RSP.get()
                P.op("pool", lambda e: e.tensor_tensor(out=sp[:],
                                                       in0=ab[:, 0, :, eb * 4:(eb + 1) * 4].unsqueeze(3).to_broadcast([128, 8, 4, 128]),
                                                       in1=ab[:, 1, :, :].unsqueeze(2).to_broadcast([128, 8, 4, 128]), op=ALU.add),
                     reads=[d_ab], writes=[d_sp])
                ee, d_ee = RE.get()
                P.op("act", lambda e: e.activation(out=ee[:], in_=sp[:], func=AF.Exp), reads=[d_sp], writes=[d_ee])
                gh, d_gh = RGH.get()
                P.op("dve", lambda e: e.scalar_tensor_tensor(out=gh[:], in0=sp[:], scalar=0.0, in1=ee[:], op0=ALU.is_ge, op1=ALU.mult),
                     reads=[d_sp, d_ee], writes=[d_gh])
                return gh, d_gh

            def stageB(eb):
                ub, d_ub = RU.get()
                P.dma(ub[:], uv[:, :, eb * 512:(eb + 1) * 512], writes=[d_ub])
                vb, d_vb = RVB.get()
                P.dma(vb[:], vv[:, eb * 4:(eb + 1) * 4, :], writes=[d_vb])
                pa, d_pa = RA.get()
                for c in range(4):
                    for kc in range(16):
                        P.op("pe", lambda e, c=c, kc=kc: e.matmul(pa[:, c * 128:(c + 1) * 128], lhsT=ub[:, kc, c * 128:(c + 1) * 128], rhs=ht[:, kc, :],
                                                                  start=(kc == 0), stop=(kc == 15)),
                             reads=[d_ub, d_ht], writes=[d_pa], sig=(c == 3 and kc == 15))
                gl, d_gl = RGL.get()
                P.op("act", lambda e: e.activation(out=gl[:], in_=pa[:], func=AF.Gelu), reads=[d_pa], writes=[d_gl])
                return gl, d_gl, vb, d_vb

            nA = stageA(0)
            nB = stageB(0)
            for eb in range(32):
                gh, d_gh = nA
                gl, d_gl, vb, d_vb = nB
                if eb + 1 < 32:
                    nA = stageA(eb + 1)
                pg, d_pg = RG.get()
                for c in range(4):
                    for h in range(8):
                        P.op("pe", lambda e, c=c, h=h: e.matmul(pg[:, c * 128:(c + 1) * 128], lhsT=gh[:, h, c, :], rhs=dz[:, h, :],
                                                                start=(h == 0), stop=(h == 7)),
                             reads=[d_gh, d_dz], writes=[d_pg], sig=(c == 3 and h == 7))
                if eb + 1 < 32:
                    nB = stageB(eb + 1)
                wa, d_wa = RWA.get()
                P.op("dve", lambda e: e.tensor_tensor(out=wa[:].rearrange("p c t -> p (c t)"), in0=pg[:], in1=gl[:], op=ALU.mult),
                     reads=[d_pg, d_gl], writes=[d_wa])
                for db in range(4):
                    po, d_po = pos[db]
                    for c in range(4):
                        P.op("pe", lambda e, c=c, db=db, po=po: e.matmul(po[:, :512], lhsT=wa[:, c, :], rhs=vb[:, c, db * 512:(db + 1) * 512],
                                                                         start=(eb == 0 and c == 0), stop=(eb == 31 and c == 3)),
                             reads=[d_wa, d_vb], writes=[d_po], sig=(c == 3))
            xt, d_xt = RX.get()
            P.dma(xt[:], x1[ti * 128:(ti + 1) * 128, :], writes=[d_xt])
            for db in range(4):
                po, d_po = pos[db]
                t, d_t = RF.get()
                P.op("dve", lambda e: e.tensor_tensor(out=t[:, :512], in0=po[:, :512], in1=gbc[r][:, db * 512:(db + 1) * 512], op=ALU.mult),
                     reads=[d_po, d_gbc[r]], writes=[d_t])
                P.op("pool", lambda e: e.tensor_tensor(out=xt[:, db * 512:(db + 1) * 512], in0=xt[:, db * 512:(db + 1) * 512], in1=t[:, :512], op=ALU.add),
                     reads=[d_t, d_xt], writes=[d_xt])
            if last:
                s8, d_s8 = RS8.get()
                xo, d_xo = RX.get()
                P.op("act", lambda e: e.activation(out=xo[:], in_=xt[:], func=AF.Square, accum_out=s8[:, 0:1]), reads=[d_xt], writes=[d_xo, d_s8])
                P.op("act", lambda e: e.activation(out=s8[:, 1:2], in_=s8[:, 0:1], func=AF.Sqrt, scale=1.0 / D, bias=EPS), reads=[d_s8], writes=[d_s8])
                P.op("dve", lambda e: e.reciprocal(out=s8[:, 2:3], in_=s8[:, 1:2]), reads=[d_s8], writes=[d_s8])
                P.op("dve", lambda e: e.scalar_tensor_tensor(out=xo[:], in0=xt[:], scalar=s8[:, 2:3], in1=fgbc[:], op0=ALU.mult, op1=ALU.mult),
                     reads=[d_xt, d_s8, d_fgbc, d_xo], writes=[d_xo])
                P.dma(xout[ti * 128:(ti + 1) * 128, :], xo[:], reads=[d_xo])
            else:
                P.dma(xout[ti * 128:(ti + 1) * 128, :], xt[:], reads=[d_xt])
        P.barrier()


def stage2(P, I, O, last):
    oaT = P.dram("oaT", [8, 128, T], BF16)
    omT = P.dram("omT", [8, 128, T], BF16)
    olT = P.dram("olT", [1024, T], BF16)
    yT = P.dram("yT", [16, 128, T], BF16)
    x1 = P.dram("x1", [T, D], F32)
    I = dict(I)
    I["ubf"] = P.dram("ubf", [D, NEXP], BF16)
    I["vbf"] = P.dram("vbf", [NEXP, D], BF16)
    for i in range(16):
        P.dma(I["ubf"][i * 128:(i + 1) * 128, :], I["peer_uT"][i * 128:(i + 1) * 128, :], eng="pool")
        P.dma(I["vbf"][i * 1024:(i + 1) * 1024, :], I["peer_v"][i * 1024:(i + 1) * 1024, :], eng="pool")
    attention(P, I, oaT, omT)
    gla_stage2(P, I, olT)
    merge_stage(P, I, oaT, olT, omT, yT)
    wout_stage(P, I, yT, x1)
    peer_stage(P, I, x1, O["xout"], last)


def build_s2(last):
    P = Prog()
    I = {k: P.dram(k, sh, dt, kind="ExternalInput") for k, (sh, dt) in S2_INPUTS.items()}
    O = {k: P.dram(k, sh, dt, kind="ExternalOutput") for k, (sh, dt) in S2_OUTPUTS.items()}
    stage2(P, I, O, last)
    return P.finish(), P


OWN_KEYS = ["mfm", "grow", "qTa", "mqT", "lqT", "lkT", "lk", "lv", "lgT", "laf", "lab", "gT"]


def s2_inputs(core, layer, xin_core, s1out, inp, shared):
    b, q = core // 4, core % 4
    L = layer
    own = s1out[core]
    grp = [s1out[b * 4 + j] for j in range(4)]
    m = {"xin": xin_core}
    for k in OWN_KEYS:
        m[k] = own[k]
    m["kTa_all"] = np.ascontiguousarray(np.concatenate([own["kTa"][:, :, :NCTX]] + [g["kTa"][:, :, NCTX:] for g in grp], axis=2))
    m["mkT_all"] = np.ascontiguousarray(np.concatenate([own["mkT"][:, :, :NCTX]] + [g["mkT"][:, :, NCTX:] for g in grp], axis=2))
    m["mkrT_all"] = np.ascontiguousarray(np.concatenate([own["mkrT"][:, :NCTX]] + [g["mkrT"][:, NCTX:] for g in grp], axis=1))
    m["va_all"] = np.ascontiguousarray(np.concatenate([own["va"][:NCTX]] + [g["va"][NCTX:] for g in grp], axis=0))
    m["vm_all"] = np.ascontiguousarray(np.concatenate([own["vm"][:NCTX]] + [g["vm"][NCTX:] for g in grp], axis=0))
    m["glaC"] = own["glaC"]
    A3 = np.ones((2, 3, 128, 4), np.float32)
    U3 = np.zeros((2, 3, 4, 128, 256), np.float32)
    for k in range(3):
        qq = q - 3 + k
        if qq >= 0:
            A3[0, k] = grp[qq]["glaA"][0][:, 0::4]
            U3[0, k] = grp[qq]["glaU"][0]
        qq = q + 3 - k
        if qq <= 3:
            A3[1, k] = grp[qq]["glaA"][1][:, 0::4]
            U3[1, k] = grp[qq]["glaU"][1]
    m["glaA3"], m["glaU3"] = A3, U3
    m.update(shared)
    return m


def s2_shared(layer, inp):
    L = layer
    sh = {}
    sh["w_br"] = np.ascontiguousarray(np.stack([inp["w_br_gqa"][L], inp["w_br_gla"][L], inp["w_br_mla"][L]], 0))
    sh["w_out"] = inp["w_out"][L]
    sh["n2g"] = _fm16(inp["norm2_g"][L])
    sh["gon"] = np.ascontiguousarray(inp["gla_on_g"][L].reshape(2, 128).T)
    sh["peer_wq"] = inp["peer_wq"][L]
    k12 = np.stack([inp["peer_k1"][L], inp["peer_k2"][L]], 1)
    sh["k12T"] = np.ascontiguousarray(k12.reshape(16, 128, 128).transpose(0, 2, 1))
    sh["peer_uT"] = np.ascontiguousarray(inp["peer_u"][L].T)
    sh["peer_v"] = inp["peer_v"][L]
    sh["fing"] = np.ascontiguousarray(inp["final_g"].reshape(1, D))
    sel = np.zeros((2, 2, 128), np.float32)
    sel[0, 0] = 1.0
    sel[1, 1] = 1.0
    sh["sel"] = sel
    c = consts_np()
    for k in ("ident", "ones", "triF", "triB"):
        sh[k] = c[k]
    return sh


_PROGS = {}


def _prog(name):
    if name not in _PROGS:
        if name == "s1":
            _PROGS[name] = build_s1()[0]
        elif name == "s2":
            _PROGS[name] = build_s2(False)[0]
        else:
            _PROGS[name] = build_s2(True)[0]
    return _PROGS[name]


def kernel_unfused(**inputs):
    inp = {k: np.asarray(v) for k, v in inputs.items()}
    xs = [np.asarray(inp["x"][b], np.float32) for b in range(2)]
    ctxs = [np.asarray(inp["ctx"][b], np.float32) for b in range(2)]
    cores = list(range(8))
    for L in range(2):
        maps1 = [s1_inputs(c, L, xs, ctxs, inp) for c in cores]
        r1 = run_bass_kernel_spmd(_prog("s1"), maps1, core_ids=cores)
        s1out = r1.results
        shared = s2_shared(L, inp)
        maps2 = [s2_inputs(c, L, maps1[c]["xin"], s1out, inp, shared) for c in cores]
        del maps1
        r2 = run_bass_kernel_spmd(_prog("s2" if L == 0 else "s2last"), maps2, core_ids=cores)
        del maps2
        new_xs = []
        for b in range(2):
            new_xs.append(np.concatenate([np.asarray(r2.results[b * 4 + j]["xout"])[NCTX:] for j in range(4)], 0))
            ctxs[b] = np.asarray(r2.results[b * 4]["xout"])[:NCTX]
        xs = new_xs
    return np.stack(xs, 0).astype(np.float32)


GROUPS4 = [[0, 1, 2, 3], [4, 5, 6, 7]]
NCHB = 164
NCHF = 17

S1_W = ["w_mod", "bmodT", "bmod_g", "n1g", "w_in", "gq", "gk", "waf", "wab", "gmq", "gmkv", "wuq", "wukv"]
S2_W = ["w_br", "w_out", "n2g", "gon", "peer_wq", "k12T", "peer_uT", "peer_v"]
SHARED_IN = ["cT", "cosA", "sinA", "cosM", "sinM", "ident", "ones", "rotA", "rotM", "triF", "triB", "sel", "fing"]


def exchange(P, O1, X):
    sendB = P.dram("sendB", [NCHB, 128, 256], BF16)
    recvB = P.dram("recvB", [NCHB, 512, 256], BF16)
    sendF = P.dram("sendF", [NCHF, 128, 128], F32)
    recvF = P.dram("recvF", [NCHF, 512, 128], F32)
    d_s, d_r = Dep(), Dep()
    L0 = NCTX
    for kvh in range(2):
        P.dma(sendB[kvh * 8:(kvh + 1) * 8], O1["kTa"][kvh][:, L0:].rearrange("p (t c) -> t p c", c=256), writes=[d_s])
    P.dma(sendB[16:32], O1["va"][L0:, :].rearrange("(i p) c -> i p c", p=128), writes=[d_s])
    for h in range(8):
        P.dma(sendB[32 + h * 8:40 + h * 8], O1["mkT"][h][:, L0:].rearrange("p (t c) -> t p c", c=256), writes=[d_s])
    vmv = sendB[96:160].rearrange("(i q) p c -> i q p c", q=4)
    for cq in range(4):
        P.dma(vmv[:, cq], O1["vm"][L0:, cq * 256:(cq + 1) * 256].rearrange("(i p) c -> i p c", p=128), writes=[d_s])
    for j in range(4):
        P.dma(sendB[160 + j].rearrange("(h p) c -> h p c", h=2),
              O1["mkrT"][:, L0 + j * 512:L0 + (j + 1) * 512].rearrange("p (h c) -> h p c", h=2), writes=[d_s])
    for d in range(2):
        for h in range(4):
            P.dma(sendF[(d * 4 + h) * 2:(d * 4 + h) * 2 + 2], O1["glaU"][d, h].rearrange("p (f c) -> f p c", f=2), writes=[d_s])
    P.dma(sendF[16][:, 0:32].rearrange("p (d c) -> p d c", d=2), O1["glaA"].rearrange("d p c -> p d c"), writes=[d_s])
    recs = []
    WIN = 4

    def gather(src, dst):
        if len(recs) >= WIN:
            P._wait("pool", recs[-WIN])
        recs.append(P.collective("AllGather", GROUPS4, src, dst, reads=[d_s], writes=[Dep()]))
    for i in range(NCHB):
        gather(sendB[i].bitcast(F32), recvB[i].bitcast(F32))
    for i in range(NCHF):
        gather(sendF[i], recvF[i])
    d_r.w = recs[-1]
    P.dma(X["kTa_all"][:, :, 0:L0], O1["kTa"][:, :, 0:L0])
    P.dma(X["mkT_all"][:, :, 0:L0], O1["mkT"][:, :, 0:L0])
    P.dma(X["mkrT_all"][:, 0:L0], O1["mkrT"][:, 0:L0])
    P.dma(X["va_all"][0:L0, :], O1["va"][0:L0, :])
    P.dma(X["vm_all"][0:L0, :], O1["vm"][0:L0, :])
    for r in range(4):
        rr = slice(r * 128, (r + 1) * 128)
        t0 = L0 + r * NLAT
        for kvh in range(2):
            P.dma(X["kTa_all"][kvh][:, t0:t0 + NLAT].rearrange("p (t c) -> t p c", c=256), recvB[kvh * 8:(kvh + 1) * 8, rr, :], reads=[d_r])
        for h in range(8):
            P.dma(X["mkT_all"][h][:, t0:t0 + NLAT].rearrange("p (t c) -> t p c", c=256), recvB[32 + h * 8:40 + h * 8, rr, :], reads=[d_r])
        P.dma(X["va_all"][t0:t0 + NLAT, :].rearrange("(i p) c -> i p c", p=128), recvB[16:32, rr, :], reads=[d_r])
        rv = recvB[96:160].rearrange("(i q) m c -> i q m c", q=4)
        for cq in range(4):
            P.dma(X["vm_all"][t0:t0 + NLAT, cq * 256:(cq + 1) * 256].rearrange("(i p) c -> i p c", p=128), rv[:, cq, rr, :], reads=[d_r])
        for j in range(4):
            P.dma(X["mkrT_all"][:, t0 + j * 512:t0 + (j + 1) * 512].rearrange("p (h c) -> h p c", h=2),
                  recvB[160 + j, rr, :].rearrange("(h p) c -> h p c", h=2), reads=[d_r])
    P.barrier()
    return recvF


def fused_input_specs():
    specs = {"xin0": ([T, D], F32), "qmask": ([128, 16], F32)}
    for k in SHARED_IN:
        specs[k] = (S1_INPUTS.get(k) or S2_INPUTS[k])
    for L in range(2):
        for k in S1_W:
            specs[f"{k}_{L}"] = S1_INPUTS[k]
        for k in S2_W:
            specs[f"{k}_{L}"] = S2_INPUTS[k]
    return specs


def build_fused():
    P = Prog()
    specs = fused_input_specs()
    E = {k: P.dram(k, sh, dt, kind="ExternalInput") for k, (sh, dt) in specs.items()}
    xout = P.dram("xout", [T, D], F32, kind="ExternalOutput")
    xmid = P.dram("xmid", [T, D], F32)
    O1 = {k: P.dram("s1_" + k, sh, dt) for k, (sh, dt) in S1_OUTPUTS.items()}
    X = {k: P.dram(k, S2_INPUTS[k][0], S2_INPUTS[k][1]) for k in ("kTa_all", "va_all", "mkT_all", "mkrT_all", "vm_all")}
    for L in range(2):
        I1 = {k: E[k] for k in SHARED_IN if k in S1_INPUTS}
        I1["xin"] = E["xin0"] if L == 0 else xmid
        for k in S1_W:
            I1[k] = E[f"{k}_{L}"]
        stage1(P, I1, O1)
        recvF = exchange(P, O1, X)
        I2 = {k: E[k] for k in SHARED_IN if k in S2_INPUTS}
        I2["xin"] = I1["xin"]
        for k in OWN_KEYS:
            I2[k] = O1[k]
        I2.update(X)
        I2["recvF"] = recvF
        I2["qmask"] = E["qmask"]
        for k in S2_W:
            I2[k] = E[f"{k}_{L}"]
        stage2(P, I2, {"xout": xmid if L == 0 else xout}, last=(L == 1))
    return P.finish(), P


def fused_inputs(core, inp, shared2):
    b, q = core // 4, core % 4
    m = {}
    xs = [inp["x"][bb] for bb in range(2)]
    ctxs = [inp["ctx"][bb] for bb in range(2)]
    for L in range(2):
        s1 = s1_inputs(core, L, xs, ctxs, inp)
        if L == 0:
            m["xin0"] = s1["xin"]
            for k in SHARED_IN:
                if k in s1:
                    m[k] = s1[k]
        for k in S1_W:
            m[f"{k}_{L}"] = s1[k]
        for k in S2_W:
            m[f"{k}_{L}"] = shared2[L][k]
    m["sel"] = shared2[0]["sel"]
    m["fing"] = shared2[0]["fing"]
    qm = np.zeros((128, 16), np.float32)
    for j in range(4):
        qm[:, j] = 1.0 if j < q else 0.0
        qm[:, 4 + j] = 1.0 if j > q else 0.0
    qm[:, 8:16] = 1.0 - qm[:, 0:8]
    m["qmask"] = qm
    return m


def kernel_fused(**inputs):
    inp = {k: np.asarray(v) for k, v in inputs.items()}
    if "fused" not in _PROGS:
        _PROGS["fused"] = build_fused()[0]
    shared2 = [s2_shared(L, inp) for L in range(2)]
    cores = list(range(8))
    maps = [fused_inputs(c, inp, shared2) for c in cores]
    r = run_bass_kernel_spmd(_PROGS["fused"], maps, core_ids=cores)
    xs = []
    for b in range(2):
        xs.append(np.concatenate([np.asarray(r.results[b * 4 + j]["xout"])[NCTX:] for j in range(4)], 0))
    return np.stack(xs, 0).astype(np.float32)


def kernel(**inputs):
    return kernel_fused(**inputs)
```

```python
from contextlib import ExitStack
import numpy as np
import ml_dtypes
import concourse.bass as bass
import concourse.mybir as mybir
from concourse.bass_utils import run_bass_kernel_spmd

F32 = mybir.dt.float32
BF16 = mybir.dt.bfloat16
ALU = mybir.AluOpType
AF = mybir.ActivationFunctionType
AX = mybir.AxisListType

ENGS = ("pe", "act", "dve", "pool", "sp")
SEMQ = ENGS + ("cc",)
SEM_ROT = 12000
NSEM = {"pe": 8, "act": 10, "dve": 14, "pool": 8, "sp": 1, "cc": 1}
NSLOT = {"sp": 16, "pool": 12, "act": 4}

D = 2048
T = 2304
NCTX = 256
NLAT = 2048
KC = 16
EPS = 1e-6
import os
STOP = os.environ.get('K_STOP', '')
BLKS = [(0, 256), (256, 512), (768, 512), (1280, 512), (1792, 512)]
IN_COLS = 11872
O_AQ, O_AK, O_AV, O_LQ, O_LK, O_LV, O_LG, O_LRF, O_LRB, O_MCQ, O_MCKV, O_MKR, O_GA = (
    0, 1024, 1280, 1536, 2048, 2560, 3584, 4608, 4624, 4640, 5152, 5664, 5728)


class Dep:
    __slots__ = ("w", "r", "x")

    def __init__(self, x=False):
        self.w = None
        self.r = []
        self.x = x


class Rec:
    __slots__ = ("eng", "cnt", "is_dma", "dma_idx", "q")

    def __init__(self, eng, is_dma=False):
        self.eng = eng
        self.cnt = None
        self.is_dma = is_dma
        self.dma_idx = None


class Prog:
    def __init__(self):
        self.nc = bass.Bass("TRN2", target_bir_lowering=False)
        nc = self.nc
        self.E = {"pe": nc.tensor, "act": nc.scalar, "dve": nc.vector, "pool": nc.gpsimd, "sp": nc.sync}
        self.stack = ExitStack()
        self.sems = {e: [self.stack.enter_context(nc.semaphore(f"s_{e}_{i}")) for i in range(NSEM[e])] for e in SEMQ}
        self.slots = {q: [self.stack.enter_context(nc.semaphore(f"s_dma_{q}_{i}")) for i in range(n)] for q, n in NSLOT.items()}
        self.count = {e: 0 for e in SEMQ}
        self.last_cc = None
        self.last = {e: None for e in ENGS}
        self.known = {e: {} for e in ENGS}
        self.pe_unsig = []
        self.ndma = 0
        self.dma_recs = {q: [] for q in NSLOT}
        self.ntile = 0
        self.ninstr = {e: 0 for e in ENGS}
        self.nwait = 0

    def dram(self, name, shape, dtype, kind="Internal"):
        if kind == "Internal":
            self.ntile += 1
            name = f"{name}_i{self.ntile}"
        return self.nc.dram_tensor(name, list(shape), dtype, kind=kind).ap()

    def sbuf(self, stack, shape, dtype, name="sb"):
        self.ntile += 1
        return stack.enter_context(self.nc.sbuf_tensor(f"{name}_{self.ntile}", list(shape), dtype))

    def psum(self, stack, shape, dtype, name="ps"):
        self.ntile += 1
        return stack.enter_context(self.nc.psum_tensor(f"{name}_{self.ntile}", list(shape), dtype))

    def _sem_of(self, rec):
        if rec.is_dma:
            k = rec.dma_idx
            ns = NSLOT[rec.q]
            return ("d", rec.q, k % ns), k // ns + 1, self.slots[rec.q][k % ns], 16 * (k // ns + 1)
        if rec.cnt is None:
            raise RuntimeError("dependency on unsignaled PE instruction")
        c = rec.cnt - 1
        return ("c", rec.eng), rec.cnt, self.sems[rec.eng][c // SEM_ROT], (c % SEM_ROT) + 1

    def _wait(self, engname, rec):
        key, val, s, v = self._sem_of(rec)
        kn = self.known[engname]
        if kn.get(key, 0) >= val:
            return
        kn[key] = val
        self.E[engname].wait_ge(s, v)
        self.nwait += 1

    def _deps(self, rec, reads, writes):
        deps = []
        for d in reads:
            if d.w is not None:
                deps.append(d.w)
            if d.x:
                deps.extend(r for r in d.r if r.eng != rec.eng)
        for d in writes:
            if d.w is not None:
                deps.append(d.w)
            deps.extend(d.r)
        seen = set()
        for x in deps:
            if id(x) in seen:
                continue
            seen.add(id(x))
            if x.eng == "pe" and rec.eng == "pe" and not x.is_dma and not rec.is_dma:
                continue
            self._wait("pool" if rec.eng == "cc" else rec.eng, x)
        for d in reads:
            d.r.append(rec)
        for d in writes:
            d.w = rec
            d.r = []

    def op(self, eng, fn, reads=(), writes=(), sig=None):
        rec = Rec(eng)
        self._deps(rec, reads, writes)
        ins = fn(self.E[eng])
        self.ninstr[eng] += 1
        if sig is None:
            sig = eng != "pe"
        if sig:
            self.count[eng] += 1
            rec.cnt = self.count[eng]
            c = rec.cnt - 1
            ins.then_inc(self.sems[eng][c // SEM_ROT], 1)
            if eng == "pe":
                for r in self.pe_unsig:
                    r.cnt = rec.cnt
                self.pe_unsig = []
        else:
            self.pe_unsig.append(rec)
        self.last[eng] = rec
        return rec

    def dma(self, out, in_, reads=(), writes=(), eng="sp", **kw):
        rec = Rec(eng, is_dma=True)
        rec.q = eng
        ns = NSLOT[eng]
        rec.dma_idx = len(self.dma_recs[eng])
        self.ndma += 1
        if rec.dma_idx >= ns:
            self._wait(eng, self.dma_recs[eng][rec.dma_idx - ns])
        self._deps(rec, reads, writes)
        ins = self.E[eng].dma_start(out=out, in_=in_, **kw)
        ins.then_inc(self.slots[eng][rec.dma_idx % ns], 16)
        self.ninstr[eng] += 1
        self.dma_recs[eng].append(rec)
        return rec

    def collective(self, kind, groups, src, dst, reads=(), writes=()):
        rec = Rec("cc")
        self._deps(rec, reads, writes)
        ins = self.nc.gpsimd.collective_compute(kind, ALU.bypass, replica_groups=groups, ins=[src.opt()], outs=[dst.opt()])
        self.count["cc"] += 1
        rec.cnt = self.count["cc"]
        ins.then_inc(self.sems["cc"][0])
        self.ninstr["pool"] += 1
        self.last_cc = rec
        return rec

    def barrier(self):
        if self.pe_unsig:
            raise RuntimeError("barrier with unsignaled PE instrs")
        lasts = [self.last[e] for e in ENGS if self.last[e] is not None and e != "sp"]
        if self.last_cc is not None:
            lasts.append(self.last_cc)
        pend = [r for q in NSLOT for r in self.dma_recs[q][-NSLOT[q]:]]
        for e in ENGS:
            for x in lasts + pend:
                if (not x.is_dma) and x.eng == e and e == "pe":
                    continue
                self._wait(e, x)

    def finish(self):
        self.barrier()
        self.stack.close()
        return self.nc


class Ring:
    def __init__(self, P, stack, shape, dtype, n, name, psum=False):
        mk = P.psum if psum else P.sbuf
        self.tiles = [(mk(stack, shape, dtype, name), Dep(x=psum)) for _ in range(n)]
        self.i = 0

    def get(self):
        t = self.tiles[self.i % len(self.tiles)]
        self.i += 1
        return t


class Consts:
    pass


def load_consts(P, st, I):
    C = Consts()

    def ld(name, shape, dtype=F32, cast=False):
        t = P.sbuf(st, shape, BF16 if cast else dtype, name)
        d = Dep()
        P.dma(t[:], I[name], writes=[d], eng="pool" if cast else "sp")
        return t, d

    C.identf, C.d_identf = ld("ident", [128, 128])
    C.identb, C.d_identb = ld("ident", [128, 128], cast=True)
    C.onesb, C.d_onesb = ld("ones", [128, 128], cast=True)
    C.onesf, C.d_onesf = ld("ones", [128, 128])
    return C


def rms_fm(P, R, C, producers, gains, n, nfeat, outs, d_outs):
    ys = []
    ss, d_ss = R["aux"].get()
    k = len(producers)
    for i, prod in enumerate(producers):
        ps, d_ps = prod()
        rows = ps.shape[0]
        y, d_y = R["f32"].get()
        sq, d_sq = R["bf"].get()
        P.op("act", lambda e, y=y, ps=ps, rows=rows: e.copy(out=y[:rows, :n], in_=ps), reads=[d_ps], writes=[d_y])
        P.op("act", lambda e, sq=sq, ps=ps, rows=rows: e.activation(out=sq[:rows, :n], in_=ps, func=AF.Square),
             reads=[d_ps], writes=[d_sq])
        P.op("pe", lambda e, sq=sq, rows=rows, i=i: e.matmul(ss[:, :n], lhsT=C.onesb[:rows, :], rhs=sq[:rows, :n],
                                                            start=(i == 0), stop=(i == k - 1)),
             reads=[d_sq, C.d_onesb], writes=[d_ss], sig=(i == k - 1))
        ys.append((y, d_y, rows))
    rs, d_rs = R["f32"].get()
    P.op("act", lambda e: e.activation(out=rs[:, :n], in_=ss[:, :n], func=AF.Sqrt, scale=1.0 / nfeat, bias=EPS),
         reads=[d_ss], writes=[d_rs])
    P.op("dve", lambda e: e.reciprocal(out=rs[:, :n], in_=rs[:, :n]), reads=[d_rs], writes=[d_rs])
    for (y, d_y, rows), g, o, d_o in zip(ys, gains, outs, d_outs):
        P.op("dve", lambda e, y=y, g=g, o=o, rows=rows: e.scalar_tensor_tensor(
            out=o, in0=y[:rows, :n], scalar=g, in1=rs[:rows, :n], op0=ALU.mult, op1=ALU.mult),
            reads=[d_y, d_rs], writes=[d_o])


def gla_pass(P, st, C, R, src, direction, chunk_ids, state, d_state, state_bf, d_state_bf, emit=None, asum=None):
    tri = C.triF if direction == 0 else C.triB
    d_tri = C.d_tri
    for ci in chunk_ids:
        t0 = ci * 128
        la, d_la = R["la"].get()
        lk, d_lk = R["lk"].get()
        lv, d_lv = R["lv"].get()
        P.dma(la[:], src["la"][t0:t0 + 128, :], writes=[d_la])
        P.dma(lk[:], src["lk"][t0:t0 + 128, :], writes=[d_lk])
        P.dma(lv[:], src["lv"][t0:t0 + 128, :], writes=[d_lv])
        pb, d_pb = R["mm"].get()
        pe_, d_pe = R["mm"].get()
        P.op("pe", lambda e: e.matmul(pb[:, :512], lhsT=tri[:], rhs=la[:], start=True, stop=True),
             reads=[d_tri, d_la], writes=[d_pb], sig=True)
        P.op("pe", lambda e: e.matmul(pe_[:, :512], lhsT=C.onesf[:], rhs=la[:], start=True, stop=True),
             reads=[C.d_onesf, d_la], writes=[d_pe], sig=True)
        bsb, d_bsb = R["f32"].get()
        P.op("act", lambda e: e.copy(out=bsb[:, :512], in_=pb[:, :512]), reads=[d_pb], writes=[d_bsb])
        dif, d_dif = R["f32"].get()
        P.op("dve", lambda e: e.tensor_tensor(out=dif[:, :512], in0=pe_[:, :512], in1=bsb[:, :512], op=ALU.subtract),
             reads=[d_pe, d_bsb], writes=[d_dif])
        P.op("act", lambda e: e.activation(out=dif[:, :512], in_=dif[:, :512], func=AF.Exp), reads=[d_dif], writes=[d_dif])
        kh, d_kh = R["kh"].get()
        P.op("dve", lambda e: e.tensor_tensor(out=kh[:, :512], in0=lk[:], in1=dif[:, :512], op=ALU.mult),
             reads=[d_lk, d_dif], writes=[d_kh])
        pbe, d_pbe = R["aux"].get()
        for h in range(4):
            P.op("pe", lambda e, h=h: e.matmul(pbe[:, 4 * h:4 * h + 4], lhsT=la[:, h * 128:(h + 1) * 128], rhs=C.onesf[:, 0:4],
                                               start=True, stop=True),
                 reads=[d_la, C.d_onesf], writes=[d_pbe], sig=(h == 3))
        ebe, d_ebe = R["small"].get()
        P.op("act", lambda e: e.activation(out=ebe[:, 0:16], in_=pbe[:, 0:16], func=AF.Exp), reads=[d_pbe], writes=[d_ebe])
        if asum is not None:
            P.op("dve", lambda e: e.tensor_tensor(out=asum[0][:, 0:16], in0=asum[0][:, 0:16], in1=pbe[:, 0:16], op=ALU.add),
                 reads=[d_pbe, asum[1]], writes=[asum[1]])
        if emit is not None:
            lqT, lkT = emit["lqT"], emit["lkT"]
            for h in range(4):
                pbt, d_pbt = R["aux"].get()
                P.op("pe", lambda e, h=h: e.matmul(pbt[:, :128], lhsT=la[:, h * 128:(h + 1) * 128], rhs=tri[:],
                                                   start=True, stop=True),
                     reads=[d_la, d_tri], writes=[d_pbt], sig=True)
                eb, d_eb = R["f32"].get()
                enb, d_enb = R["f32"].get()
                P.op("act", lambda e: e.activation(out=eb[:, :128], in_=pbt[:, :128], func=AF.Exp), reads=[d_pbt], writes=[d_eb])
                P.op("act", lambda e: e.activation(out=enb[:, :128], in_=pbt[:, :128], func=AF.Exp, scale=-1.0),
                     reads=[d_pbt], writes=[d_enb])
                qt, d_qt = R["bf"].get()
                kt, d_kt = R["bf"].get()
                P.op("dve", lambda e, h=h: e.tensor_tensor(out=qt[:, :128], in0=lqT[:, h, t0:t0 + 128], in1=eb[:, :128], op=ALU.mult),
                     reads=[emit["d_lqT"], d_eb], writes=[d_qt])
                P.op("pool", lambda e, h=h: e.tensor_tensor(out=kt[:, :128], in0=lkT[:, h, t0:t0 + 128], in1=enb[:, :128], op=ALU.mult),
                     reads=[emit["d_lkT"], d_enb], writes=[d_kt])
                psc, d_psc = R["aux"].get()
                P.op("pe", lambda e: e.matmul(psc[:, :128], lhsT=kt[:, :128], rhs=qt[:, :128], start=True, stop=True),
                     reads=[d_kt, d_qt], writes=[d_psc], sig=True)
                scm, d_scm = R["bf"].get()
                P.op("dve", lambda e: e.tensor_tensor(out=scm[:, :128], in0=psc[:, :128], in1=tri[:], op=ALU.mult),
                     reads=[d_psc, d_tri], writes=[d_scm])
                po, d_po = R["mm"].get()
                for j in range(2):
                    P.op("pe", lambda e, h=h, j=j: e.matmul(po[:, j * 128:(j + 1) * 128],
                                                            lhsT=lv[:, h * 256 + j * 128:h * 256 + (j + 1) * 128],
                                                            rhs=scm[:, :128], start=True, stop=False),
                         reads=[d_lv, d_scm], writes=[d_po], sig=False)
                    P.op("pe", lambda e, h=h, j=j: e.matmul(po[:, j * 128:(j + 1) * 128],
                                                            lhsT=state_bf[h][:, j * 128:(j + 1) * 128],
                                                            rhs=qt[:, :128], start=False, stop=True),
                         reads=[d_state_bf[h], d_qt], writes=[d_po], sig=(j == 1))
                oT, d_oT = emit["oT"], emit["d_oT"][ci]
                if emit["first"]:
                    P.op("act", lambda e, h=h: e.copy(out=oT[:, 2 * h:2 * h + 2, t0:t0 + 128],
                                                     in_=po[:, 0:256].rearrange("p (j t) -> p j t", j=2)),
                         reads=[d_po], writes=[d_oT])
                else:
                    P.op("dve", lambda e, h=h: e.tensor_tensor(out=oT[:, 2 * h:2 * h + 2, t0:t0 + 128],
                                                               in0=oT[:, 2 * h:2 * h + 2, t0:t0 + 128],
                                                               in1=po[:, 0:256].rearrange("p (j t) -> p j t", j=2), op=ALU.add),
                         reads=[d_po, d_oT], writes=[d_oT])
        for h in range(4):
            pu, d_pu = R["aux"].get()
            P.op("pe", lambda e, h=h: e.matmul(pu[:, :256], lhsT=kh[:, h * 128:(h + 1) * 128], rhs=lv[:, h * 256:(h + 1) * 256],
                                               start=True, stop=True),
                 reads=[d_kh, d_lv], writes=[d_pu], sig=True)
            P.op("dve", lambda e, h=h: e.scalar_tensor_tensor(out=state[h][:], in0=state[h][:], scalar=ebe[:, 4 * h:4 * h + 1],
                                                              in1=pu[:, :256], op0=ALU.mult, op1=ALU.add),
                 reads=[d_pu, d_ebe, d_state[h]] + ([d_state_bf[h]] if state_bf is not None else []), writes=[d_state[h]])
            if state_bf is not None:
                P.op("act", lambda e, h=h: e.copy(out=state_bf[h][:], in_=state[h][:]), reads=[d_state[h]], writes=[d_state_bf[h]])


def mk_rings(P, st, n_mm=3, n_aux=2):
    R = {}
    R["mm"] = Ring(P, st, [128, 512], F32, n_mm, "psmm", psum=True)
    R["aux"] = Ring(P, st, [128, 512], F32, n_aux, "psaux", psum=True)
    R["f32"] = Ring(P, st, [128, 512], F32, 12, "rf32")
    R["bf"] = Ring(P, st, [128, 512], BF16, 8, "rbf")
    R["small"] = Ring(P, st, [128, 16], F32, 8, "rsm")
    R["kh"] = Ring(P, st, [128, 512], BF16, 2, "rkh")
    return R


S1_INPUTS = {
    "xin": ([T, D], F32), "cT": ([128, 16, 2], F32), "w_mod": ([D, 6 * D], F32), "bmodT": ([128, 96], F32),
    "bmod_g": ([2, 2, D], F32), "n1g": ([128, 16], F32), "w_in": ([D, IN_COLS], F32),
    "gq": ([128, 1], F32), "gk": ([128, 1], F32), "waf": ([17, 512], F32), "wab": ([17, 512], F32),
    "gmq": ([128, 4], F32), "gmkv": ([128, 4], F32), "wuq": ([512, 1536], F32), "wukv": ([512, 2048], F32),
    "cosA": ([128, NLAT], F32), "sinA": ([128, NLAT], F32), "cosM": ([64, NLAT], F32), "sinM": ([64, NLAT], F32),
    "ident": ([128, 128], F32), "ones": ([128, 128], F32), "rotA": ([128, 128], F32), "rotM": ([64, 64], F32),
    "triF": ([128, 128], F32), "triB": ([128, 128], F32),
}
S1_OUTPUTS = {
    "mfm": ([128, 96, 2], F32), "grow": ([2, 2, D], F32),
    "qTa": ([8, 128, T], BF16), "kTa": ([2, 128, T], BF16), "va": ([T, 256], BF16),
    "lqT": ([4, 128, T], BF16), "lkT": ([4, 128, T], BF16), "lk": ([T, 512], BF16), "lv": ([T, 1024], BF16),
    "lgT": ([1024, T], BF16), "laf": ([T, 512], F32), "lab": ([T, 512], F32),
    "mqT": ([8, 192, T], BF16), "mkT": ([8, 128, T], BF16), "mkrT": ([64, T], BF16), "vm": ([T, 1024], BF16),
    "gT": ([3, D, T], BF16),
    "glaA": ([2, 128, 16], F32), "glaU": ([2, 4, 128, 256], F32), "glaC": ([2, 4, 128, 256], F32),
}


def stage1(P, I, O):
    with ExitStack() as st:
        C = load_consts(P, st, I)
        C.triF = P.sbuf(st, [128, 128], F32, "triF")
        C.triB = P.sbuf(st, [128, 128], F32, "triB")
        C.d_tri = Dep()
        P.dma(C.triF[:], I["triF"], writes=[C.d_tri])
        P.dma(C.triB[:], I["triB"], writes=[C.d_tri])
        R = mk_rings(P, st)
        R["w"] = Ring(P, st, [128, 16, 512], BF16, 2, "wt")
        hT = P.sbuf(st, [128, 16, T], BF16, "hT")
        d_hT = [(Dep(), Dep()) for _ in range(18)]
        mfm = P.sbuf(st, [128, 96, 2], F32, "mfm")
        d_mfm = Dep()
        gm1 = P.sbuf(st, [128, 16, 2], F32, "gm1")
        d_gm1 = Dep()

        with ExitStack() as sa:
            cT = P.sbuf(sa, [128, 16, 2], F32, "cT")
            d_cT = Dep()
            scT = P.sbuf(sa, [128, 16, 2], BF16, "scT")
            d_scT = Dep()
            bmodT = P.sbuf(sa, [128, 96], F32, "bmodT")
            d_bm = Dep()
            bmg = P.sbuf(sa, [2, 2, D], F32, "bmg")
            d_bmg = Dep()
            grow = P.sbuf(sa, [2, 2, D], F32, "grow")
            d_grow = Dep()
            n1g = P.sbuf(sa, [128, 16], F32, "n1g")
            d_n1g = Dep()
            P.dma(cT[:], I["cT"], writes=[d_cT])
            P.dma(bmodT[:], I["bmodT"], writes=[d_bm])
            P.dma(bmg[:], I["bmod_g"], writes=[d_bmg])
            P.dma(n1g[:], I["n1g"], writes=[d_n1g])
            P.op("act", lambda e: e.activation(out=scT[:], in_=cT[:], func=AF.Silu), reads=[d_cT], writes=[d_scT])
            wmv = I["w_mod"].rearrange("(kc p) c -> p kc c", p=128)
            for g in range(24):
                wt, d_wt = R["w"].get()
                P.dma(wt[:], wmv[:, :, g * 512:(g + 1) * 512], writes=[d_wt], eng="pool")
                for j in range(4):
                    cc = g * 4 + j
                    ps, d_ps = R["aux"].get()
                    for kc in range(16):
                        P.op("pe", lambda e, kc=kc, j=j, wt=wt, ps=ps: e.matmul(
                            ps[:, 0:2], lhsT=wt[:, kc, j * 128:(j + 1) * 128], rhs=scT[:, kc, :],
                            start=(kc == 0), stop=(kc == 15)), reads=[d_wt, d_scT], writes=[d_ps], sig=(kc == 15))
                    P.op("dve", lambda e, cc=cc, ps=ps: e.tensor_scalar(out=mfm[:, cc, :], in0=ps[:, 0:2], scalar1=bmodT[:, cc:cc + 1],
                                                                       scalar2=None, op0=ALU.add),
                         reads=[d_ps, d_bm], writes=[d_mfm])
                gi = {2: 0, 5: 1}.get(g // 4)
                if gi is not None:
                    off = (g % 4) * 512
                    psr, d_psr = R["mm"].get()
                    for kc in range(16):
                        P.op("pe", lambda e, kc=kc, wt=wt, psr=psr: e.matmul(
                            psr[0:2, :], lhsT=scT[:, kc, :], rhs=wt[:, kc, :], start=(kc == 0), stop=(kc == 15)),
                            reads=[d_wt, d_scT], writes=[d_psr], sig=(kc == 15))
                    P.op("dve", lambda e, gi=gi, off=off, psr=psr: e.tensor_tensor(
                        out=grow[0:2, gi, off:off + 512], in0=psr[0:2, :], in1=bmg[0:2, gi, off:off + 512], op=ALU.add),
                        reads=[d_psr, d_bmg], writes=[d_grow])
            P.dma(O["mfm"], mfm[:], reads=[d_mfm])
            P.dma(O["grow"], grow[:], reads=[d_grow])
            P.op("dve", lambda e: e.tensor_scalar(out=gm1[:], in0=mfm[:, 16:32, :], scalar1=1.0, scalar2=None, op0=ALU.add),
                 reads=[d_mfm], writes=[d_gm1])
            P.op("dve", lambda e: e.tensor_tensor(out=gm1[:], in0=gm1[:], in1=n1g[:].unsqueeze(2).to_broadcast([128, 16, 2]),
                                                  op=ALU.mult), reads=[d_gm1, d_n1g], writes=[d_gm1])
            P.barrier()

        if STOP == "a":
            P.barrier()
            return
        norm_to_fm(P, st, C, I["xin"], hT, d_hT, gm1, d_gm1, mfm, d_mfm, 0)
        if STOP == "b":
            for kc in range(8):
                P.dma(O["lgT"][kc * 128:(kc + 1) * 128, :], hT[:, kc, :], reads=[x for p in d_hT for x in p])
            P.barrier()
            return

        w_in_v = I["w_in"].rearrange("(kc p) c -> p kc c", p=128)

        def hdeps(t0, n):
            out = []
            for ti in range(t0 // 128, (t0 + n) // 128):
                out.extend(d_hT[ti])
            return out

        def load_w(off, ncols):
            wt, d_wt = R["w"].get()
            P.dma(wt[:, :, :ncols], w_in_v[:, :, off:off + ncols], writes=[d_wt], eng="pool")
            return wt, d_wt

        def fm_chunk(wt, d_wt, c0, rows, blk):
            t0, n = blk
            ps, d_ps = R["mm"].get()
            for kc in range(16):
                P.op("pe", lambda e, kc=kc: e.matmul(ps[:rows, :n], lhsT=wt[:, kc, c0:c0 + rows], rhs=hT[:, kc, t0:t0 + n],
                                                     start=(kc == 0), stop=(kc == 15)),
                     reads=[d_wt] + hdeps(t0, n), writes=[d_ps], sig=(kc == 15))
            return ps[:rows, :n], d_ps

        def tm_tile(wt, d_wt, c0, ncols, ti):
            ps, d_ps = R["mm"].get()
            for kc in range(16):
                P.op("pe", lambda e, kc=kc: e.matmul(ps[:, :ncols], lhsT=hT[:, kc, ti * 128:(ti + 1) * 128],
                                                     rhs=wt[:, kc, c0:c0 + ncols], start=(kc == 0), stop=(kc == 15)),
                     reads=[d_wt] + list(d_hT[ti]), writes=[d_ps], sig=(kc == 15))
            return ps[:, :ncols], d_ps

        def store(dst, src, dep):
            P.dma(dst, src, reads=[dep])

        def evac_bf(ps, d_ps, rows, n, func=AF.Copy, scale=1.0, eng="act"):
            ob, d_ob = R["bf"].get()
            if eng == "act":
                P.op("act", lambda e: e.activation(out=ob[:rows, :n], in_=ps, func=func, scale=scale), reads=[d_ps], writes=[d_ob])
            else:
                P.op("dve", lambda e: e.tensor_copy(out=ob[:rows, :n], in_=ps), reads=[d_ps], writes=[d_ob])
            return ob, d_ob

        def simple_fm(off, ncols, dst_fn, func=AF.Copy, scale=1.0):
            wt, d_wt = load_w(off, ncols)
            for blk in BLKS:
                for c in range(ncols // 128):
                    ps, d_ps = fm_chunk(wt, d_wt, c * 128, 128, blk)
                    ob, d_ob = evac_bf(ps, d_ps, 128, blk[1], func, scale)
                    store(dst_fn(c, blk), ob[:, :blk[1]], d_ob)

        def simple_tm(off, ncols, dst, dcol, wt_pair=None):
            wt, d_wt = wt_pair or load_w(off, ncols)
            for ti in range(18):
                ps, d_ps = tm_tile(wt, d_wt, 0, ncols, ti)
                ob, d_ob = evac_bf(ps, d_ps, 128, ncols, eng="dve")
                store(dst[ti * 128:(ti + 1) * 128, dcol:dcol + ncols], ob[:, :ncols], d_ob)
            return wt, d_wt

        with ExitStack() as sc:
            def ldc(name, shape, cast=True):
                t = P.sbuf(sc, shape, BF16 if cast else F32, name)
                d = Dep()
                P.dma(t[:], I[name], writes=[d], eng="pool" if cast else "sp")
                return t, d
            cosA, d_cosA = ldc("cosA", [128, NLAT])
            sinA, d_sinA = ldc("sinA", [128, NLAT])
            cosM, d_cosM = ldc("cosM", [64, NLAT])
            sinM, d_sinM = ldc("sinM", [64, NLAT])
            rotA, d_rotA = ldc("rotA", [128, 128])
            rotM, d_rotM = ldc("rotM", [64, 64])
            gq, d_gq = ldc("gq", [128, 1], cast=False)
            gk, d_gk = ldc("gk", [128, 1], cast=False)
            gmq, d_gmq = ldc("gmq", [128, 4], cast=False)
            gmkv, d_gmkv = ldc("gmkv", [128, 4], cast=False)
            P.op("dve", lambda e: e.tensor_scalar(out=gq[:], in0=gq[:], scalar1=128 ** -0.5, scalar2=None, op0=ALU.mult),
                 reads=[d_gq], writes=[d_gq])

            def rope(o, d_o, rows, blk, rot, d_rot, cos, d_cos, sin, d_sin):
                t0, n = blk
                ob, d_ob = R["bf"].get()
                if t0 < NCTX:
                    P.op("act", lambda e: e.copy(out=ob[:rows, :n], in_=o[:rows, :n]), reads=[d_o], writes=[d_ob])
                    return ob, d_ob
                l0 = t0 - NCTX
                P.op("act", lambda e: e.copy(out=ob[:rows, :n], in_=o[:rows, :n]), reads=[d_o], writes=[d_ob])
                pr, d_pr = R["aux"].get()
                P.op("pe", lambda e: e.matmul(pr[:rows, :n], lhsT=rot[:rows, :rows], rhs=ob[:rows, :n], start=True, stop=True),
                     reads=[d_rot, d_ob], writes=[d_pr], sig=True)
                t2, d_t2 = R["f32"].get()
                P.op("dve", lambda e: e.tensor_tensor(out=t2[:rows, :n], in0=pr[:rows, :n], in1=sin[:rows, l0:l0 + n], op=ALU.mult),
                     reads=[d_pr, d_sin], writes=[d_t2])
                P.op("pool", lambda e: e.tensor_tensor(out=o[:rows, :n], in0=o[:rows, :n], in1=cos[:rows, l0:l0 + n], op=ALU.mult),
                     reads=[d_o, d_cos], writes=[d_o])
                ob2, d_ob2 = R["bf"].get()
                P.op("pool", lambda e: e.tensor_tensor(out=ob2[:rows, :n], in0=o[:rows, :n], in1=t2[:rows, :n], op=ALU.add),
                     reads=[d_o, d_t2], writes=[d_ob2])
                return ob2, d_ob2

            def gqa_heads(off, nheads, gain, d_gain, dst):
                for g0 in range(0, nheads, 4):
                    nh = min(4, nheads - g0)
                    wt, d_wt = load_w(off + g0 * 128, nh * 128)
                    for blk in BLKS:
                        for hh in range(nh):
                            o, d_o = R["f32"].get()
                            rms_fm(P, R, C, [lambda hh=hh: fm_chunk(wt, d_wt, hh * 128, 128, blk)], [gain[:, 0:1]], blk[1], 128,
                                   [o[:, :blk[1]]], [d_o])
                            ob, d_ob = rope(o, d_o, 128, blk, rotA, d_rotA, cosA, d_cosA, sinA, d_sinA)
                            store(dst[g0 + hh, :, blk[0]:blk[0] + blk[1]], ob[:, :blk[1]], d_ob)
            gqa_heads(O_AQ, 8, gq, d_gq, O["qTa"])
            wt, d_wt = load_w(O_AK, 512)
            for blk in BLKS:
                for hh in range(2):
                    o, d_o = R["f32"].get()
                    rms_fm(P, R, C, [lambda hh=hh: fm_chunk(wt, d_wt, hh * 128, 128, blk)], [gk[:, 0:1]], blk[1], 128,
                           [o[:, :blk[1]]], [d_o])
                    ob, d_ob = rope(o, d_o, 128, blk, rotA, d_rotA, cosA, d_cosA, sinA, d_sinA)
                    store(O["kTa"][hh, :, blk[0]:blk[0] + blk[1]], ob[:, :blk[1]], d_ob)
            for ti in range(18):
                ps, d_ps = tm_tile(wt, d_wt, 256, 256, ti)
                ob, d_ob = evac_bf(ps, d_ps, 128, 256, eng="dve")
                store(O["va"][ti * 128:(ti + 1) * 128, :], ob[:, :256], d_ob)
            if STOP == "c1":
                P.barrier()
                return
            simple_fm(O_LQ, 512, lambda c, blk: O["lqT"][c, :, blk[0]:blk[0] + blk[1]], scale=128 ** -0.5)
            wt, d_wt = load_w(O_LK, 512)
            for blk in BLKS:
                for c in range(4):
                    ps, d_ps = fm_chunk(wt, d_wt, c * 128, 128, blk)
                    ob, d_ob = evac_bf(ps, d_ps, 128, blk[1])
                    store(O["lkT"][c, :, blk[0]:blk[0] + blk[1]], ob[:, :blk[1]], d_ob)
            simple_tm(O_LK, 512, O["lk"], 0, wt_pair=(wt, d_wt))
            simple_tm(O_LV, 512, O["lv"], 0)
            simple_tm(O_LV + 512, 512, O["lv"], 512)
            simple_fm(O_LG, 512, lambda c, blk: O["lgT"][c * 128:(c + 1) * 128, blk[0]:blk[0] + blk[1]], func=AF.Silu)
            simple_fm(O_LG + 512, 512, lambda c, blk: O["lgT"][512 + c * 128:512 + (c + 1) * 128, blk[0]:blk[0] + blk[1]], func=AF.Silu)
            for g in range(12):
                simple_fm(O_GA + g * 512, 512,
                          lambda c, blk, g=g: O["gT"][g // 4, (g % 4) * 512 + c * 128:(g % 4) * 512 + (c + 1) * 128, blk[0]:blk[0] + blk[1]],
                          func=AF.Sigmoid)
            if STOP == "c2":
                P.barrier()
                return
            with ExitStack() as sl:
                lrA = [P.sbuf(sl, [32, T], F32, f"lrA{d}") for d in range(2)]
                d_lrA = [Dep(), Dep()]
                wa = [P.sbuf(sl, [17, 512], F32, f"wa{d}") for d in range(2)]
                d_wa = [Dep(), Dep()]
                for d in range(2):
                    P.op("pool", lambda e, d=d: e.memset(lrA[d][:], 1.0), writes=[d_lrA[d]])
                    P.dma(wa[d][:], I["waf" if d == 0 else "wab"], writes=[d_wa[d]])
                wt, d_wt = load_w(O_LRF, 32)
                for blk in BLKS:
                    for d in range(2):
                        ps, d_ps = fm_chunk(wt, d_wt, d * 16, 16, blk)
                        P.op("act", lambda e, d=d, ps=ps: e.copy(out=lrA[d][0:16, blk[0]:blk[0] + blk[1]], in_=ps),
                             reads=[d_ps], writes=[d_lrA[d]])
                for d in range(2):
                    for ti in range(18):
                        ps, d_ps = R["mm"].get()
                        P.op("pe", lambda e, d=d, ti=ti, ps=ps: e.matmul(ps[:, :512], lhsT=lrA[d][0:17, ti * 128:(ti + 1) * 128],
                                                                         rhs=wa[d][:], start=True, stop=True),
                             reads=[d_lrA[d], d_wa[d]], writes=[d_ps], sig=True)
                        ex, d_ex = R["f32"].get()
                        P.op("act", lambda e, ps=ps, ex=ex: e.activation(out=ex[:, :512], in_=ps[:, :512], func=AF.Exp, scale=-1.0),
                             reads=[d_ps], writes=[d_ex])
                        P.op("act", lambda e, ex=ex: e.activation(out=ex[:, :512], in_=ex[:, :512], func=AF.Ln, bias=1.0),
                             reads=[d_ex], writes=[d_ex])
                        P.op("pool", lambda e, ex=ex: e.tensor_scalar(out=ex[:, :512], in0=ex[:, :512], scalar1=-1.0 / 16.0, scalar2=None,
                                                                      op0=ALU.mult), reads=[d_ex], writes=[d_ex])
                        store(O["laf" if d == 0 else "lab"][ti * 128:(ti + 1) * 128, :], ex[:, :512], d_ex)
                P.barrier()
            if STOP == "c3":
                P.barrier()
                return
            with ExitStack() as sm:
                wuq = P.sbuf(sm, [128, 4, 1536], BF16, "wuq")
                wukv = P.sbuf(sm, [128, 4, 2048], BF16, "wukv")
                d_wuq, d_wukv = Dep(), Dep()
                P.dma(wuq[:], I["wuq"].rearrange("(kc p) c -> p kc c", p=128), writes=[d_wuq], eng="pool")
                P.dma(wukv[:], I["wukv"].rearrange("(kc p) c -> p kc c", p=128), writes=[d_wukv], eng="pool")
                RC = Ring(P, sm, [128, 4, 512], BF16, 2, "cn")
                wq_t, d_wq_t = load_w(O_MCQ, 512)
                wkv_t, d_wkv_t = load_w(O_MCKV, 512)
                for blk in BLKS:
                    t0, n = blk
                    cqn, d_cqn = RC.get()
                    rms_fm(P, R, C, [lambda c=c: fm_chunk(wq_t, d_wq_t, c * 128, 128, blk) for c in range(4)],
                           [gmq[:, c:c + 1] for c in range(4)], n, 512, [cqn[:, c, :n] for c in range(4)], [d_cqn] * 4)
                    for h in range(8):
                        ps, d_ps = R["mm"].get()
                        for kc in range(4):
                            P.op("pe", lambda e, kc=kc, h=h, ps=ps: e.matmul(ps[:, :n], lhsT=wuq[:, kc, h * 192:h * 192 + 128],
                                                                             rhs=cqn[:, kc, :n], start=(kc == 0), stop=(kc == 3)),
                                 reads=[d_wuq, d_cqn], writes=[d_ps], sig=(kc == 3))
                        ob, d_ob = evac_bf(ps[:, :n], d_ps, 128, n, scale=192 ** -0.5)
                        store(O["mqT"][h, 0:128, t0:t0 + n], ob[:, :n], d_ob)
                        ps2, d_ps2 = R["mm"].get()
                        for kc in range(4):
                            P.op("pe", lambda e, kc=kc, h=h, ps2=ps2: e.matmul(ps2[:64, :n], lhsT=wuq[:, kc, h * 192 + 128:h * 192 + 192],
                                                                               rhs=cqn[:, kc, :n], start=(kc == 0), stop=(kc == 3)),
                                 reads=[d_wuq, d_cqn], writes=[d_ps2], sig=(kc == 3))
                        o, d_o = R["f32"].get()
                        P.op("act", lambda e, o=o, ps2=ps2: e.activation(out=o[:64, :n], in_=ps2[:64, :n], func=AF.Copy, scale=192 ** -0.5),
                             reads=[d_ps2], writes=[d_o])
                        ob, d_ob = rope(o, d_o, 64, blk, rotM, d_rotM, cosM, d_cosM, sinM, d_sinM)
                        store(O["mqT"][h, 128:192, t0:t0 + n], ob[:64, :n], d_ob)
                    ckn, d_ckn = RC.get()
                    rms_fm(P, R, C, [lambda c=c: fm_chunk(wkv_t, d_wkv_t, c * 128, 128, blk) for c in range(4)],
                           [gmkv[:, c:c + 1] for c in range(4)], n, 512, [ckn[:, c, :n] for c in range(4)], [d_ckn] * 4)
                    for h in range(8):
                        ps, d_ps = R["mm"].get()
                        for kc in range(4):
                            P.op("pe", lambda e, kc=kc, h=h, ps=ps: e.matmul(ps[:, :n], lhsT=wukv[:, kc, h * 256:h * 256 + 128],
                                                                             rhs=ckn[:, kc, :n], start=(kc == 0), stop=(kc == 3)),
                                 reads=[d_wukv, d_ckn], writes=[d_ps], sig=(kc == 3))
                        ob, d_ob = evac_bf(ps[:, :n], d_ps, 128, n)
                        store(O["mkT"][h, :, t0:t0 + n], ob[:, :n], d_ob)
                    wv = wukv[:].rearrange("p k (h c) -> p k h c", c=256)
                    for tt in range(n // 128):
                        for half in range(2):
                            ps, d_ps = R["mm"].get()
                            for kc in range(4):
                                P.op("pe", lambda e, kc=kc, half=half, tt=tt, ps=ps: e.matmul(
                                    ps[:, :512].rearrange("p (h c) -> p h c", c=128), lhsT=ckn[:, kc, tt * 128:(tt + 1) * 128],
                                    rhs=wv[:, kc, half * 4:(half + 1) * 4, 128:256], start=(kc == 0), stop=(kc == 3)),
                                    reads=[d_wukv, d_ckn], writes=[d_ps], sig=(kc == 3))
                            ob, d_ob = evac_bf(ps[:, :512], d_ps, 128, 512, eng="dve")
                            store(O["vm"][t0 + tt * 128:t0 + (tt + 1) * 128, half * 512:(half + 1) * 512], ob[:, :512], d_ob)
                wt, d_wt = load_w(O_MKR, 64)
                for blk in BLKS:
                    ps, d_ps = fm_chunk(wt, d_wt, 0, 64, blk)
                    o, d_o = R["f32"].get()
                    P.op("act", lambda e, o=o, ps=ps: e.copy(out=o[:64, :blk[1]], in_=ps), reads=[d_ps], writes=[d_o])
                    ob, d_ob = rope(o, d_o, 64, blk, rotM, d_rotM, cosM, d_cosM, sinM, d_sinM)
                    store(O["mkrT"][:, blk[0]:blk[0] + blk[1]], ob[:64, :blk[1]], d_ob)
                P.barrier()
            P.barrier()
        P.barrier()

    if STOP == "c4":
        return
    with ExitStack() as st:
        C = load_consts(P, st, I)
        C.triF = P.sbuf(st, [128, 128], F32, "triF")
        C.triB = P.sbuf(st, [128, 128], F32, "triB")
        C.d_tri = Dep()
        P.dma(C.triF[:], I["triF"], writes=[C.d_tri])
        P.dma(C.triB[:], I["triB"], writes=[C.d_tri])
        R = mk_rings(P, st)
        R["la"] = Ring(P, st, [128, 512], F32, 2, "la")
        R["lk"] = Ring(P, st, [128, 512], BF16, 2, "lk")
        R["lv"] = Ring(P, st, [128, 1024], BF16, 2, "lv")
        state = [P.sbuf(st, [128, 256], F32, f"st{h}") for h in range(4)]
        d_state = [Dep() for _ in range(4)]
        asum = (P.sbuf(st, [128, 16], F32, "asum"), Dep())
        for d in range(2):
            src = {"la": O["laf" if d == 0 else "lab"], "lk": O["lk"], "lv": O["lv"]}
            for which in ("ctx", "lat"):
                for h in range(4):
                    P.op("pool", lambda e, h=h: e.memset(state[h][:], 0.0), writes=[d_state[h]])
                P.op("pool", lambda e: e.memset(asum[0][:], 0.0), writes=[asum[1]])
                ids = [0, 1] if which == "ctx" else list(range(2, 18))
                if d == 1:
                    ids = ids[::-1]
                gla_pass(P, st, C, R, src, d, ids, state, d_state, None, None, emit=None, asum=asum)
                dst = O["glaC"] if which == "ctx" else O["glaU"]
                for h in range(4):
                    P.dma(dst[d, h], state[h][:], reads=[d_state[h]])
                if which == "lat":
                    ea, d_ea = R["small"].get()
                    P.op("act", lambda e, ea=ea: e.activation(out=ea[:, 0:16], in_=asum[0][:, 0:16], func=AF.Exp),
                         reads=[asum[1]], writes=[d_ea])
                    P.dma(O["glaA"][d], ea[:, 0:16], reads=[d_ea])
        P.barrier()


def norm_to_fm(P, st, C, xsrc, hT, d_hT, gm, d_gm, mfm, d_mfm, shift_group):
    with ExitStack() as sb:
        RX = Ring(P, sb, [128, D], F32, 2, "x")
        RXN = Ring(P, sb, [128, D], BF16, 2, "xn")
        RT = Ring(P, sb, [128, 8, 128], BF16, 2, "tp", psum=True)
        RS = Ring(P, sb, [128, 8], F32, 4, "xs")
        for ti in range(18):
            r = 1 if ti < 2 else 0
            xt, d_xt = RX.get()
            P.dma(xt[:], xsrc[ti * 128:(ti + 1) * 128, :], writes=[d_xt])
            xn, d_xn = RXN.get()
            ssq, d_ssq = RS.get()
            P.op("act", lambda e: e.activation(out=xn[:], in_=xt[:], func=AF.Square, accum_out=ssq[:, 0:1]),
                 reads=[d_xt], writes=[d_xn, d_ssq])
            P.op("act", lambda e: e.activation(out=ssq[:, 1:2], in_=ssq[:, 0:1], func=AF.Sqrt, scale=1.0 / D, bias=EPS),
                 reads=[d_ssq], writes=[d_ssq])
            P.op("dve", lambda e: e.reciprocal(out=ssq[:, 2:3], in_=ssq[:, 1:2]), reads=[d_ssq], writes=[d_ssq])
            P.op("act", lambda e: e.activation(out=xn[:], in_=xt[:], func=AF.Copy, scale=ssq[:, 2:3]),
                 reads=[d_xt, d_ssq, d_xn], writes=[d_xn])
            tps = [RT.get(), RT.get()]
            for kc in range(16):
                tp, d_tp = tps[kc // 8]
                P.op("pe", lambda e, kc=kc: e.transpose(out=tp[:, kc % 8, :], in_=xn[:, kc * 128:(kc + 1) * 128], identity=C.identb[:]),
                     reads=[d_xn, C.d_identb], writes=[d_tp], sig=(kc % 8 == 7))
            for kc in range(16):
                tp, d_tp = tps[kc // 8]
                sl = hT[:, kc, ti * 128:(ti + 1) * 128]
                if kc < 8:
                    P.op("dve", lambda e, kc=kc, sl=sl: e.tensor_scalar(out=sl, in0=tp[:, kc % 8, :], scalar1=gm[:, kc, r:r + 1],
                                                                       scalar2=mfm[:, shift_group * 16 + kc, r:r + 1],
                                                                       op0=ALU.mult, op1=ALU.add),
                         reads=[d_tp, d_gm, d_mfm], writes=[d_hT[ti][0]])
                else:
                    P.op("act", lambda e, kc=kc, sl=sl: e.activation(out=sl, in_=tp[:, kc % 8, :], func=AF.Identity,
                                                                    scale=gm[:, kc, r:r + 1], bias=mfm[:, shift_group * 16 + kc, r:r + 1]),
                         reads=[d_tp, d_gm, d_mfm], writes=[d_hT[ti][1]])
        P.barrier()


def _fm16(v):
    return np.ascontiguousarray(np.asarray(v, np.float32).reshape(16, 128).T)


def _rope_tables(dim, pos0):
    quarter = dim // 4
    inv = (10000.0 ** (-np.arange(quarter, dtype=np.float32) / quarter)).astype(np.float32)
    t = np.arange(pos0, pos0 + NLAT)
    row = (t // 64).astype(np.float32)
    col = (t % 64).astype(np.float32)
    ang = np.concatenate([row[:, None] * inv, col[:, None] * inv], axis=-1)
    cos = np.cos(ang).astype(np.float32).T
    sin = np.sin(ang).astype(np.float32).T
    return (np.ascontiguousarray(np.concatenate([cos, cos], 0)), np.ascontiguousarray(np.concatenate([sin, sin], 0)))


def _rot_mat(dim):
    half = dim // 2
    L = np.zeros((dim, dim), np.float32)
    for m in range(half):
        L[m + half, m] = -1.0
        L[m, m + half] = 1.0
    return L


def consts_np():
    tri = np.triu(np.ones((128, 128), np.float32))
    return {"ident": np.eye(128, dtype=np.float32), "ones": np.ones((128, 128), np.float32),
            "rotA": _rot_mat(128), "rotM": _rot_mat(64), "triF": tri, "triB": np.ascontiguousarray(tri.T)}


def s1_inputs(core, layer, xs, ctxs, inp):
    b, q = core // 4, core % 4
    L = layer
    m = {}
    m["xin"] = np.ascontiguousarray(np.concatenate([ctxs[b], xs[b][q * NLAT:(q + 1) * NLAT]], 0))
    cc = np.stack([inp["c"][b], inp["c_ctx"]], -1)
    m["cT"] = np.ascontiguousarray(cc.reshape(16, 128, 2).transpose(1, 0, 2))
    m["w_mod"] = inp["w_mod"][L]
    bm = inp["b_mod"][L]
    m["bmodT"] = np.ascontiguousarray(bm.reshape(96, 128).T)
    g = np.stack([bm[2 * D:3 * D], bm[5 * D:6 * D]], 0)
    m["bmod_g"] = np.ascontiguousarray(np.stack([g, g], 0))
    m["n1g"] = _fm16(inp["norm1_g"][L])
    m["w_in"] = inp["w_in"][L]
    m["gq"] = np.ascontiguousarray(inp["gqa_qn_g"][L].reshape(128, 1))
    m["gk"] = np.ascontiguousarray(inp["gqa_kn_g"][L].reshape(128, 1))
    m["waf"] = np.ascontiguousarray(np.concatenate([inp["gla_wa2_f"][L], inp["gla_ba_f"][L][None]], 0))
    m["wab"] = np.ascontiguousarray(np.concatenate([inp["gla_wa2_b"][L], inp["gla_ba_b"][L][None]], 0))
    m["gmq"] = np.ascontiguousarray(inp["mla_qn_g"][L].reshape(4, 128).T)
    m["gmkv"] = np.ascontiguousarray(inp["mla_kvn_g"][L].reshape(4, 128).T)
    m["wuq"] = inp["mla_wuq"][L]
    m["wukv"] = inp["mla_wukv"][L]
    m["cosA"], m["sinA"] = _rope_tables(128, q * NLAT)
    m["cosM"], m["sinM"] = _rope_tables(64, q * NLAT)
    m.update(consts_np())
    return m


def build_s1():
    P = Prog()
    I = {k: P.dram(k, sh, dt, kind="ExternalInput") for k, (sh, dt) in S1_INPUTS.items()}
    O = {k: P.dram(k, sh, dt, kind="ExternalOutput") for k, (sh, dt) in S1_OUTPUTS.items()}
    stage1(P, I, O)
    return P.finish(), P


NK = NCTX + 4 * NLAT
NKT = NK // 128
NEXP = 16384

S2_INPUTS = {
    "xin": ([T, D], F32), "mfm": ([128, 96, 2], F32), "grow": ([2, 2, D], F32),
    "qTa": ([8, 128, T], BF16), "mqT": ([8, 192, T], BF16),
    "lqT": ([4, 128, T], BF16), "lkT": ([4, 128, T], BF16), "lk": ([T, 512], BF16), "lv": ([T, 1024], BF16),
    "lgT": ([1024, T], BF16), "laf": ([T, 512], F32), "lab": ([T, 512], F32), "gT": ([3, D, T], BF16),
    "kTa_all": ([2, 128, NK], BF16), "va_all": ([NK, 256], BF16),
    "mkT_all": ([8, 128, NK], BF16), "mkrT_all": ([64, NK], BF16), "vm_all": ([NK, 1024], BF16),
    "glaC": ([2, 4, 128, 256], F32), "glaA3": ([2, 3, 128, 4], F32), "glaU3": ([2, 3, 4, 128, 256], F32),
    "w_br": ([3, 1024, D], F32), "w_out": ([D, D], F32), "n2g": ([128, 16], F32), "gon": ([128, 2], F32),
    "peer_wq": ([D, D], F32), "k12T": ([16, 128, 128], F32), "peer_uT": ([D, NEXP], F32), "peer_v": ([NEXP, D], F32),
    "fing": ([1, D], F32), "sel": ([2, 2, 128], F32),
    "ident": ([128, 128], F32), "ones": ([128, 128], F32), "triF": ([128, 128], F32), "triB": ([128, 128], F32),
}
S2_OUTPUTS = {"xout": ([T, D], F32)}

QBLKS = [(0, 256, 2)] + [(256 + i * 512, 512, NKT) for i in range(4)]


def attention(P, I, oaT, omT):
    with ExitStack() as st:
        C = load_consts(P, st, I)
        RS = Ring(P, st, [128, 512], F32, 4, "ps_s", psum=True)
        RO = Ring(P, st, [128, 512], F32, 2, "ps_o", psum=True)
        RD = Ring(P, st, [128, 512], F32, 2, "ps_d", psum=True)
        RP = Ring(P, st, [128, 512], BF16, 4, "pT")
        RQ = Ring(P, st, [128, 512], BF16, 3, "qT")
        RQ2 = Ring(P, st, [64, 512], BF16, 3, "qT2")
        RF = Ring(P, st, [128, 512], F32, 3, "rden")
        ROB = Ring(P, st, [128, 512], BF16, 3, "ob")
        RK = Ring(P, st, [128, NK], BF16, 2, "kT")
        RV = Ring(P, st, [128, NKT, 128], BF16, 2, "V")
        kr = P.sbuf(st, [64, NK], BF16, "krope")
        d_kr = Dep()
        P.dma(kr[:], I["mkrT_all"], writes=[d_kr])

        def run_head(kT, d_kT, V, d_V, qsrc, dst, rope_q=None):
            for (t0, n, nkt) in QBLKS:
                q, d_q = RQ.get()
                P.dma(q[:, :n], qsrc[0:128, t0:t0 + n], writes=[d_q])
                if rope_q is not None:
                    q2, d_q2 = RQ2.get()
                    P.dma(q2[:, :n], qsrc[128:192, t0:t0 + n], writes=[d_q2])
                po, d_po = RO.get()
                pd, d_pd = RD.get()

                def qk(kt):
                    ps, d_ps = RS.get()
                    if rope_q is None:
                        P.op("pe", lambda e: e.matmul(ps[:, :n], lhsT=kT[:, kt * 128:(kt + 1) * 128], rhs=q[:, :n], start=True, stop=True),
                             reads=[d_kT, d_q], writes=[d_ps], sig=True)
                    else:
                        P.op("pe", lambda e: e.matmul(ps[:, :n], lhsT=kT[:, kt * 128:(kt + 1) * 128], rhs=q[:, :n], start=True, stop=False),
                             reads=[d_kT, d_q], writes=[d_ps], sig=False)
                        P.op("pe", lambda e: e.matmul(ps[:, :n], lhsT=kr[:, kt * 128:(kt + 1) * 128], rhs=q2[:, :n], start=False, stop=True),
                             reads=[d_kr, d_q2], writes=[d_ps], sig=True)
                    return ps, d_ps
                LOOK = 2
                pend = [qk(j) for j in range(min(LOOK, nkt))]
                for kt in range(nkt):
                    ps, d_ps = pend.pop(0)
                    if kt + LOOK < nkt:
                        pend.append(qk(kt + LOOK))
                    pT, d_pT = RP.get()
                    P.op("act", lambda e: e.activation(out=pT[:, :n], in_=ps[:, :n], func=AF.Exp), reads=[d_ps], writes=[d_pT])
                    P.op("pe", lambda e: e.matmul(po[:, :n], lhsT=V[:, kt, :], rhs=pT[:, :n], start=(kt == 0), stop=(kt == nkt - 1)),
                         reads=[d_V, d_pT], writes=[d_po], sig=False)
                    P.op("pe", lambda e: e.matmul(pd[:, :n], lhsT=C.onesb[:], rhs=pT[:, :n], start=(kt == 0), stop=(kt == nkt - 1)),
                         reads=[C.d_onesb, d_pT], writes=[d_pd], sig=True)
                rd, d_rd = RF.get()
                P.op("dve", lambda e: e.reciprocal(out=rd[:, :n], in_=pd[:, :n]), reads=[d_pd], writes=[d_rd])
                ob, d_ob = ROB.get()
                P.op("dve", lambda e: e.tensor_tensor(out=ob[:, :n], in0=po[:, :n], in1=rd[:, :n], op=ALU.mult),
                     reads=[d_po, d_rd], writes=[d_ob])
                P.dma(dst[:, t0:t0 + n], ob[:, :n], reads=[d_ob])

        for kvh in range(2):
            kT, d_kT = RK.get()
            V, d_V = RV.get()
            P.dma(kT[:], I["kTa_all"][kvh], writes=[d_kT])
            P.dma(V[:], I["va_all"].rearrange("(kt p) c -> p kt c", p=128)[:, :, kvh * 128:(kvh + 1) * 128], writes=[d_V])
            for g in range(4):
                h = kvh * 4 + g
                run_head(kT, d_kT, V, d_V, I["qTa"][h], oaT[h])
        for h in range(8):
            kT, d_kT = RK.get()
            V, d_V = RV.get()
            P.dma(kT[:], I["mkT_all"][h], writes=[d_kT])
            P.dma(V[:], I["vm_all"].rearrange("(kt p) c -> p kt c", p=128)[:, :, h * 128:(h + 1) * 128], writes=[d_V])
            run_head(kT, d_kT, V, d_V, I["mqT"][h], omT[h], rope_q=True)
        P.barrier()


def load_tri(P, st, C, I):
    C.triF = P.sbuf(st, [128, 128], F32, "triF")
    C.triB = P.sbuf(st, [128, 128], F32, "triB")
    C.d_tri = Dep()
    P.dma(C.triF[:], I["triF"], writes=[C.d_tri])
    P.dma(C.triB[:], I["triB"], writes=[C.d_tri])


def gla_stage2(P, I, olT):
    with ExitStack() as st:
        C = load_consts(P, st, I)
        load_tri(P, st, C, I)
        R = mk_rings(P, st)
        R["la"] = Ring(P, st, [128, 512], F32, 2, "la")
        R["lk"] = Ring(P, st, [128, 512], BF16, 2, "lk")
        R["lv"] = Ring(P, st, [128, 1024], BF16, 2, "lv")
        lqT = P.sbuf(st, [128, 4, T], BF16, "lqT")
        lkT = P.sbuf(st, [128, 4, T], BF16, "lkT")
        d_lqT, d_lkT = Dep(), Dep()
        P.dma(lqT[:], I["lqT"].rearrange("h p t -> p h t"), writes=[d_lqT])
        P.dma(lkT[:], I["lkT"].rearrange("h p t -> p h t"), writes=[d_lkT])
        oT = P.sbuf(st, [128, 8, T], F32, "oT")
        d_oT = [Dep() for _ in range(18)]
        state = [P.sbuf(st, [128, 256], F32, f"st{h}") for h in range(4)]
        state_bf = [P.sbuf(st, [128, 256], BF16, f"stb{h}") for h in range(4)]
        d_state = [Dep() for _ in range(4)]
        d_state_bf = [Dep() for _ in range(4)]
        gon = P.sbuf(st, [128, 2], F32, "gon")
        d_gon = Dep()
        P.dma(gon[:], I["gon"], writes=[d_gon])
        RA = Ring(P, st, [128, 4], F32, 2, "trA")
        RA16 = Ring(P, st, [128, 16], F32, 2, "trA16")
        RU = Ring(P, st, [128, 256], F32, 3, "trU")
        if "recvF" in I:
            qmt = P.sbuf(st, [128, 16], F32, "qmask")
            d_qmt = Dep()
            P.dma(qmt[:], I["qmask"], writes=[d_qmt])
            I = dict(I)
            I["qmask_sb"] = (qmt, d_qmt)
        for d in range(2):
            src = {"la": I["laf" if d == 0 else "lab"], "lk": I["lk"], "lv": I["lv"]}
            emit = dict(oT=oT, d_oT=d_oT, first=(d == 0), lqT=lqT, lkT=lkT, d_lqT=d_lqT, d_lkT=d_lkT)
            for h in range(4):
                P.op("pool", lambda e, h=h: e.memset(state[h][:], 0.0), writes=[d_state[h]])
                P.op("pool", lambda e, h=h: e.memset(state_bf[h][:], 0.0), writes=[d_state_bf[h]])
            ids = [0, 1] if d == 0 else [1, 0]
            gla_pass(P, st, C, R, src, d, ids, state, d_state, state_bf, d_state_bf, emit=emit)
            if "recvF" in I:
                qm = I["qmask_sb"]
                order = range(4) if d == 0 else range(3, -1, -1)
                for j in order:
                    A, d_A = RA16.get()
                    P.dma(A[:], I["recvF"][16, j * 128:(j + 1) * 128, d * 16:(d + 1) * 16], writes=[d_A])
                    mcol = d * 4 + j
                    P.op("dve", lambda e, A=A: e.tensor_scalar(out=A[:], in0=A[:], scalar1=qm[0][:, mcol:mcol + 1], scalar2=qm[0][:, 8 + mcol:9 + mcol],
                                                               op0=ALU.mult, op1=ALU.add), reads=[d_A, qm[1]], writes=[d_A])
                    for h in range(4):
                        U, d_U = RU.get()
                        for f in range(2):
                            P.dma(U[:, f * 128:(f + 1) * 128], I["recvF"][(d * 4 + h) * 2 + f, j * 128:(j + 1) * 128, :], writes=[d_U])
                        P.op("pool", lambda e, U=U: e.tensor_scalar(out=U[:], in0=U[:], scalar1=qm[0][:, mcol:mcol + 1], scalar2=None, op0=ALU.mult),
                             reads=[d_U, qm[1]], writes=[d_U])
                        P.op("dve", lambda e, h=h, A=A, U=U: e.scalar_tensor_tensor(out=state[h][:], in0=state[h][:], scalar=A[:, 4 * h:4 * h + 1],
                                                                                    in1=U[:], op0=ALU.mult, op1=ALU.add),
                             reads=[d_A, d_U, d_state[h]], writes=[d_state[h]])
            for k in (range(3) if "recvF" not in I else ()):
                A, d_A = RA.get()
                P.dma(A[:], I["glaA3"][d, k], writes=[d_A])
                for h in range(4):
                    U, d_U = RU.get()
                    P.dma(U[:], I["glaU3"][d, k, h], writes=[d_U])
                    P.op("dve", lambda e, h=h, A=A, U=U: e.scalar_tensor_tensor(out=state[h][:], in0=state[h][:], scalar=A[:, h:h + 1],
                                                                                in1=U[:], op0=ALU.mult, op1=ALU.add),
                         reads=[d_A, d_U, d_state[h]], writes=[d_state[h]])
            for h in range(4):
                P.op("act", lambda e, h=h: e.copy(out=state_bf[h][:], in_=state[h][:]), reads=[d_state[h]], writes=[d_state_bf[h]])
            ids = list(range(2, 18)) if d == 0 else list(range(17, 1, -1))
            gla_pass(P, st, C, R, src, d, ids, state, d_state, state_bf, d_state_bf, emit=emit)
        RG = Ring(P, st, [128, 512], BF16, 3, "lg")
        for (t0, n) in BLKS:
            deps = [d_oT[ti] for ti in range(t0 // 128, (t0 + n) // 128)]
            dd = Dep()
            for h in range(4):
                outs = [R["f32"].get() for _ in range(2)]
                prods = []
                for j in range(2):
                    def prod(j=j, h=h):
                        d0 = Dep()
                        return oT[:, 2 * h + j, t0:t0 + n], deps
                    prods.append(prod)
                rms_fm_multi(P, R, C, prods, [gon[:, j:j + 1] for j in range(2)], n, 256,
                             [outs[j][0][:, :n] for j in range(2)], [outs[j][1] for j in range(2)])
                for j in range(2):
                    lg, d_lg = RG.get()
                    c = 2 * h + j
                    P.dma(lg[:, :n], I["lgT"][c * 128:(c + 1) * 128, t0:t0 + n], writes=[d_lg])
                    ob, d_ob = R["bf"].get()
                    P.op("dve", lambda e, j=j, lg=lg, ob=ob: e.tensor_tensor(out=ob[:, :n], in0=outs[j][0][:, :n], in1=lg[:, :n], op=ALU.mult),
                         reads=[outs[j][1], d_lg], writes=[d_ob])
                    P.dma(olT[c * 128:(c + 1) * 128, t0:t0 + n], ob[:, :n], reads=[d_ob])
        P.barrier()


def rms_fm_multi(P, R, C, producers, gains, n, nfeat, outs, d_outs):
    ys = []
    ss, d_ss = R["aux"].get()
    k = len(producers)
    for i, prod in enumerate(producers):
        src, deps = prod()
        rows = src.shape[0]
        sq, d_sq = R["bf"].get()
        P.op("act", lambda e: e.activation(out=sq[:rows, :n], in_=src, func=AF.Square), reads=list(deps), writes=[d_sq])
        P.op("pe", lambda e: e.matmul(ss[:, :n], lhsT=C.onesb[:rows, :], rhs=sq[:rows, :n], start=(i == 0), stop=(i == k - 1)),
             reads=[d_sq, C.d_onesb], writes=[d_ss], sig=(i == k - 1))
        ys.append((src, deps, rows))
    rs, d_rs = R["f32"].get()
    P.op("act", lambda e: e.activation(out=rs[:, :n], in_=ss[:, :n], func=AF.Sqrt, scale=1.0 / nfeat, bias=EPS),
         reads=[d_ss], writes=[d_rs])
    P.op("dve", lambda e: e.reciprocal(out=rs[:, :n], in_=rs[:, :n]), reads=[d_rs], writes=[d_rs])
    for (src, deps, rows), g, o, d_o in zip(ys, gains, outs, d_outs):
        P.op("dve", lambda e: e.scalar_tensor_tensor(out=o, in0=src, scalar=g, in1=rs[:rows, :n], op0=ALU.mult, op1=ALU.mult),
             reads=list(deps) + [d_rs], writes=[d_o])


def merge_stage(P, I, oaT, olT, omT, yT):
    with ExitStack() as st:
        R = mk_rings(P, st)
        acts = [P.sbuf(st, [128, 8, T], BF16, f"act{b}") for b in range(3)]
        d_acts = [Dep() for _ in range(3)]
        P.dma(acts[0][:], oaT.rearrange("h p t -> p h t"), writes=[d_acts[0]])
        P.dma(acts[1][:], olT.rearrange("(c p) t -> p c t", p=128), writes=[d_acts[1]])
        P.dma(acts[2][:], omT.rearrange("h p t -> p h t"), writes=[d_acts[2]])
        RW = Ring(P, st, [128, 8, 128], BF16, 6, "wbr")
        RG = Ring(P, st, [128, 512], BF16, 4, "gate")
        for nch in range(16):
            wb = []
            for br in range(3):
                w, d_w = RW.get()
                P.dma(w[:], I["w_br"][br].rearrange("(kc p) n -> p kc n", p=128)[:, :, nch * 128:(nch + 1) * 128], writes=[d_w], eng="pool")
                wb.append((w, d_w))
            for (t0, n) in BLKS:
                acc, d_acc = R["f32"].get()
                for br in range(3):
                    ps, d_ps = R["mm"].get()
                    for kc in range(8):
                        P.op("pe", lambda e, kc=kc: e.matmul(ps[:, :n], lhsT=wb[br][0][:, kc, :], rhs=acts[br][:, kc, t0:t0 + n],
                                                             start=(kc == 0), stop=(kc == 7)),
                             reads=[wb[br][1], d_acts[br]], writes=[d_ps], sig=(kc == 7))
                    g, d_g = RG.get()
                    P.dma(g[:, :n], I["gT"][br, nch * 128:(nch + 1) * 128, t0:t0 + n], writes=[d_g])
                    if br == 0:
                        P.op("dve", lambda e: e.tensor_tensor(out=acc[:, :n], in0=ps[:, :n], in1=g[:, :n], op=ALU.mult),
                             reads=[d_ps, d_g], writes=[d_acc])
                    else:
                        tmp, d_tmp = R["f32"].get()
                        P.op("dve", lambda e: e.tensor_tensor(out=tmp[:, :n], in0=ps[:, :n], in1=g[:, :n], op=ALU.mult),
                             reads=[d_ps, d_g], writes=[d_tmp])
                        P.op("pool", lambda e: e.tensor_tensor(out=acc[:, :n], in0=acc[:, :n], in1=tmp[:, :n], op=ALU.add),
                             reads=[d_tmp, d_acc], writes=[d_acc])
                ob, d_ob = R["bf"].get()
                P.op("act", lambda e: e.copy(out=ob[:, :n], in_=acc[:, :n]), reads=[d_acc], writes=[d_ob])
                P.dma(yT[nch, :, t0:t0 + n], ob[:, :n], reads=[d_ob])
        P.barrier()


def bcast_rows(P, R, sel, d_sel, rowsrc, d_rowsrc, gi, dst, d_dst):
    for r in range(2):
        for cb in range(4):
            ps, d_ps = R["aux"].get()
            P.op("pe", lambda e: e.matmul(ps[:, :512], lhsT=sel[0:2, r, :], rhs=rowsrc[0:2, gi, cb * 512:(cb + 1) * 512], start=True, stop=True),
                 reads=[d_sel, d_rowsrc], writes=[d_ps], sig=True)
            P.op("act", lambda e: e.copy(out=dst[r][:, cb * 512:(cb + 1) * 512], in_=ps[:, :512]), reads=[d_ps], writes=[d_dst[r]])


def wout_stage(P, I, yT, x1):
    with ExitStack() as st:
        R = mk_rings(P, st)
        wo = P.sbuf(st, [128, 16, D], BF16, "wo")
        d_wo = Dep()
        wv = I["w_out"].rearrange("(kc p) n -> p kc n", p=128)
        for q4 in range(4):
            P.dma(wo[:, q4 * 4:(q4 + 1) * 4, :], wv[:, q4 * 4:(q4 + 1) * 4, :], writes=[d_wo], eng="pool")
        sel = P.sbuf(st, [2, 2, 128], F32, "sel")
        grow = P.sbuf(st, [2, 2, D], F32, "grow")
        d_sel, d_grow = Dep(), Dep()
        P.dma(sel[:], I["sel"], writes=[d_sel])
        P.dma(grow[:], I["grow"], writes=[d_grow])
        gbc = [P.sbuf(st, [128, D], F32, f"g1bc{r}") for r in range(2)]
        d_gbc = [Dep(), Dep()]
        bcast_rows(P, R, sel, d_sel, grow, d_grow, 0, gbc, d_gbc)
        RX = Ring(P, st, [128, D], F32, 2, "x")
        RY = Ring(P, st, [128, 16, 128], BF16, 2, "yTt")
        for ti in range(18):
            r = 1 if ti < 2 else 0
            xt, d_xt = RX.get()
            P.dma(xt[:], I["xin"][ti * 128:(ti + 1) * 128, :], writes=[d_xt])
            yt, d_yt = RY.get()
            P.dma(yt[:], yT[:, :, ti * 128:(ti + 1) * 128].rearrange("c p t -> p c t"), writes=[d_yt])
            for cb in range(4):
                ps, d_ps = R["mm"].get()
                for kc in range(16):
                    P.op("pe", lambda e, kc=kc: e.matmul(ps[:, :512], lhsT=yt[:, kc, :], rhs=wo[:, kc, cb * 512:(cb + 1) * 512],
                                                         start=(kc == 0), stop=(kc == 15)),
                         reads=[d_yt, d_wo], writes=[d_ps], sig=(kc == 15))
                t, d_t = R["f32"].get()
                P.op("dve", lambda e: e.tensor_tensor(out=t[:, :512], in0=ps[:, :512], in1=gbc[r][:, cb * 512:(cb + 1) * 512], op=ALU.mult),
                     reads=[d_ps, d_gbc[r]], writes=[d_t])
                P.op("pool", lambda e: e.tensor_tensor(out=xt[:, cb * 512:(cb + 1) * 512], in0=xt[:, cb * 512:(cb + 1) * 512],
                                                       in1=t[:, :512], op=ALU.add), reads=[d_t, d_xt], writes=[d_xt])
            P.dma(x1[ti * 128:(ti + 1) * 128, :], xt[:], reads=[d_xt])
        P.barrier()


def peer_stage(P, I, x1, xout, last):
    S = P.dram("peerS", [T, 16, 128], F32)
    AB = P.dram("peerAB", [T, 2, 8, 128], F32)
    ZI = P.dram("peerZI", [T, 8], F32)
    h2T = P.dram("h2T", [16, 128, T], BF16)
    ubf = P.dram("ubf", [D, NEXP], BF16)
    vbf = P.dram("vbf", [NEXP, D], BF16)
    for i in range(16):
        P.dma(ubf[i * 128:(i + 1) * 128, :], I["peer_uT"][i * 128:(i + 1) * 128, :], eng="pool")
        P.dma(vbf[i * 1024:(i + 1) * 1024, :], I["peer_v"][i * 1024:(i + 1) * 1024, :], eng="pool")
    with ExitStack() as st:
        C = load_consts(P, st, I)
        hT = P.sbuf(st, [128, 16, T], BF16, "h2Ts")
        d_hT = [(Dep(), Dep()) for _ in range(18)]
        mfm = P.sbuf(st, [128, 96, 2], F32, "mfm")
        n2g = P.sbuf(st, [128, 16], F32, "n2g")
        gm2 = P.sbuf(st, [128, 16, 2], F32, "gm2")
        d_mfm, d_n2g, d_gm2 = Dep(), Dep(), Dep()
        P.dma(mfm[:], I["mfm"], writes=[d_mfm])
        P.dma(n2g[:], I["n2g"], writes=[d_n2g])
        P.op("dve", lambda e: e.tensor_scalar(out=gm2[:], in0=mfm[:, 64:80, :], scalar1=1.0, scalar2=None, op0=ALU.add),
             reads=[d_mfm], writes=[d_gm2])
        P.op("dve", lambda e: e.tensor_tensor(out=gm2[:], in0=gm2[:], in1=n2g[:].unsqueeze(2).to_broadcast([128, 16, 2]), op=ALU.mult),
             reads=[d_gm2, d_n2g], writes=[d_gm2])
        norm_to_fm(P, st, C, x1, hT, d_hT, gm2, d_gm2, mfm, d_mfm, 3)
        for kc in range(16):
            P.dma(h2T[kc], hT[:, kc, :], reads=[x for p in d_hT for x in p])
        P.barrier()
    with ExitStack() as st:
        R = mk_rings(P, st)
        kT = P.sbuf(st, [128, 16, 128], F32, "k12T")
        d_kT = Dep()
        P.dma(kT[:], I["k12T"].rearrange("c d n -> d c n"), writes=[d_kT])
        wq = P.sbuf(st, [128, 16, D], BF16, "wq")
        d_wq = Dep()
        wv = I["peer_wq"].rearrange("(kc p) n -> p kc n", p=128)
        for q4 in range(4):
            P.dma(wq[:, q4 * 4:(q4 + 1) * 4, :], wv[:, q4 * 4:(q4 + 1) * 4, :], writes=[d_wq], eng="pool")
        RH = Ring(P, st, [128, 16, 512], BF16, 2, "h2blk")
        RSC = Ring(P, st, [128, 4, 16, 128], F32, 1, "sc")
        for (t0, n) in BLKS:
            hb, d_hb = RH.get()
            P.dma(hb[:, :, :n], h2T[:, :, t0:t0 + n].rearrange("c p t -> p c t"), writes=[d_hb])
            sc, d_sc = RSC.get()
            for c in range(16):
                ps, d_ps = R["mm"].get()
                for kc in range(16):
                    P.op("pe", lambda e, kc=kc: e.matmul(ps[:, :n], lhsT=wq[:, kc, c * 128:(c + 1) * 128], rhs=hb[:, kc, :n],
                                                         start=(kc == 0), stop=(kc == 15)),
                         reads=[d_wq, d_hb], writes=[d_ps], sig=(kc == 15))
                qc, d_qc = R["f32"].get()
                P.op("act", lambda e: e.copy(out=qc[:, :n], in_=ps[:, :n]), reads=[d_ps], writes=[d_qc])
                for tt in range(n // 128):
                    ps2, d_ps2 = R["aux"].get()
                    P.op("pe", lambda e: e.matmul(ps2[:, :128], lhsT=qc[:, tt * 128:(tt + 1) * 128], rhs=kT[:, c, :], start=True, stop=True),
                         reads=[d_qc, d_kT], writes=[d_ps2], sig=True)
                    P.op("dve", lambda e: e.tensor_copy(out=sc[:, tt, c, :], in_=ps2[:, :128]), reads=[d_ps2], writes=[d_sc])
            for tt in range(n // 128):
                P.dma(S[t0 + tt * 128:t0 + (tt + 1) * 128], sc[:, tt], reads=[d_sc])
        P.barrier()
    NEG = -1.0e30
    with ExitStack() as st:
        RSC = Ring(P, st, [128, 16, 128], F32, 2, "sct")
        RAB = Ring(P, st, [128, 2, 8, 128], F32, 2, "abt")
        RZ = Ring(P, st, [128, 8], F32, 2, "zit")
        RW = Ring(P, st, [128, 256], F32, 4, "wk")
        RV = Ring(P, st, [128, 2, 16], F32, 2, "v12")
        RM = Ring(P, st, [128, 32], F32, 4, "m8")
        for ti in range(18):
            sc, d_sc = RSC.get()
            P.dma(sc[:], S[ti * 128:(ti + 1) * 128], writes=[d_sc])
            ab, d_ab = RAB.get()
            zi, d_zi = RZ.get()
            for h in range(8):
                v12, d_v = RV.get()
                for half in range(2):
                    s = sc[:, 2 * h + half, :]
                    wk, d_wk = RW.get()
                    P.op("dve", lambda e: e.max(out=v12[:, half, 0:8], in_=s), reads=[d_sc], writes=[d_v])
                    P.op("dve", lambda e: e.match_replace(out=wk[:, :128], in_to_replace=v12[:, half, 0:8], in_values=s, imm_value=NEG),
                         reads=[d_sc, d_v], writes=[d_wk])
                    P.op("dve", lambda e: e.max(out=v12[:, half, 8:16], in_=wk[:, :128]), reads=[d_wk], writes=[d_v])
                cand, d_cand = RW.get()
                P.op("dve", lambda e: e.tensor_tensor(out=cand[:].rearrange("p (a b) -> p a b", a=16),
                                                      in0=v12[:, 0, :].unsqueeze(2).to_broadcast([128, 16, 16]),
                                                      in1=v12[:, 1, :].unsqueeze(1).to_broadcast([128, 16, 16]), op=ALU.add),
                     reads=[d_v], writes=[d_cand])
                m8, d_m8 = RM.get()
                wk2, d_wk2 = RW.get()
                P.op("dve", lambda e: e.max(out=m8[:, 0:8], in_=cand[:]), reads=[d_cand], writes=[d_m8])
                P.op("dve", lambda e: e.match_replace(out=wk2[:], in_to_replace=m8[:, 0:8], in_values=cand[:], imm_value=NEG),
                     reads=[d_cand, d_m8], writes=[d_wk2])
                P.op("dve", lambda e: e.max(out=m8[:, 8:16], in_=wk2[:]), reads=[d_wk2], writes=[d_m8])
                P.op("dve", lambda e: e.match_replace(out=wk2[:], in_to_replace=m8[:, 8:16], in_values=wk2[:], imm_value=NEG),
                     reads=[d_m8, d_wk2], writes=[d_wk2])
                P.op("dve", lambda e: e.max(out=m8[:, 16:24], in_=wk2[:]), reads=[d_wk2], writes=[d_m8])
                P.op("dve", lambda e: e.tensor_tensor(out=m8[:, 25:26], in0=m8[:, 15:16], in1=m8[:, 16:17], op=ALU.add),
                     reads=[d_m8], writes=[d_m8])
                P.op("dve", lambda e: e.tensor_scalar(out=m8[:, 24:25], in0=m8[:, 25:26], scalar1=-0.5, scalar2=None, op0=ALU.mult),
                     reads=[d_m8], writes=[d_m8])
                ex, d_ex = RM.get()
                P.op("act", lambda e: e.activation(out=ex[:, 0:16], in_=m8[:, 0:16], func=AF.Exp, bias=m8[:, 24:25], accum_out=ex[:, 16:17]),
                     reads=[d_m8], writes=[d_ex])
                P.op("dve", lambda e: e.reciprocal(out=zi[:, h:h + 1], in_=ex[:, 16:17]), reads=[d_ex], writes=[d_zi])
                P.op("dve", lambda e: e.tensor_scalar(out=ab[:, 0, h, :], in0=sc[:, 2 * h, :], scalar1=m8[:, 24:25], scalar2=None, op0=ALU.add),
                     reads=[d_sc, d_m8], writes=[d_ab])
                P.op("pool", lambda e: e.tensor_copy(out=ab[:, 1, h, :], in_=sc[:, 2 * h + 1, :]), reads=[d_sc], writes=[d_ab])
            P.dma(AB[ti * 128:(ti + 1) * 128], ab[:], reads=[d_ab])
            P.dma(ZI[ti * 128:(ti + 1) * 128], zi[:], reads=[d_zi])
        P.barrier()
    with ExitStack() as st:
        C = load_consts(P, st, I)
        RO = Ring(P, st, [128, 512], F32, 4, "po", psum=True)
        RA = Ring(P, st, [128, 512], F32, 2, "paT", psum=True)
        RG = Ring(P, st, [128, 512], F32, 2, "pgT", psum=True)
        RU = Ring(P, st, [128, 16, 512], BF16, 2, "ublk")
        RVB = Ring(P, st, [128, 4, D], BF16, 2, "vblk")
        RSP = Ring(P, st, [128, 8, 4, 128], F32, 1, "sp")
        RE = Ring(P, st, [128, 8, 4, 128], BF16, 1, "ee")
        RGH = Ring(P, st, [128, 8, 4, 128], BF16, 2, "gh")
        RGL = Ring(P, st, [128, 512], F32, 2, "gelu")
        RWA = Ring(P, st, [128, 4, 128], BF16, 2, "wact")
        RAB = Ring(P, st, [128, 2, 8, 128], F32, 1, "ab")
        RZ = Ring(P, st, [128, 8], F32, 2, "zi")
        RDZ = Ring(P, st, [128, 8, 128], BF16, 2, "dz")
        RH = Ring(P, st, [128, 16, 128], BF16, 2, "h2t")
        RX = Ring(P, st, [128, D], F32, 2, "x1t")
        RF = Ring(P, st, [128, 512], F32, 3, "tmpf")
        RS8 = Ring(P, st, [128, 8], F32, 4, "s8")
        sel = P.sbuf(st, [2, 2, 128], F32, "sel")
        grow = P.sbuf(st, [2, 2, D], F32, "grow")
        d_sel, d_grow = Dep(), Dep()
        P.dma(sel[:], I["sel"], writes=[d_sel])
        P.dma(grow[:], I["grow"], writes=[d_grow])
        gbc = [P.sbuf(st, [128, D], F32, f"g2bc{r}") for r in range(2)]
        d_gbc = [Dep(), Dep()]
        Rtmp = {"aux": RA}
        bcast_rows(P, Rtmp, sel, d_sel, grow, d_grow, 1, gbc, d_gbc)
        if last:
            fg = P.sbuf(st, [1, D], F32, "fg")
            d_fg = Dep()
            P.dma(fg[:], I["fing"], writes=[d_fg])
            fgbc = P.sbuf(st, [128, D], F32, "fgbc")
            d_fgbc = Dep()
            for cb in range(4):
                ps, d_ps = RA.get()
                P.op("pe", lambda e: e.matmul(ps[:, :512], lhsT=C.onesf[0:1, :], rhs=fg[0:1, cb * 512:(cb + 1) * 512], start=True, stop=True),
                     reads=[C.d_onesf, d_fg], writes=[d_ps], sig=True)
                P.op("act", lambda e: e.copy(out=fgbc[:, cb * 512:(cb + 1) * 512], in_=ps[:, :512]), reads=[d_ps], writes=[d_fgbc])
        uv = ubf.rearrange("(kc p) e -> p kc e", p=128)
        vv = vbf.rearrange("(c p) d -> p c d", p=128)
        for ti in range(18):
            r = 1 if ti < 2 else 0
            ht, d_ht = RH.get()
            P.dma(ht[:], h2T[:, :, ti * 128:(ti + 1) * 128].rearrange("c p t -> p c t"), writes=[d_ht])
            ab, d_ab = RAB.get()
            P.dma(ab[:], AB[ti * 128:(ti + 1) * 128], writes=[d_ab])
            zi, d_zi = RZ.get()
            P.dma(zi[:], ZI[ti * 128:(ti + 1) * 128], writes=[d_zi])
            dz, d_dz = RDZ.get()
            for h in range(8):
                P.op("pool", lambda e, h=h: e.tensor_scalar(out=dz[:, h, :], in0=C.identf[:], scalar1=zi[:, h:h + 1], scalar2=None, op0=ALU.mult),
                     reads=[C.d_identf, d_zi], writes=[d_dz])
            pos = [RO.get() for _ in range(4)]

            def stageA(eb):
                sp, d_sp = RSP.get()
                P.op("pool", lambda e: e.tensor_tensor(out=sp[:],
                                                       in0=ab[:, 0, :, eb * 4:(eb + 1) * 4].unsqueeze(3).to_broadcast([128, 8, 4, 128]),
                                                       in1=ab[:, 1, :, :].unsqueeze(2).to_broadcast([128, 8, 4, 128]), op=ALU.add),
                     reads=[d_ab], writes=[d_sp])
                ee, d_ee = RE.get()
                P.op("act", lambda e: e.activation(out=ee[:], in_=sp[:], func=AF.Exp), reads=[d_sp], writes=[d_ee])
                gh, d_gh = RGH.get()
                P.op("dve", lambda e: e.scalar_tensor_tensor(out=gh[:], in0=sp[:], scalar=0.0, in1=ee[:], op0=ALU.is_ge, op1=ALU.mult),
                     reads=[d_sp, d_ee], writes=[d_gh])
                return gh, d_gh

            def stageB(eb):
                ub, d_ub = RU.get()
                P.dma(ub[:], uv[:, :, eb * 512:(eb + 1) * 512], writes=[d_ub])
                vb, d_vb = RVB.get()
                P.dma(vb[:], vv[:, eb * 4:(eb + 1) * 4, :], writes=[d_vb])
                pa, d_pa = RA.get()
                for c in range(4):
                    for kc in range(16):
                        P.op("pe", lambda e, c=c, kc=kc: e.matmul(pa[:, c * 128:(c + 1) * 128], lhsT=ub[:, kc, c * 128:(c + 1) * 128], rhs=ht[:, kc, :],
                                                                  start=(kc == 0), stop=(kc == 15)),
                             reads=[d_ub, d_ht], writes=[d_pa], sig=(c == 3 and kc == 15))
                gl, d_gl = RGL.get()
                P.op("act", lambda e: e.activation(out=gl[:], in_=pa[:], func=AF.Gelu), reads=[d_pa], writes=[d_gl])
                return gl, d_gl, vb, d_vb

            nA = stageA(0)
            nB = stageB(0)
            for eb in range(32):
                gh, d_gh = nA
                gl, d_gl, vb, d_vb = nB
                if eb + 1 < 32:
                    nA = stageA(eb + 1)
                pg, d_pg = RG.get()
                for c in range(4):
                    for h in range(8):
                        P.op("pe", lambda e, c=c, h=h: e.matmul(pg[:, c * 128:(c + 1) * 128], lhsT=gh[:, h, c, :], rhs=dz[:, h, :],
                                                                start=(h == 0), stop=(h == 7)),
                             reads=[d_gh, d_dz], writes=[d_pg], sig=(c == 3 and h == 7))
                if eb + 1 < 32:
                    nB = stageB(eb + 1)
                wa, d_wa = RWA.get()
                P.op("dve", lambda e: e.tensor_tensor(out=wa[:].rearrange("p c t -> p (c t)"), in0=pg[:], in1=gl[:], op=ALU.mult),
                     reads=[d_pg, d_gl], writes=[d_wa])
                for db in range(4):
                    po, d_po = pos[db]
                    for c in range(4):
                        P.op("pe", lambda e, c=c, db=db, po=po: e.matmul(po[:, :512], lhsT=wa[:, c, :], rhs=vb[:, c, db * 512:(db + 1) * 512],
                                                                         start=(eb == 0 and c == 0), stop=(eb == 31 and c == 3)),
                             reads=[d_wa, d_vb], writes=[d_po], sig=(c == 3))
            xt, d_xt = RX.get()
            P.dma(xt[:], x1[ti * 128:(ti + 1) * 128, :], writes=[d_xt])
            for db in range(4):
                po, d_po = pos[db]
                t, d_t = RF.get()
                P.op("dve", lambda e: e.tensor_tensor(out=t[:, :512], in0=po[:, :512], in1=gbc[r][:, db * 512:(db + 1) * 512], op=ALU.mult),
                     reads=[d_po, d_gbc[r]], writes=[d_t])
                P.op("pool", lambda e: e.tensor_tensor(out=xt[:, db * 512:(db + 1) * 512], in0=xt[:, db * 512:(db + 1) * 512], in1=t[:, :512], op=ALU.add),
                     reads=[d_t, d_xt], writes=[d_xt])
            if last:
                s8, d_s8 = RS8.get()
                xo, d_xo = RX.get()
                P.op("act", lambda e: e.activation(out=xo[:], in_=xt[:], func=AF.Square, accum_out=s8[:, 0:1]), reads=[d_xt], writes=[d_xo, d_s8])
                P.op("act", lambda e: e.activation(out=s8[:, 1:2], in_=s8[:, 0:1], func=AF.Sqrt, scale=1.0 / D, bias=EPS), reads=[d_s8], writes=[d_s8])
                P.op("dve", lambda e: e.reciprocal(out=s8[:, 2:3], in_=s8[:, 1:2]), reads=[d_s8], writes=[d_s8])
                P.op("dve", lambda e: e.scalar_tensor_tensor(out=xo[:], in0=xt[:], scalar=s8[:, 2:3], in1=fgbc[:], op0=ALU.mult, op1=ALU.mult),
                     reads=[d_xt, d_s8, d_fgbc, d_xo], writes=[d_xo])
                P.dma(xout[ti * 128:(ti + 1) * 128, :], xo[:], reads=[d_xo])
            else:
                P.dma(xout[ti * 128:(ti + 1) * 128, :], xt[:], reads=[d_xt])
        P.barrier()


def stage2(P, I, O, last):
    oaT = P.dram("oaT", [8, 128, T], BF16)
    omT = P.dram("omT", [8, 128, T], BF16)
    olT = P.dram("olT", [1024, T], BF16)
    yT = P.dram("yT", [16, 128, T], BF16)
    x1 = P.dram("x1", [T, D], F32)
    attention(P, I, oaT, omT)
    gla_stage2(P, I, olT)
    merge_stage(P, I, oaT, olT, omT, yT)
    wout_stage(P, I, yT, x1)
    peer_stage(P, I, x1, O["xout"], last)


def build_s2(last):
    P = Prog()
    I = {k: P.dram(k, sh, dt, kind="ExternalInput") for k, (sh, dt) in S2_INPUTS.items()}
    O = {k: P.dram(k, sh, dt, kind="ExternalOutput") for k, (sh, dt) in S2_OUTPUTS.items()}
    stage2(P, I, O, last)
    return P.finish(), P


OWN_KEYS = ["mfm", "grow", "qTa", "mqT", "lqT", "lkT", "lk", "lv", "lgT", "laf", "lab", "gT"]


def s2_inputs(core, layer, xin_core, s1out, inp, shared):
    b, q = core // 4, core % 4
    L = layer
    own = s1out[core]
    grp = [s1out[b * 4 + j] for j in range(4)]
    m = {"xin": xin_core}
    for k in OWN_KEYS:
        m[k] = own[k]
    m["kTa_all"] = np.ascontiguousarray(np.concatenate([own["kTa"][:, :, :NCTX]] + [g["kTa"][:, :, NCTX:] for g in grp], axis=2))
    m["mkT_all"] = np.ascontiguousarray(np.concatenate([own["mkT"][:, :, :NCTX]] + [g["mkT"][:, :, NCTX:] for g in grp], axis=2))
    m["mkrT_all"] = np.ascontiguousarray(np.concatenate([own["mkrT"][:, :NCTX]] + [g["mkrT"][:, NCTX:] for g in grp], axis=1))
    m["va_all"] = np.ascontiguousarray(np.concatenate([own["va"][:NCTX]] + [g["va"][NCTX:] for g in grp], axis=0))
    m["vm_all"] = np.ascontiguousarray(np.concatenate([own["vm"][:NCTX]] + [g["vm"][NCTX:] for g in grp], axis=0))
    m["glaC"] = own["glaC"]
    A3 = np.ones((2, 3, 128, 4), np.float32)
    U3 = np.zeros((2, 3, 4, 128, 256), np.float32)
    for k in range(3):
        qq = q - 3 + k
        if qq >= 0:
            A3[0, k] = grp[qq]["glaA"][0][:, 0::4]
            U3[0, k] = grp[qq]["glaU"][0]
        qq = q + 3 - k
        if qq <= 3:
            A3[1, k] = grp[qq]["glaA"][1][:, 0::4]
            U3[1, k] = grp[qq]["glaU"][1]
    m["glaA3"], m["glaU3"] = A3, U3
    m.update(shared)
    return m


def s2_shared(layer, inp):
    L = layer
    sh = {}
    sh["w_br"] = np.ascontiguousarray(np.stack([inp["w_br_gqa"][L], inp["w_br_gla"][L], inp["w_br_mla"][L]], 0))
    sh["w_out"] = inp["w_out"][L]
    sh["n2g"] = _fm16(inp["norm2_g"][L])
    sh["gon"] = np.ascontiguousarray(inp["gla_on_g"][L].reshape(2, 128).T)
    sh["peer_wq"] = inp["peer_wq"][L]
    k12 = np.stack([inp["peer_k1"][L], inp["peer_k2"][L]], 1)
    sh["k12T"] = np.ascontiguousarray(k12.reshape(16, 128, 128).transpose(0, 2, 1))
    sh["peer_uT"] = np.ascontiguousarray(inp["peer_u"][L].T)
    sh["peer_v"] = inp["peer_v"][L]
    sh["fing"] = np.ascontiguousarray(inp["final_g"].reshape(1, D))
    sel = np.zeros((2, 2, 128), np.float32)
    sel[0, 0] = 1.0
    sel[1, 1] = 1.0
    sh["sel"] = sel
    c = consts_np()
    for k in ("ident", "ones", "triF", "triB"):
        sh[k] = c[k]
    return sh


_PROGS = {}


def _prog(name):
    if name not in _PROGS:
        if name == "s1":
            _PROGS[name] = build_s1()[0]
        elif name == "s2":
            _PROGS[name] = build_s2(False)[0]
        else:
            _PROGS[name] = build_s2(True)[0]
    return _PROGS[name]


def kernel_unfused(**inputs):
    inp = {k: np.asarray(v) for k, v in inputs.items()}
    xs = [np.asarray(inp["x"][b], np.float32) for b in range(2)]
    ctxs = [np.asarray(inp["ctx"][b], np.float32) for b in range(2)]
    cores = list(range(8))
    for L in range(2):
        maps1 = [s1_inputs(c, L, xs, ctxs, inp) for c in cores]
        r1 = run_bass_kernel_spmd(_prog("s1"), maps1, core_ids=cores)
        s1out = r1.results
        shared = s2_shared(L, inp)
        maps2 = [s2_inputs(c, L, maps1[c]["xin"], s1out, inp, shared) for c in cores]
        del maps1
        r2 = run_bass_kernel_spmd(_prog("s2" if L == 0 else "s2last"), maps2, core_ids=cores)
        del maps2
        new_xs = []
        for b in range(2):
            new_xs.append(np.concatenate([np.asarray(r2.results[b * 4 + j]["xout"])[NCTX:] for j in range(4)], 0))
            ctxs[b] = np.asarray(r2.results[b * 4]["xout"])[:NCTX]
        xs = new_xs
    return np.stack(xs, 0).astype(np.float32)


GROUPS4 = [[0, 1, 2, 3], [4, 5, 6, 7]]
NCHB = 164
NCHF = 17

S1_W = ["w_mod", "bmodT", "bmod_g", "n1g", "w_in", "gq", "gk", "waf", "wab", "gmq", "gmkv", "wuq", "wukv"]
S2_W = ["w_br", "w_out", "n2g", "gon", "peer_wq", "k12T", "peer_uT", "peer_v"]
SHARED_IN = ["cT", "cosA", "sinA", "cosM", "sinM", "ident", "ones", "rotA", "rotM", "triF", "triB", "sel", "fing"]


def exchange(P, O1, X):
    sendB = P.dram("sendB", [NCHB, 128, 256], BF16)
    recvB = P.dram("recvB", [NCHB, 512, 256], BF16)
    sendF = P.dram("sendF", [NCHF, 128, 128], F32)
    recvF = P.dram("recvF", [NCHF, 512, 128], F32)
    d_s, d_r = Dep(), Dep()
    L0 = NCTX
    for kvh in range(2):
        P.dma(sendB[kvh * 8:(kvh + 1) * 8], O1["kTa"][kvh][:, L0:].rearrange("p (t c) -> t p c", c=256), writes=[d_s])
    P.dma(sendB[16:32], O1["va"][L0:, :].rearrange("(i p) c -> i p c", p=128), writes=[d_s])
    for h in range(8):
        P.dma(sendB[32 + h * 8:40 + h * 8], O1["mkT"][h][:, L0:].rearrange("p (t c) -> t p c", c=256), writes=[d_s])
    vmv = sendB[96:160].rearrange("(i q) p c -> i q p c", q=4)
    for cq in range(4):
        P.dma(vmv[:, cq], O1["vm"][L0:, cq * 256:(cq + 1) * 256].rearrange("(i p) c -> i p c", p=128), writes=[d_s])
    for j in range(4):
        P.dma(sendB[160 + j].rearrange("(h p) c -> h p c", h=2),
              O1["mkrT"][:, L0 + j * 512:L0 + (j + 1) * 512].rearrange("p (h c) -> h p c", h=2), writes=[d_s])
    for d in range(2):
        for h in range(4):
            P.dma(sendF[(d * 4 + h) * 2:(d * 4 + h) * 2 + 2], O1["glaU"][d, h].rearrange("p (f c) -> f p c", f=2), writes=[d_s])
    P.dma(sendF[16][:, 0:32].rearrange("p (d c) -> p d c", d=2), O1["glaA"].rearrange("d p c -> p d c"), writes=[d_s])
    recs = []
    WIN = 4

    def gather(src, dst):
        if len(recs) >= WIN:
            P._wait("pool", recs[-WIN])
        recs.append(P.collective("AllGather", GROUPS4, src, dst, reads=[d_s], writes=[Dep()]))
    for i in range(NCHB):
        gather(sendB[i].bitcast(F32), recvB[i].bitcast(F32))
    for i in range(NCHF):
        gather(sendF[i], recvF[i])
    d_r.w = recs[-1]
    P.dma(X["kTa_all"][:, :, 0:L0], O1["kTa"][:, :, 0:L0])
    P.dma(X["mkT_all"][:, :, 0:L0], O1["mkT"][:, :, 0:L0])
    P.dma(X["mkrT_all"][:, 0:L0], O1["mkrT"][:, 0:L0])
    P.dma(X["va_all"][0:L0, :], O1["va"][0:L0, :])
    P.dma(X["vm_all"][0:L0, :], O1["vm"][0:L0, :])
    for r in range(4):
        rr = slice(r * 128, (r + 1) * 128)
        t0 = L0 + r * NLAT
        for kvh in range(2):
            P.dma(X["kTa_all"][kvh][:, t0:t0 + NLAT].rearrange("p (t c) -> t p c", c=256), recvB[kvh * 8:(kvh + 1) * 8, rr, :], reads=[d_r])
        for h in range(8):
            P.dma(X["mkT_all"][h][:, t0:t0 + NLAT].rearrange("p (t c) -> t p c", c=256), recvB[32 + h * 8:40 + h * 8, rr, :], reads=[d_r])
        P.dma(X["va_all"][t0:t0 + NLAT, :].rearrange("(i p) c -> i p c", p=128), recvB[16:32, rr, :], reads=[d_r])
        rv = recvB[96:160].rearrange("(i q) m c -> i q m c", q=4)
        for cq in range(4):
            P.dma(X["vm_all"][t0:t0 + NLAT, cq * 256:(cq + 1) * 256].rearrange("(i p) c -> i p c", p=128), rv[:, cq, rr, :], reads=[d_r])
        for j in range(4):
            P.dma(X["mkrT_all"][:, t0 + j * 512:t0 + (j + 1) * 512].rearrange("p (h c) -> h p c", h=2),
                  recvB[160 + j, rr, :].rearrange("(h p) c -> h p c", h=2), reads=[d_r])
    P.barrier()
    return recvF


def fused_input_specs():
    specs = {"xin0": ([T, D], F32), "qmask": ([128, 16], F32)}
    for k in SHARED_IN:
        specs[k] = (S1_INPUTS.get(k) or S2_INPUTS[k])
    for L in range(2):
        for k in S1_W:
            specs[f"{k}_{L}"] = S1_INPUTS[k]
        for k in S2_W:
            specs[f"{k}_{L}"] = S2_INPUTS[k]
    return specs


def build_fused():
    P = Prog()
    specs = fused_input_specs()
    E = {k: P.dram(k, sh, dt, kind="ExternalInput") for k, (sh, dt) in specs.items()}
    xout = P.dram("xout", [T, D], F32, kind="ExternalOutput")
    xmid = P.dram("xmid", [T, D], F32)
    O1 = {k: P.dram("s1_" + k, sh, dt) for k, (sh, dt) in S1_OUTPUTS.items()}
    X = {k: P.dram(k, S2_INPUTS[k][0], S2_INPUTS[k][1]) for k in ("kTa_all", "va_all", "mkT_all", "mkrT_all", "vm_all")}
    for L in range(2):
        I1 = {k: E[k] for k in SHARED_IN if k in S1_INPUTS}
        I1["xin"] = E["xin0"] if L == 0 else xmid
        for k in S1_W:
            I1[k] = E[f"{k}_{L}"]
        stage1(P, I1, O1)
        recvF = exchange(P, O1, X)
        I2 = {k: E[k] for k in SHARED_IN if k in S2_INPUTS}
        I2["xin"] = I1["xin"]
        for k in OWN_KEYS:
            I2[k] = O1[k]
        I2.update(X)
        I2["recvF"] = recvF
        I2["qmask"] = E["qmask"]
        for k in S2_W:
            I2[k] = E[f"{k}_{L}"]
        stage2(P, I2, {"xout": xmid if L == 0 else xout}, last=(L == 1))
    return P.finish(), P


def fused_inputs(core, inp, shared2):
    b, q = core // 4, core % 4
    m = {}
    xs = [inp["x"][bb] for bb in range(2)]
    ctxs = [inp["ctx"][bb] for bb in range(2)]
    for L in range(2):
        s1 = s1_inputs(core, L, xs, ctxs, inp)
        if L == 0:
            m["xin0"] = s1["xin"]
            for k in SHARED_IN:
                if k in s1:
                    m[k] = s1[k]
        for k in S1_W:
            m[f"{k}_{L}"] = s1[k]
        for k in S2_W:
            m[f"{k}_{L}"] = shared2[L][k]
    m["sel"] = shared2[0]["sel"]
    m["fing"] = shared2[0]["fing"]
    qm = np.zeros((128, 16), np.float32)
    for j in range(4):
        qm[:, j] = 1.0 if j < q else 0.0
        qm[:, 4 + j] = 1.0 if j > q else 0.0
    qm[:, 8:16] = 1.0 - qm[:, 0:8]
    m["qmask"] = qm
    return m


def kernel_fused(**inputs):
    inp = {k: np.asarray(v) for k, v in inputs.items()}
    if "fused" not in _PROGS:
        _PROGS["fused"] = build_fused()[0]
    shared2 = [s2_shared(L, inp) for L in range(2)]
    cores = list(range(8))
    maps = [fused_inputs(c, inp, shared2) for c in cores]
    r = run_bass_kernel_spmd(_PROGS["fused"], maps, core_ids=cores)
    xs = []
    for b in range(2):
        xs.append(np.concatenate([np.asarray(r.results[b * 4 + j]["xout"])[NCTX:] for j in range(4)], 0))
    return np.stack(xs, 0).astype(np.float32)


def kernel(**inputs):
    return kernel_fused(**inputs)
```
